# Optimizing a Trainium2 kernel written in Bass

```python
import jax, jax.numpy as jnp
from jax import lax
import numpy as np

D_MODEL = 1024
BATCH = 32
SEQ = 2048
DEPTH = 2

EPS = 1e-6
N_BRANCH = 3
BRANCH_WIDTH = 512

ATTN_HEADS = 8
ATTN_KV_HEADS = 2
ATTN_HEAD_DIM = 64
ATTN_WIDTH = ATTN_HEADS * ATTN_HEAD_DIM
ATTN_KV_WIDTH = ATTN_KV_HEADS * ATTN_HEAD_DIM
WINDOW = 128
ATTN_BLOCK = 128
ROPE_THETA = 10000.0

GLA_HEADS = 4
GLA_KEY_WIDTH = 256
GLA_VALUE_WIDTH = 512
GLA_DK = GLA_KEY_WIDTH // GLA_HEADS
GLA_DV = GLA_VALUE_WIDTH // GLA_HEADS
GLA_GATE_RANK = 16
GLA_GATE_NORMALIZER = 16.0
GLA_CHUNK = 64

SSD_D_INNER = 512
SSD_HEAD_DIM = 64
SSD_HEADS = SSD_D_INNER // SSD_HEAD_DIM
SSD_GROUPS = 2
SSD_D_STATE = 128
SSD_CONV = 4
SSD_CONV_DIM = SSD_D_INNER + 2 * SSD_GROUPS * SSD_D_STATE
SSD_CHUNK = 64

IN_PROJ_SIZES = (
    ATTN_WIDTH, ATTN_KV_WIDTH, ATTN_KV_WIDTH, ATTN_WIDTH,
    GLA_KEY_WIDTH, GLA_KEY_WIDTH, GLA_VALUE_WIDTH, GLA_VALUE_WIDTH,
    GLA_GATE_RANK,
    SSD_CONV_DIM, SSD_HEADS, SSD_D_INNER,
    N_BRANCH * D_MODEL,
)
IN_PROJ_DIM = sum(IN_PROJ_SIZES)

kernel_name = "hybrid_swa_gla_ssd_gated_merge"


def rms_norm(x, g):
    xf = x.astype(jnp.float32)
    y = xf * lax.rsqrt(jnp.mean(xf * xf, axis=-1, keepdims=True) + EPS)
    return (y * g.astype(jnp.float32)).astype(x.dtype)


def rope_tables(positions):
    inv_freq = ROPE_THETA ** (-jnp.arange(0, ATTN_HEAD_DIM, 2, dtype=jnp.float32) / ATTN_HEAD_DIM)
    ang = positions.astype(jnp.float32)[..., None] * inv_freq
    return jnp.cos(ang)[:, :, None, :], jnp.sin(ang)[:, :, None, :]


def apply_rope(t, cos, sin):
    tf = t.astype(jnp.float32)
    t1, t2 = jnp.split(tf, 2, axis=-1)
    return jnp.concatenate([t1 * cos - t2 * sin, t2 * cos + t1 * sin], axis=-1).astype(t.dtype)


def sliding_window_attention(q, k, v, sinks):
    b, s = q.shape[0], q.shape[1]
    nb = s // ATTN_BLOCK
    grp = ATTN_HEADS // ATTN_KV_HEADS
    qb = q.reshape(b, nb, ATTN_BLOCK, ATTN_KV_HEADS, grp, ATTN_HEAD_DIM)

    def band(t):
        tp = jnp.pad(t, ((0, 0), (ATTN_BLOCK, 0), (0, 0), (0, 0)))
        prev = tp[:, :s].reshape(b, nb, ATTN_BLOCK, ATTN_KV_HEADS, ATTN_HEAD_DIM)
        cur = t.reshape(b, nb, ATTN_BLOCK, ATTN_KV_HEADS, ATTN_HEAD_DIM)
        return jnp.concatenate([prev, cur], axis=2)

    kb, vb = band(k), band(v)
    scale = ATTN_HEAD_DIM ** -0.5
    scores = jnp.einsum("bnqhgd,bnkhd->bnhgqk", qb, kb).astype(jnp.float32) * scale
    qi = jnp.arange(ATTN_BLOCK)[:, None]
    ki = jnp.arange(2 * ATTN_BLOCK)[None, :]
    rel = qi + ATTN_BLOCK - ki
    key_pos = jnp.arange(nb)[:, None, None] * ATTN_BLOCK - ATTN_BLOCK + ki
    mask = (rel >= 0) & (rel < WINDOW) & (key_pos >= 0)
    scores = jnp.where(mask[None, :, None, None], scores, jnp.finfo(jnp.float32).min)
    sink = jnp.broadcast_to(sinks.astype(jnp.float32).reshape(1, 1, ATTN_KV_HEADS, grp, 1, 1),
                            scores.shape[:-1] + (1,))
    probs = jax.nn.softmax(jnp.concatenate([scores, sink], axis=-1), axis=-1)[..., :-1]
    out = jnp.einsum("bnhgqk,bnkhd->bnqhgd", probs.astype(v.dtype), vb)
    return out.reshape(b, s, ATTN_WIDTH)


def gated_linear_attention(q, k, v, log_alpha):
    b, s, nh, dk = q.shape
    dv = v.shape[-1]
    c = GLA_CHUNK
    nc = s // c
    f32 = jnp.float32
    qc = (q.astype(f32) * dk ** -0.5).reshape(b, nc, c, nh, dk)
    kc = k.astype(f32).reshape(b, nc, c, nh, dk)
    vc = v.astype(f32).reshape(b, nc, c, nh, dv)
    bcum = jnp.cumsum(log_alpha.astype(f32).reshape(b, nc, c, nh, dk), axis=2)
    q_dec = qc * jnp.exp(bcum)
    k_inv = kc * jnp.exp(-bcum)
    causal = jnp.tril(jnp.ones((c, c), bool))
    attn = jnp.where(causal, jnp.einsum("bnihd,bnjhd->bnhij", q_dec, k_inv), 0.0)
    o_intra = jnp.einsum("bnhij,bnjhv->bnihv", attn, vc)
    b_last = bcum[:, :, -1]
    k_end = kc * jnp.exp(b_last[:, :, None] - bcum)
    chunk_states = jnp.einsum("bnjhd,bnjhv->bnhdv", k_end, vc)
    chunk_decay = jnp.exp(b_last)

    def step(state, inp):
        dec, upd = inp
        return dec[..., None] * state + upd, state

    init = jnp.zeros((b, nh, dk, dv), f32)
    _, prev = lax.scan(step, init, (jnp.moveaxis(chunk_decay, 1, 0), jnp.moveaxis(chunk_states, 1, 0)))
    prev = jnp.moveaxis(prev, 0, 1)
    o_inter = jnp.einsum("bnihd,bnhdv->bnihv", q_dec, prev)
    return (o_intra + o_inter).reshape(b, s, nh, dv)


def causal_depthwise_conv(u, w, bias):
    kw = w.shape[0]
    s = u.shape[1]
    up = jnp.pad(u, ((0, 0), (kw - 1, 0), (0, 0)))
    out = bias
    for i in range(kw):
        out = out + up[:, i:i + s] * w[i]
    return out


def ssd_scan(x, dt, a_head, bmat, cmat):
    b, s, nh, p = x.shape
    g, n = bmat.shape[2], bmat.shape[3]
    hpg = nh // g
    l = SSD_CHUNK
    nc = s // l
    f32 = jnp.float32
    dtf = dt.astype(f32)
    xc = (x.astype(f32) * dtf[..., None]).reshape(b, nc, l, g, hpg, p)
    a_cs = jnp.cumsum((dtf * a_head.astype(f32)).reshape(b, nc, l, g, hpg), axis=2)
    bc = bmat.astype(f32).reshape(b, nc, l, g, n)
    cc = cmat.astype(f32).reshape(b, nc, l, g, n)
    tril = jnp.tril(jnp.ones((l, l), bool))
    seg = a_cs[:, :, :, None] - a_cs[:, :, None, :]
    decay_ls = jnp.exp(jnp.where(tril[:, :, None, None], seg, -jnp.inf))
    cb = jnp.einsum("bclgn,bcsgn->bclsg", cc, bc)
    y_diag = jnp.einsum("bclsgh,bcsghp->bclghp", cb[..., None] * decay_ls, xc)
    decay_to_end = jnp.exp(a_cs[:, :, -1:] - a_cs)
    chunk_states = jnp.einsum("bclgn,bclghp->bcghpn", bc, xc * decay_to_end[..., None])
    chunk_decay = jnp.exp(a_cs[:, :, -1])

    def step(state, inp):
        dec, upd = inp
        return dec[..., None, None] * state + upd, state

    init = jnp.zeros((b, g, hpg, p, n), f32)
    _, prev = lax.scan(step, init, (jnp.moveaxis(chunk_decay, 1, 0), jnp.moveaxis(chunk_states, 1, 0)))
    prev = jnp.moveaxis(prev, 0, 1)
    y_off = jnp.einsum("bclgn,bcghpn->bclghp", cc, prev) * jnp.exp(a_cs)[..., None]
    return (y_diag + y_off).reshape(b, s, nh, p)


def gated_group_rms_norm(y, z, g, groups):
    shp = y.shape
    u = (y.astype(jnp.float32) * jax.nn.silu(z.astype(jnp.float32)))
    u = u.reshape(shp[:-1] + (groups, shp[-1] // groups))
    u = u * lax.rsqrt(jnp.mean(u * u, axis=-1, keepdims=True) + EPS)
    return (u.reshape(shp) * g.astype(jnp.float32)).astype(y.dtype)


def hybrid_layer(x, cos, sin, norm_g, w_in, attn_q_norm, attn_k_norm, attn_sinks,
                 gla_w_gate_up, gla_b_gate, gla_out_norm, ssd_conv_w, ssd_conv_b,
                 ssd_dt_bias, ssd_A_log, ssd_D, ssd_out_norm, w_branch, w_out):
    b, s, _ = x.shape
    h = rms_norm(x, norm_g)
    proj = h @ w_in
    split_points = np.cumsum(IN_PROJ_SIZES)[:-1].tolist()
    (aq, ak, av, ag, gq, gk, gv, gg, gdown, sxbc, sdt, sz, mg) = jnp.split(proj, split_points, axis=-1)

    q = apply_rope(rms_norm(aq.reshape(b, s, ATTN_HEADS, ATTN_HEAD_DIM), attn_q_norm), cos, sin)
    k = apply_rope(rms_norm(ak.reshape(b, s, ATTN_KV_HEADS, ATTN_HEAD_DIM), attn_k_norm), cos, sin)
    v = av.reshape(b, s, ATTN_KV_HEADS, ATTN_HEAD_DIM)
    y_a = sliding_window_attention(q, k, v, attn_sinks) * jax.nn.silu(ag)

    gate_logit = (gdown @ gla_w_gate_up + gla_b_gate).astype(jnp.float32)
    log_alpha = jax.nn.log_sigmoid(gate_logit) / GLA_GATE_NORMALIZER
    o_b = gated_linear_attention(gq.reshape(b, s, GLA_HEADS, GLA_DK),
                                 gk.reshape(b, s, GLA_HEADS, GLA_DK),
                                 gv.reshape(b, s, GLA_HEADS, GLA_DV),
                                 log_alpha.reshape(b, s, GLA_HEADS, GLA_DK)).astype(x.dtype)
    y_b = rms_norm(o_b, gla_out_norm).reshape(b, s, GLA_VALUE_WIDTH) * jax.nn.silu(gg)

    xbc = jax.nn.silu(causal_depthwise_conv(sxbc, ssd_conv_w, ssd_conv_b))
    sx, sb, sc = jnp.split(xbc, [SSD_D_INNER, SSD_D_INNER + SSD_GROUPS * SSD_D_STATE], axis=-1)
    dt = jax.nn.softplus((sdt + ssd_dt_bias).astype(jnp.float32))
    a_head = -jnp.exp(ssd_A_log.astype(jnp.float32))
    xh = sx.reshape(b, s, SSD_HEADS, SSD_HEAD_DIM)
    y_c = ssd_scan(xh, dt, a_head,
                   sb.reshape(b, s, SSD_GROUPS, SSD_D_STATE),
                   sc.reshape(b, s, SSD_GROUPS, SSD_D_STATE))
    y_c = (y_c + xh.astype(jnp.float32) * ssd_D.astype(jnp.float32)[:, None]).astype(x.dtype)
    y_c = gated_group_rms_norm(y_c.reshape(b, s, SSD_D_INNER), sz, ssd_out_norm, SSD_GROUPS)

    branches = jnp.stack([y_a, y_b, y_c], axis=2)
    u = jnp.einsum("bsnw,nwd->bsnd", branches, w_branch)
    gates = jax.nn.sigmoid(mg.reshape(b, s, N_BRANCH, D_MODEL))
    merged = jnp.sum(gates * u, axis=2)
    return x + merged @ w_out


def setup_inputs(seed: int = 0) -> dict:
    key = jax.random.key(seed)
    ks = jax.random.split(key, 20)
    f32 = jnp.float32
    nrm = lambda k, shp: jax.random.normal(k, shp, f32)
    x = nrm(ks[0], (BATCH, SEQ, D_MODEL))
    offset = jax.random.randint(ks[1], (BATCH, 1), 0, 4096, dtype=jnp.int32)
    positions = (jnp.arange(SEQ, dtype=jnp.int32)[None, :] + offset).astype(jnp.int32)
    norm_g = 1.0 + 0.02 * nrm(ks[2], (DEPTH, D_MODEL))
    w_in = nrm(ks[3], (DEPTH, D_MODEL, IN_PROJ_DIM)) * D_MODEL ** -0.5
    attn_q_norm = 1.0 + 0.02 * nrm(ks[4], (DEPTH, ATTN_HEAD_DIM))
    attn_k_norm = 1.0 + 0.02 * nrm(ks[5], (DEPTH, ATTN_HEAD_DIM))
    attn_sinks = 0.5 * nrm(ks[6], (DEPTH, ATTN_HEADS))
    gla_w_gate_up = nrm(ks[7], (DEPTH, GLA_GATE_RANK, GLA_KEY_WIDTH)) * GLA_GATE_RANK ** -0.5
    gla_b_gate = 0.02 * nrm(ks[8], (DEPTH, GLA_KEY_WIDTH))
    gla_out_norm = 1.0 + 0.02 * nrm(ks[9], (DEPTH, GLA_DV))
    ssd_conv_w = nrm(ks[10], (DEPTH, SSD_CONV, SSD_CONV_DIM)) * SSD_CONV ** -0.5
    ssd_conv_b = 0.02 * nrm(ks[11], (DEPTH, SSD_CONV_DIM))
    dt0 = jnp.exp(jax.random.uniform(ks[12], (DEPTH, SSD_HEADS), f32, np.log(1e-3), np.log(1e-1)))
    ssd_dt_bias = dt0 + jnp.log(-jnp.expm1(-dt0))
    ssd_A_log = jnp.log(jax.random.uniform(ks[13], (DEPTH, SSD_HEADS), f32, 1.0, 16.0))
    ssd_D = 1.0 + 0.02 * nrm(ks[14], (DEPTH, SSD_HEADS))
    ssd_out_norm = 1.0 + 0.02 * nrm(ks[15], (DEPTH, SSD_D_INNER))
    w_branch = nrm(ks[16], (DEPTH, N_BRANCH, BRANCH_WIDTH, D_MODEL)) * BRANCH_WIDTH ** -0.5
    w_out = nrm(ks[17], (DEPTH, D_MODEL, D_MODEL)) * D_MODEL ** -0.5
    return {"x": x, "positions": positions, "norm_g": norm_g, "w_in": w_in,
            "attn_q_norm": attn_q_norm, "attn_k_norm": attn_k_norm, "attn_sinks": attn_sinks,
            "gla_w_gate_up": gla_w_gate_up, "gla_b_gate": gla_b_gate, "gla_out_norm": gla_out_norm,
            "ssd_conv_w": ssd_conv_w, "ssd_conv_b": ssd_conv_b, "ssd_dt_bias": ssd_dt_bias,
            "ssd_A_log": ssd_A_log, "ssd_D": ssd_D, "ssd_out_norm": ssd_out_norm,
            "w_branch": w_branch, "w_out": w_out}


def reference(x, positions, norm_g, w_in, attn_q_norm, attn_k_norm, attn_sinks,
              gla_w_gate_up, gla_b_gate, gla_out_norm, ssd_conv_w, ssd_conv_b,
              ssd_dt_bias, ssd_A_log, ssd_D, ssd_out_norm, w_branch, w_out):
    cos, sin = rope_tables(positions)
    for i in range(DEPTH):
        x = hybrid_layer(x, cos, sin, norm_g[i], w_in[i], attn_q_norm[i], attn_k_norm[i],
                         attn_sinks[i], gla_w_gate_up[i], gla_b_gate[i], gla_out_norm[i],
                         ssd_conv_w[i], ssd_conv_b[i], ssd_dt_bias[i], ssd_A_log[i], ssd_D[i],
                         ssd_out_norm[i], w_branch[i], w_out[i])
    return x
```

```python
import math
import os
GLA_STOP = int(os.environ.get('GLA_STOP', '99'))
STRICT = int(os.environ.get('STRICT', '1'))
GLA_VAR = int(os.environ.get('GLA_VAR', '0'))
ATT_PAD = int(os.environ.get('ATT_PAD', '1'))
PIPE = int(os.environ.get('PIPE', '1'))
STAGGER = int(os.environ.get('STAGGER', '1'))
GLA_NOINTER = int(os.environ.get('GLA_NOINTER', '0'))
from contextlib import ExitStack

import numpy as np
import concourse.bass as bass
import concourse.mybir as mybir
from concourse.bass_utils import run_bass_kernel_spmd

F32 = mybir.dt.float32
BF16 = mybir.dt.bfloat16
I32 = mybir.dt.int32
AF = mybir.ActivationFunctionType
ALU = mybir.AluOpType
AX = mybir.AxisListType

D = 1024
KC = 8
NCORES = 8
IN_DIM = 7448
EPS = 1e-6
UNIT = 1024
TPU = UNIT // 128
BPU = UNIT // 512
RING = 6
RING_ELEMS = 4096
TWO_PI = 2.0 * math.pi

C_AQ, C_AK, C_AV, C_AG = 0, 512, 640, 768
C_GQ, C_GK, C_GV, C_GG, C_GD = 1280, 1536, 1792, 2304, 2816
C_XBC, C_DT, C_Z, C_MG = 2832, 3856, 3864, 4376


class Buf:
    __slots__ = ("name", "R", "W", "psum")

    def __init__(self, name, psum=False):
        self.name = name
        self.R = {}
        self.W = {}
        self.psum = psum


class T:
    def __init__(self, t, buf=None, name=None):
        self.t = t
        self.b = buf if buf is not None else Buf(name or "t")

    def __getitem__(self, k):
        return self.t[k]


def _bufs(lst):
    out = []
    for x in lst:
        if x is None:
            continue
        out.append(x.b if isinstance(x, T) else x)
    return out


class Ctx:
    def __init__(self, nc, es, needed=None):
        self.nc = nc
        self.es = es
        self.rank = None
        if needed is not None:
            self.rank = {k: {v: i + 1 for i, v in enumerate(sorted(vs))} for k, vs in needed.items()}
        self.waited = {k: set() for k in ("pe", "act", "dve", "pool")}
        self.E = {"pe": nc.tensor, "act": nc.scalar, "dve": nc.vector, "pool": nc.gpsimd, "sp": nc.sync}
        self.sem = {k: es.enter_context(nc.semaphore("sem_" + k)) for k in ("pe", "act", "dve", "pool")}
        self.cnt = {k: 0 for k in self.sem}
        self.seen = {k: {} for k in self.E}
        self.nsem = 0
        self.epoch_bufs = []
        self.inherit = ({}, {})
        self.n_inst = 0

    def sb(self, name, shape, dt=F32):
        return T(self.es.enter_context(self.nc.sbuf_tensor(name, shape, dt)), name=name)

    def new_sem(self, name):
        self.nsem += 1
        return self.es.enter_context(self.nc.semaphore(name))

    def _wait(self, eng, deps):
        e = self.E[eng]
        seen = self.seen[eng]
        for key, (sem, val) in deps.items():
            if seen.get(key, 0) >= val:
                continue
            if key in self.waited:
                self.waited[key].add(val)
                e.wait_ge(sem, self.rank[key][val] if self.rank is not None else val)
            else:
                e.wait_ge(sem, val)
            seen[key] = val

    def _deps(self, eng, R, W):
        deps = {}

        def add(key, sv):
            if key == eng and eng == "pe":
                return
            cur = deps.get(key)
            if cur is None or cur[1] < sv[1]:
                deps[key] = sv

        for b in R:
            for k, sv in b.W.items():
                add(k, sv)
            if b.psum:
                for k, sv in b.R.items():
                    if k != eng:
                        add(k, sv)
        for b in W:
            for k, sv in b.R.items():
                if k != eng or STRICT:
                    add(k, sv)
            for k, sv in b.W.items():
                if k != eng or STRICT:
                    add(k, sv)
        return deps

    def op(self, eng, fn, R=(), W=()):
        R = _bufs(R)
        W = _bufs(W)
        self._wait(eng, self._deps(eng, R, W))
        inst = fn(self.E[eng])
        self.cnt[eng] += 1
        self.n_inst += 1
        if self.rank is None or self.cnt[eng] in self.rank[eng]:
            inst.then_inc(self.sem[eng], 1)
        sv = (self.sem[eng], self.cnt[eng])
        for b in R:
            b.R[eng] = sv
        for b in W:
            b.W[eng] = sv
        return inst

    def dma(self, q, out, in_, sem, semstate, R=(), W=(), **kw):
        R = _bufs(R)
        W = _bufs(W)
        self._wait(q, self._deps(q, R, W))
        self.E[q].dma_start(out=out, in_=in_, **kw).then_inc(sem, 16)
        semstate[0] += 16
        key = "dma:" + str(id(sem))
        sv = (sem, semstate[0])
        for b in R:
            b.R[key] = sv
        for b in W:
            b.W[key] = sv

    def new_epoch(self):
        R, W = {}, {}
        for b in self.epoch_bufs:
            for src in (b.R, b.W):
                for k, sv in src.items():
                    if k not in R or R[k][1] < sv[1]:
                        R[k] = sv
        for k, sv in self.inherit[0].items():
            if k not in R or R[k][1] < sv[1]:
                R[k] = sv
        self.inherit = (R, dict(R))
        self.epoch_bufs = []

    def scratch_buf(self, name):
        b = Buf(name)
        b.R = dict(self.inherit[0])
        b.W = dict(self.inherit[1])
        self.epoch_bufs.append(b)
        return b


class Arena:
    def __init__(self, C, name, nbytes):
        self.C = C
        self.n32 = nbytes // 4
        self.t = C.es.enter_context(C.nc.sbuf_tensor(name, [128, self.n32], F32))
        self.off = 0

    def reset(self):
        self.off = 0
        self.C.new_epoch()

    def rewind(self, mark, old):
        R = dict(self.C.inherit[0])
        for t in old:
            for src in (t.b.R, t.b.W):
                for k, sv in src.items():
                    if k not in R or R[k][1] < sv[1]:
                        R[k] = sv
        self.C.inherit = (R, dict(R))
        self.off = mark

    def alloc(self, name, nelem, dt=F32, parts=128):
        n32 = nelem if dt in (F32, I32) else (nelem + 1) // 2
        assert self.off + n32 <= self.n32, (name, self.off, n32, self.n32)
        ap = self.t[0:parts, self.off:self.off + n32]
        if dt != F32:
            ap = ap.bitcast(dt)
        self.off += n32
        return T(ap, self.C.scratch_buf(name))


def _v3(ap, a, b):
    return ap.rearrange("p (a b) -> p a b", a=a, b=b)


def _bc_last(ap2, n):
    return ap2.unsqueeze(2).broadcast_to([ap2.shape[0], ap2.shape[1], n])


def _bc_mid(ap2, n):
    return ap2.unsqueeze(1).broadcast_to([ap2.shape[0], n, ap2.shape[1]])


def build1(n_seq, seq_len, depth, dbg=None, needed=None):
    assert seq_len % UNIT == 0
    upseq = seq_len // UNIT
    n_units = n_seq * upseq
    NT = n_seq * seq_len
    nc = bass.Bass("TRN2", target_bir_lowering=False)

    def din(name, shape, dt=F32):
        return nc.dram_tensor(name, shape, dt, kind="ExternalInput")

    xT_d = din("xT", [D, NT]).ap()
    pos_d = din("pos", [n_units, 128, TPU], I32).ap()
    invf_d = din("inv_freq", [1, 32])
    norm_g_d = din("norm_g", [depth, D]).ap()
    w_in_d = din("w_in", [depth, D, IN_DIM]).ap()
    qn_d = din("attn_q_norm", [depth, 64])
    kn_d = din("attn_k_norm", [depth, 64])
    sinks_d = din("attn_sinks", [depth, 8])
    wup_d = din("gla_w_gate_up", [depth, 16, 256]).ap()
    bg_d = din("gla_b_gate", [depth, 256]).ap()
    gon_d = din("gla_out_norm", [depth, 128])
    cw_d = din("ssd_conv_w", [depth, 4, D]).ap()
    cb_d = din("ssd_conv_b", [depth, D]).ap()
    dtb_d = din("ssd_dt_bias", [depth, 8])
    alog_d = din("ssd_A_log", [depth, 8])
    dsk_d = din("ssd_D", [depth, 8])
    son_d = din("ssd_out_norm", [depth, 512])
    wbr_d = din("w_branch", [depth, 3, 512, D]).ap()
    wout_d = din("w_out", [depth, D, D]).ap()
    outT_d = nc.dram_tensor("outT", [D, NT], F32, kind="ExternalOutput").ap()
    dbg_out = {}

    with ExitStack() as es:
        C = Ctx(nc, es, needed)
        es.enter_context(nc.allow_non_contiguous_dma(reason="small parameter layouts"))

        def bcast_src(handle, row, n, reps=None):
            if reps is None:
                return bass.AP(handle, row * n, [[0, 128], [1, n]])
            return bass.AP(handle, row * n, [[0, 128], [0, reps], [1, n]])

        xT = C.sb("xTs", [128, KC * UNIT])
        xT_b = [[Buf("xT%d_%d" % (k, b)) for b in range(BPU)] for k in range(KC)]
        hT = C.sb("hT", [128, KC * UNIT], BF16)
        hT_b = [Buf("hT%d" % b) for b in range(BPU)]
        yT = [C.sb("yT%d" % i, [128, 4 * UNIT], BF16) for i in range(3)]
        ring = [C.sb("ring%d" % i, [128, RING_ELEMS], BF16) for i in range(RING)]
        ring_sem = [C.new_sem("rsem%d" % i) for i in range(RING)]
        ring_cnt = [[0] for _ in range(RING)]
        arena = Arena(C, "arena", 49 * 1024)

        def xv(k, b):
            return xT[:, k * UNIT + b * 512: k * UNIT + (b + 1) * 512]

        def hv(k, lo, n):
            return hT[:, k * UNIT + lo: k * UNIT + lo + n]

        ones_bf = C.sb("ones_bf", [128, 128], BF16)
        ident_bf = C.sb("ident_bf", [128, 128], BF16)
        tri_f = C.sb("tri_f", [128, 128])
        sg_f = C.sb("sg_f", [128, 128])
        nsg_f = C.sb("nsg_f", [128, 128])
        ones_f = C.sb("ones_f", [128, 128])
        tri_bf = C.sb("tri_bf", [128, 128], BF16)
        sg_bf = C.sb("sg_bf", [128, 128], BF16)
        g_col = C.sb("g_col", [128, depth * KC])
        qkgain = C.sb("qkgain", [128, depth * 640])
        expsink = C.sb("expsink", [128, depth * 8])
        wup_bf = C.sb("wup_bf", [32, depth * 256], BF16)
        gainB = C.sb("gainB", [128, depth * 128])
        cw = C.sb("cw", [128, depth * 32])
        cb = C.sb("cb", [128, depth * 8])
        dtb = C.sb("dtb", [128, depth * 8])
        Abc = C.sb("Abc", [128, depth * 8])
        Dbc = C.sb("Dbc", [128, depth * 8])
        gainC = C.sb("gainC", [128, depth * 512])
        invf = C.sb("invf", [128, 32])
        posi = C.sb("posi", [128, TPU], I32)
        cs2 = C.sb("cs2", [128, TPU * 64])
        sn2 = C.sb("sn2", [128, TPU * 64])
        kT_st = [[C.sb("kT%d_%d" % (l, i), [128, 256], BF16) for i in range(3)] for l in range(depth)]
        v_st = [[C.sb("vaug%d_%d" % (l, i), [128, 130], BF16) for i in range(3)] for l in range(depth)]
        S_gla = [C.sb("Sgla%d" % l, [128, 512]) for l in range(depth)]
        S_gla_bf = [C.sb("Sglab%d" % l, [128, 512], BF16) for l in range(depth)]
        S_ssd = [C.sb("Sssd%d" % l, [128, 512]) for l in range(depth)]
        S_ssd_bf = [C.sb("Sssdb%d" % l, [128, 512], BF16) for l in range(depth)]
        ctail = [C.sb("ctail%d" % l, [128, 24]) for l in range(depth)]

        psum = [T(es.enter_context(nc.psum_tensor("ps%d" % i, [128, 512], F32)), buf=Buf("ps%d" % i, psum=True)) for i in range(8)]
        ps_i = [0]

        def ps():
            p = psum[ps_i[0] % 8]
            ps_i[0] += 1
            return p

        identf = arena.alloc("identf", 128)
        wup_f = arena.alloc("wup_f", depth * 256)
        s_const = C.new_sem("s_const")
        cst = [0]

        def cdma(out, in_, W):
            C.dma("sp", out, in_, s_const, cst, W=[W])

        for l in range(depth):
            cdma(g_col[:, l * KC:(l + 1) * KC], norm_g_d[l].rearrange("(k p) -> p k", p=128), g_col)
            cdma(_v3(qkgain[:, l * 640: l * 640 + 512], 8, 64), bcast_src(qn_d, l, 64, 8), qkgain)
            cdma(_v3(qkgain[:, l * 640 + 512:(l + 1) * 640], 2, 64), bcast_src(kn_d, l, 64, 2), qkgain)
            cdma(expsink[:, l * 8:(l + 1) * 8], bcast_src(sinks_d, l, 8), expsink)
            cdma(wup_f[0:16, l * 256:(l + 1) * 256], wup_d[l], wup_f)
            cdma(wup_f[16:17, l * 256:(l + 1) * 256], bg_d[l:l + 1, :], wup_f)
            cdma(gainB[:, l * 128:(l + 1) * 128], bcast_src(gon_d, l, 128), gainB)
            for k in range(4):
                cdma(_v3(cw[:, l * 32:(l + 1) * 32], 8, 4)[:, :, k], cw_d[l, k].rearrange("(c p) -> p c", p=128), cw)
            cdma(cb[:, l * 8:(l + 1) * 8], cb_d[l].rearrange("(c p) -> p c", p=128), cb)
            cdma(dtb[:, l * 8:(l + 1) * 8], bcast_src(dtb_d, l, 8), dtb)
            cdma(Abc[:, l * 8:(l + 1) * 8], bcast_src(alog_d, l, 8), Abc)
            cdma(Dbc[:, l * 8:(l + 1) * 8], bcast_src(dsk_d, l, 8), Dbc)
            cdma(gainC[:, l * 512:(l + 1) * 512], bcast_src(son_d, l, 512), gainC)
        cdma(invf[:, :], bcast_src(invf_d, 0, 32), invf)
        for t_ in (g_col, qkgain, expsink, wup_f, gainB, cw, cb, dtb, Abc, Dbc, gainC, invf):
            for k_ in list(t_.b.W):
                t_.b.W[k_] = (s_const, cst[0])

        P = lambda fn, R=(), W=(): C.op("pool", fn, R, W)
        V = lambda fn, R=(), W=(): C.op("dve", fn, R, W)
        A = lambda fn, R=(), W=(): C.op("act", fn, R, W)
        M = lambda fn, R=(), W=(): C.op("pe", fn, R, W)

        P(lambda e: e.memset(ones_f[:, :], 1.0), W=[ones_f])
        P(lambda e: e.memset(tri_f[:, :], 1.0), W=[tri_f])
        P(lambda e: e.memset(sg_f[:, :], 1.0), W=[sg_f])
        P(lambda e: e.memset(identf[:, :], 0.0), W=[identf])
        P(lambda e: e.affine_select(out=tri_f[:, :], in_=tri_f[:, :], pattern=[[1, 128]], compare_op=ALU.is_ge,
                                    fill=0.0, base=0, channel_multiplier=-1), R=[tri_f], W=[tri_f])
        P(lambda e: e.affine_select(out=sg_f[:, :], in_=sg_f[:, :], pattern=[[-1, 128]], compare_op=ALU.is_gt,
                                    fill=0.0, base=0, channel_multiplier=1), R=[sg_f], W=[sg_f])
        P(lambda e: e.affine_select(out=identf[:, :], in_=identf[:, :], pattern=[[-1, 128]], compare_op=ALU.not_equal,
                                    fill=1.0, base=0, channel_multiplier=1), R=[identf], W=[identf])
        V(lambda e: e.tensor_copy(out=ones_bf[:, :], in_=ones_f[:, :]), R=[ones_f], W=[ones_bf])
        V(lambda e: e.tensor_copy(out=tri_bf[:, :], in_=tri_f[:, :]), R=[tri_f], W=[tri_bf])
        V(lambda e: e.tensor_copy(out=sg_bf[:, :], in_=sg_f[:, :]), R=[sg_f], W=[sg_bf])
        V(lambda e: e.tensor_copy(out=ident_bf[:, :], in_=identf[:, :]), R=[identf], W=[ident_bf])
        V(lambda e: e.tensor_scalar(out=nsg_f[:, :], in0=sg_f[:, :], scalar1=-1.0, scalar2=None, op0=ALU.mult),
          R=[sg_f], W=[nsg_f])
        A(lambda e: e.activation(out=expsink[:, :], in_=expsink[:, :], func=AF.Exp), R=[expsink], W=[expsink])
        A(lambda e: e.activation(out=Abc[:, :], in_=Abc[:, :], func=AF.Exp), R=[Abc], W=[Abc])
        V(lambda e: e.tensor_scalar(out=Abc[:, :], in0=Abc[:, :], scalar1=-1.0, scalar2=None, op0=ALU.mult),
          R=[Abc], W=[Abc])
        V(lambda e: e.tensor_copy(out=wup_bf[0:17, :], in_=wup_f[0:17, :]), R=[wup_f], W=[wup_bf])
        for l in range(depth):
            for i in range(3):
                V(lambda e, t=v_st[l][i]: e.memset(t[:, :], 1.0), W=[v_st[l][i]])
                V(lambda e, t=kT_st[l][i]: e.memset(t[:, :], 0.0), W=[kT_st[l][i]])

        def w_in_piece(l, c0, n):
            return w_in_d[l][:, c0:c0 + n].rearrange("(k p) c -> p k c", p=128), KC * n

        pieces = []
        for u in range(n_units):
            for l in range(depth):
                pl = [("A_qkv", [w_in_piece(l, C_AQ, 512)]), ("A_kv", [w_in_piece(l, C_AK, 256)]),
                      ("A_g", [w_in_piece(l, C_AG, 512)]),
                      ("B_g", [w_in_piece(l, C_GG, 512)]), ("B_qk", [w_in_piece(l, C_GQ, 512)]),
                      ("B_v", [w_in_piece(l, C_GV, 512)]), ("B_d", [w_in_piece(l, C_GD, 16)]),
                      ("C_z", [w_in_piece(l, C_Z, 512)]), ("C_x", [w_in_piece(l, C_XBC, 512)]),
                      ("C_bc", [w_in_piece(l, C_XBC + 512, 512)]), ("C_dt", [w_in_piece(l, C_DT, 8)])]
                for g4 in range(2):
                    for br in range(3):
                        pl.append(("M_br%d_%d" % (br, g4),
                                   [(wbr_d[l, br][:, g4 * 512:(g4 + 1) * 512].rearrange("(k p) c -> p k c", p=128), 4 * 512)]))
                        pl.append(("M_mg%d_%d" % (br, g4), [w_in_piece(l, C_MG + br * 1024 + g4 * 512, 512)]))
                for oh in range(2):
                    pl.append(("O_%d" % oh, [(wout_d[l][:, oh * 512:(oh + 1) * 512].rearrange("(k p) c -> p k c", p=128), KC * 512)]))
                pieces.extend(pl)
        wst = {"issued": 0, "released": set(), "next": 0}

        def w_pump():
            while wst["issued"] < len(pieces):
                j = wst["issued"]
                if j >= RING and (j - RING) not in wst["released"]:
                    break
                if j - wst["next"] >= RING:
                    break
                slot = j % RING
                name, parts = pieces[j]
                off = 0
                for src, n in parts:
                    kk = src.shape[1]
                    dst = ring[slot][:, off:off + n].rearrange("p (k c) -> p k c", k=kk)
                    C.dma("pool", dst, src, ring_sem[slot], ring_cnt[slot], W=[ring[slot]])
                    off += n
                wst["issued"] += 1

        def w_next(name):
            j = wst["next"]
            assert pieces[j][0] == name, (pieces[j][0], name)
            w_pump()
            assert wst["issued"] > j, "weight ring too small at piece %d %s" % (j, name)
            wst["next"] += 1
            return j, ring[j % RING]

        def w_release(j):
            wst["released"].add(j)
            w_pump()

        def wv(rt, n):
            return lambda k, c0=0, cn=None: rt[:, k * n + c0: k * n + (n if cn is None else c0 + cn)]

        s_x = [[C.new_sem("s_x%d_%d" % (k, b)) for b in range(BPU)] for k in range(KC)]
        x_cnt = [[[0] for b in range(BPU)] for k in range(KC)]
        s_o = [[C.new_sem("s_o%d_%d" % (k, b)) for b in range(BPU)] for k in range(KC)]
        o_cnt = [[[0] for b in range(BPU)] for k in range(KC)]
        s_dbg = C.new_sem("s_dbg")
        dbg_cnt = [0]
        s_pos = C.new_sem("s_pos")
        pos_cnt = [0]

        def load_x(u):
            t0 = u * UNIT
            for b in range(BPU):
                for k in range(KC):
                    C.dma("sp", xv(k, b), xT_d[k * 128:(k + 1) * 128, t0 + b * 512: t0 + (b + 1) * 512], s_x[k][b], x_cnt[k][b],
                          W=[xT_b[k][b]])

        def store_x(u, k, b):
            t0 = u * UNIT
            C.dma("sp", outT_d[k * 128:(k + 1) * 128, t0 + b * 512: t0 + (b + 1) * 512], xv(k, b), s_o[k][b], o_cnt[k][b],
                  R=[xT_b[k][b]])

        def rope_tables(u):
            arena.reset()
            s_p = None
            C.dma("sp", posi[:, :], pos_d[u], s_pos, pos_cnt, W=[posi])
            posf = arena.alloc("posf", TPU)
            ang = arena.alloc("ang", TPU * 32)
            kf = arena.alloc("kf", TPU * 32)
            ki = arena.alloc("ki", TPU * 32, I32)
            V(lambda e: e.tensor_copy(out=posf[:, :], in_=posi[:, :]), R=[posi], W=[posf])
            V(lambda e: e.tensor_tensor(out=_v3(ang[:, :], TPU, 32), in0=_bc_last(posf[:, :], 32),
                                        in1=_bc_mid(invf[:, :], TPU), op=ALU.mult), R=[posf, invf], W=[ang])
            for shift, half in ((0.0, "sin"), (0.5 * math.pi, "cos")):
                V(lambda e: e.tensor_scalar(out=kf[:, :], in0=ang[:, :], scalar1=shift, scalar2=1.0 / TWO_PI,
                                            op0=ALU.add, op1=ALU.mult), R=[ang], W=[kf])
                V(lambda e: e.tensor_copy(out=ki[:, :], in_=kf[:, :]), R=[kf], W=[ki])
                V(lambda e: e.tensor_copy(out=kf[:, :], in_=ki[:, :]), R=[ki], W=[kf])
                V(lambda e: e.scalar_tensor_tensor(out=kf[:, :], in0=kf[:, :], scalar=-TWO_PI, in1=ang[:, :],
                                                   op0=ALU.mult, op1=ALU.add), R=[kf, ang], W=[kf])
                if half == "sin":
                    A(lambda e: e.activation(out=_v3(sn2[:, :], TPU, 64)[:, :, 32:64], in_=_v3(kf[:, :], TPU, 32),
                                             func=AF.Sin, bias=0.0, scale=1.0), R=[kf], W=[sn2])
                    V(lambda e: e.tensor_scalar(out=_v3(sn2[:, :], TPU, 64)[:, :, 0:32],
                                                in0=_v3(sn2[:, :], TPU, 64)[:, :, 32:64], scalar1=-1.0, scalar2=None,
                                                op0=ALU.mult), R=[sn2], W=[sn2])
                else:
                    A(lambda e: e.activation(out=_v3(cs2[:, :], TPU, 64)[:, :, 0:32], in_=_v3(kf[:, :], TPU, 32),
                                             func=AF.Sin, bias=shift, scale=1.0), R=[kf], W=[cs2])
                    V(lambda e: e.tensor_copy(out=_v3(cs2[:, :], TPU, 64)[:, :, 32:64],
                                              in_=_v3(cs2[:, :], TPU, 64)[:, :, 0:32]), R=[cs2], W=[cs2])

        def stage_norm(l):
            arena.reset()
            sq = [arena.alloc("sq%d" % i, KC * 512, BF16) for i in range(1)]
            lnv = arena.alloc("lnv", 512)
            rstd = [arena.alloc("rstd%d" % i, 512) for i in range(2)]
            for b in range(BPU):
                s = sq[0]
                for k in range(KC):
                    A(lambda e: e.activation(out=s[:, k * 512:(k + 1) * 512], in_=xv(k, b), func=AF.Square),
                      R=[xT_b[k][b]], W=[s])
                p = ps()
                for k in range(KC):
                    M(lambda e: e.matmul(p[:, :], lhsT=ones_bf[:, :], rhs=s[:, k * 512:(k + 1) * 512],
                                         start=(k == 0), stop=(k == KC - 1)), R=[ones_bf, s], W=[p])
                A(lambda e: e.activation(out=lnv[:, :], in_=p[:, :], func=AF.Ln, scale=1.0 / D, bias=EPS),
                  R=[p], W=[lnv])
                r = rstd[b % 2]
                A(lambda e: e.activation(out=r[:, :], in_=lnv[:, :], func=AF.Exp, scale=-0.5), R=[lnv], W=[r])
                for k in range(KC):
                    V(lambda e: e.scalar_tensor_tensor(out=hv(k, b * 512, 512), in0=xv(k, b),
                                                       scalar=g_col[:, l * KC + k: l * KC + k + 1], in1=r[:, :],
                                                       op0=ALU.mult, op1=ALU.mult),
                      R=[xT_b[k][b], g_col, r], W=[hT_b[b]])

        def proj_tok(p, n_out, wfn, t, c0=0, wt=None, pcol=0):
            for k in range(KC):
                M(lambda e: e.matmul(p[:, pcol:pcol + n_out], lhsT=hv(k, t * 128, 128), rhs=wfn(k, c0, n_out),
                                     start=(k == 0), stop=(k == KC - 1)), R=[hT_b[t // 4], wt], W=[p])

        def transposes_to(dst_fn, src, nchunks, Rsrc, evac="act", p=None):
            if p is None:
                p = ps()
            pb = p[:, :].bitcast(BF16)
            for c in range(nchunks):
                M(lambda e: e.transpose(out=pb[:, c * 128:(c + 1) * 128], in_=src[:, c * 128:(c + 1) * 128],
                                        identity=ident_bf[:, :]), R=[Rsrc, ident_bf], W=[p])
            return p, pb

        def run_pipelined(gens, depth=2):
            if not PIPE:
                depth = 1
            tokens = set()
            active, blocked, idx = [], {}, 0
            while active or idx < len(gens):
                while len(active) < depth and idx < len(gens):
                    active.append(gens[idx])
                    idx += 1
                progressed = False
                for g in list(active):
                    need = blocked.get(id(g))
                    if need is not None:
                        if need not in tokens:
                            continue
                        blocked[id(g)] = None
                    progressed = True
                    try:
                        r = next(g)
                    except StopIteration:
                        active.remove(g)
                        continue
                    if r is not None:
                        kind, tok = r
                        if kind == "set":
                            tokens.add(tok)
                        elif tok not in tokens:
                            blocked[id(g)] = tok
                assert progressed, "pipeline deadlock"

        def stagger(mk, n):
            def wrap(t):
                if t > 0 and STAGGER:
                    yield ("need", ("mid", t - 1))
                yield from mk(t)
            return [wrap(t) for t in range(n)]

        def stage_attn(u, l):
            arena.reset()
            first_unit = (u % upseq == 0)
            sgA = arena.alloc("sgA", TPU * 512, BF16)
            sqbs = [arena.alloc("sqb%d" % i, 640) for i in range(2)]
            sss = [arena.alloc("ss%d" % i, 16) for i in range(2)]
            qkns = [arena.alloc("qkn%d" % i, 640) for i in range(2)]
            tmp1s = [arena.alloc("tmp1%d" % i, 640) for i in range(2)]
            tmp2s = [arena.alloc("tmp2%d" % i, 640) for i in range(2)]
            qkr = [arena.alloc("qkr%d" % i, 640, BF16) for i in range(2)]
            qTs = [arena.alloc("qT%d" % i, 512, BF16) for i in range(2)]
            Eb = [[arena.alloc("E%d_%d" % (i, j), 512, BF16) for j in range(4)] for i in range(2)]
            dens = [arena.alloc("den%d" % i, 16) for i in range(2)]
            yAs = [arena.alloc("yA%d" % i, 512) for i in range(2)]
            y_a = [arena.alloc("y_a%d" % i, 512, BF16) for i in range(2)]
            jq, wq = w_next("A_qkv")
            jk, wk = w_next("A_kv")
            jg, wg = w_next("A_g")
            wqf, wkf, wgf = wv(wq, 512), wv(wk, 256), wv(wg, 512)
            for t in range(TPU):
                p = ps()
                proj_tok(p, 512, wgf, t, wt=wg)
                A(lambda e: e.activation(out=sgA[:, t * 512:(t + 1) * 512], in_=p[:, :], func=AF.Silu), R=[p], W=[sgA])
            w_release(jg)

            def tile(t):
                gt = u * TPU + t
                first = first_unit and t == 0
                kT_c, kT_p = kT_st[l][gt % 3], kT_st[l][(gt + 2) % 3]
                v_c, v_p = v_st[l][gt % 3], v_st[l][(gt + 2) % 3]
                sqb, ss, qkn, tmp1, tmp2 = sqbs[t % 2], sss[t % 2], qkns[t % 2], tmp1s[t % 2], tmp2s[t % 2]
                den, yA = dens[t % 2], yAs[t % 2]
                p1, p2 = ps(), ps()
                proj_tok(p1, 512, wqf, t, wt=wq)
                proj_tok(p2, 256, wkf, t, wt=wk)
                yield
                A(lambda e: e.activation(out=sqb[:, 0:512], in_=p1[:, :], func=AF.Square), R=[p1], W=[sqb])
                A(lambda e: e.activation(out=sqb[:, 512:640], in_=p2[:, 0:128], func=AF.Square), R=[p2], W=[sqb])
                yield
                V(lambda e: e.reduce_sum(out=ss[:, 0:10], in_=_v3(sqb[:, :], 10, 64), axis=AX.X), R=[sqb], W=[ss])
                yield
                A(lambda e: e.activation(out=ss[:, 0:10], in_=ss[:, 0:10], func=AF.Ln, scale=1.0 / 64, bias=EPS),
                  R=[ss], W=[ss])
                A(lambda e: e.activation(out=ss[:, 0:10], in_=ss[:, 0:10], func=AF.Exp, scale=-0.5), R=[ss], W=[ss])
                yield
                V(lambda e: e.tensor_tensor(out=_v3(qkn[:, 0:512], 8, 64), in0=_v3(p1[:, :], 8, 64),
                                            in1=_bc_last(ss[:, 0:8], 64), op=ALU.mult), R=[p1, ss], W=[qkn])
                V(lambda e: e.tensor_tensor(out=_v3(qkn[:, 512:640], 2, 64), in0=_v3(p2[:, 0:128], 2, 64),
                                            in1=_bc_last(ss[:, 8:10], 64), op=ALU.mult), R=[p2, ss], W=[qkn])
                yield
                A(lambda e: e.activation(out=_v3(v_c[:, :], 2, 65)[:, :, 0:64], in_=_v3(p2[:, 128:256], 2, 64),
                                         func=AF.Copy), R=[p2], W=[v_c])
                P(lambda e: e.tensor_tensor(out=qkn[:, :], in0=qkn[:, :], in1=qkgain[:, l * 640:(l + 1) * 640],
                                            op=ALU.mult), R=[qkn, qkgain], W=[qkn])
                yield
                cs_t = cs2[:, t * 64:(t + 1) * 64]
                sn_t = sn2[:, t * 64:(t + 1) * 64]
                P(lambda e: e.tensor_tensor(out=_v3(tmp1[:, :], 10, 64), in0=_v3(qkn[:, :], 10, 64),
                                            in1=_bc_mid(cs_t, 10), op=ALU.mult), R=[qkn, cs2], W=[tmp1])
                V(lambda e: e.tensor_tensor(out=_v3(tmp2[:, :], 10, 64)[:, :, 0:32], in0=_v3(qkn[:, :], 10, 64)[:, :, 32:64],
                                            in1=_bc_mid(sn_t[:, 0:32], 10), op=ALU.mult), R=[qkn, sn2], W=[tmp2])
                V(lambda e: e.tensor_tensor(out=_v3(tmp2[:, :], 10, 64)[:, :, 32:64], in0=_v3(qkn[:, :], 10, 64)[:, :, 0:32],
                                            in1=_bc_mid(sn_t[:, 32:64], 10), op=ALU.mult), R=[qkn, sn2], W=[tmp2])
                yield
                qk = qkr[t % 2]
                V(lambda e: e.tensor_tensor(
                    out=qk[:, 0:512].rearrange("p (a g d) -> p g a d", a=4, g=2, d=64),
                    in0=tmp1[:, 0:512].rearrange("p (g a d) -> p g a d", g=2, a=4, d=64),
                    in1=tmp2[:, 0:512].rearrange("p (g a d) -> p g a d", g=2, a=4, d=64), op=ALU.add),
                  R=[tmp1, tmp2], W=[qk])
                V(lambda e: e.tensor_tensor(out=qk[:, 512:640], in0=tmp1[:, 512:640], in1=tmp2[:, 512:640], op=ALU.add),
                  R=[tmp1, tmp2], W=[qk])
                yield
                pT, pTb = transposes_to(None, qk, 5, qk)
                yield ("set", ("mid", t))
                qT = qTs[t % 2]
                A(lambda e: e.activation(out=qT[:, :], in_=pTb[:, 0:512], func=AF.Copy), R=[pT], W=[qT])
                for g in range(2):
                    V(lambda e: e.tensor_copy(out=kT_c[g * 64:(g + 1) * 64, g * 128:(g + 1) * 128],
                                              in_=pTb[g * 64:(g + 1) * 64, 512:640]), R=[pT], W=[kT_c])
                yield ("set", ("kv", gt))
                if not first and t > 0:
                    yield ("need", ("kv", gt - 1))
                E = Eb[t % 2]
                blocks = [("c", kT_c, v_c)] if first else [("p", kT_p, v_p), ("c", kT_c, v_c)]
                for g in range(2):
                    for bi, (tag, kTt, _) in enumerate(blocks):
                        p = ps()
                        M(lambda e: e.matmul(p[:, :], lhsT=kTt[:, g * 128:(g + 1) * 128], rhs=qT[:, :],
                                             start=True, stop=True), R=[kTt, qT], W=[p])
                        yield
                        Et = E[g * 2 + bi]
                        A(lambda e: e.activation(out=Et[:, :], in_=p[:, :], func=AF.Exp, scale=0.125), R=[p], W=[Et])
                        yield
                        msk = tri_bf if tag == "c" else sg_bf
                        (P if (g * 2 + bi) % 2 == 0 else V)(
                            lambda e: e.tensor_tensor(out=_v3(Et[:, :], 4, 128), in0=_v3(Et[:, :], 4, 128),
                                                      in1=_bc_mid(msk[:, :], 4), op=ALU.mult), R=[Et, msk], W=[Et])
                yield
                po = [ps(), ps()]
                for h in range(8):
                    g, a = h // 4, h % 4
                    for bi, (tag, _, vt) in enumerate(blocks):
                        Et = E[g * 2 + bi]
                        M(lambda e: e.matmul(po[g][:, a * 65:(a + 1) * 65], lhsT=Et[:, a * 128:(a + 1) * 128],
                                             rhs=vt[:, g * 65:(g + 1) * 65], start=(bi == 0),
                                             stop=(bi == len(blocks) - 1)), R=[Et, vt], W=[po[g]])
                yield
                for g in range(2):
                    V(lambda e: e.tensor_tensor(out=den[:, g * 4:(g + 1) * 4], in0=_v3(po[g][:, 0:260], 4, 65)[:, :, 64],
                                                in1=expsink[:, l * 8 + g * 4: l * 8 + (g + 1) * 4], op=ALU.add),
                      R=[po[g], expsink], W=[den])
                V(lambda e: e.reciprocal(out=den[:, 0:8], in_=den[:, 0:8]), R=[den], W=[den])
                for g in range(2):
                    V(lambda e: e.tensor_tensor(out=_v3(yA[:, g * 256:(g + 1) * 256], 4, 64),
                                                in0=_v3(po[g][:, 0:260], 4, 65)[:, :, 0:64],
                                                in1=_bc_last(den[:, g * 4:(g + 1) * 4], 64), op=ALU.mult),
                      R=[po[g], den], W=[yA])
                ya = y_a[t % 2]
                P(lambda e: e.tensor_tensor(out=ya[:, :], in0=yA[:, :], in1=sgA[:, t * 512:(t + 1) * 512], op=ALU.mult),
                  R=[yA, sgA], W=[ya])
                yield
                pT2, pT2b = transposes_to(None, ya, 4, ya)
                yield
                A(lambda e: e.activation(out=_v3(yT[0][:, :], 4, UNIT)[:, :, t * 128:(t + 1) * 128],
                                         in_=_v3(pT2b[:, 0:512], 4, 128), func=AF.Copy), R=[pT2], W=[yT[0]])

            run_pipelined(stagger(tile, TPU))
            w_release(jq)
            w_release(jk)

        def stage_gla(u, l):
            arena.reset()
            first_unit = (u % upseq == 0)
            sgB = arena.alloc("sgB", TPU * 512, BF16)
            D2 = lambda name, n, dt=F32: [arena.alloc("%s%d" % (name, i), n, dt) for i in range(2)]
            e1s, sps, Eqs, Eks, Ees, decs = D2("e1", 256), D2("sp", 256), D2("Eq", 256), D2("Ek", 256), D2("Ee", 256), D2("dec", 2)
            qds, kis, kes = D2("qd", 256, BF16), D2("ki", 256, BF16), D2("ke", 256, BF16)
            vbfs, qkTs, kzs, ATs = D2("vbf", 512, BF16), D2("qkT", 256, BF16), D2("kz", 512, BF16), D2("AT", 512, BF16)
            sqos, ssbs, t1s, ybs = D2("sqo", 512), D2("ssb", 4), D2("t1", 512), D2("yb", 512, BF16)
            gdTs = D2("gdT", 128, BF16)
            S, Sb = S_gla[l], S_gla_bf[l]
            jg, wg = w_next("B_g")
            wgf = wv(wg, 512)
            for t in range(TPU):
                p = ps()
                proj_tok(p, 512, wgf, t, wt=wg)
                A(lambda e: e.activation(out=sgB[:, t * 512:(t + 1) * 512], in_=p[:, :], func=AF.Silu), R=[p], W=[sgB])
            w_release(jg)
            jqk, wqk = w_next("B_qk")
            jv, wvv = w_next("B_v")
            jd, wd = w_next("B_d")
            wqkf, wvf = wv(wqk, 512), wv(wvv, 512)
            for kz_ in kzs:
                V(lambda e: e.memset(kz_[:, :], 0.0), W=[kz_])
            for g_ in gdTs:
                V(lambda e: e.memset(g_[0:32, :], 1.0), W=[g_])
            if first_unit:
                V(lambda e: e.memset(S[:, :], 0.0), W=[S])
                V(lambda e: e.memset(Sb[:, :], 0.0), W=[Sb])

            def tile(t):
                first = first_unit and t == 0
                i2 = t % 2
                e1, sp, Eq, Ek, Ee, dec = e1s[i2], sps[i2], Eqs[i2], Eks[i2], Ees[i2], decs[i2]
                qd, ki, ke, vbf, qkT, kz, AT = qds[i2], kis[i2], kes[i2], vbfs[i2], qkTs[i2], kzs[i2], ATs[i2]
                sqo, ssb, t1, yb, gdTt = sqos[i2], ssbs[i2], t1s[i2], ybs[i2], gdTs[i2]
                pqk, pv, pl = ps(), ps(), ps()
                proj_tok(pqk, 512, wqkf, t, wt=wqk)
                proj_tok(pv, 512, wvf, t, wt=wvv)
                for k in range(KC):
                    M(lambda e: e.matmul(pl[0:16, 256:384], lhsT=wd[:, k * 16:(k + 1) * 16], rhs=hv(k, t * 128, 128),
                                         start=(k == 0), stop=(k == KC - 1)), R=[wd, hT_b[t // 4]], W=[pl])
                yield
                A(lambda e: e.activation(out=gdTt[0:16, :], in_=pl[0:16, 256:384], func=AF.Copy), R=[pl], W=[gdTt])
                A(lambda e: e.activation(out=vbf[:, :], in_=pv[:, :], func=AF.Copy), R=[pv], W=[vbf])
                yield
                M(lambda e: e.matmul(pl[:, 0:256], lhsT=gdTt[0:17, :], rhs=wup_bf[0:17, l * 256:(l + 1) * 256],
                                     start=True, stop=True), R=[gdTt, wup_bf], W=[pl])
                yield
                A(lambda e: e.activation(out=e1[:, :], in_=pl[:, 0:256], func=AF.Exp, scale=-1.0), R=[pl], W=[e1])
                A(lambda e: e.activation(out=sp[:, :], in_=e1[:, :], func=AF.Ln, bias=1.0), R=[e1], W=[sp])
                yield
                pc = ps()
                M(lambda e: e.matmul(pc[:, 0:256], lhsT=tri_f[:, :], rhs=sp[:, :], start=True, stop=True),
                  R=[tri_f, sp], W=[pc])
                M(lambda e: e.matmul(pc[:, 256:512], lhsT=nsg_f[:, :], rhs=sp[:, :], start=True, stop=True),
                  R=[nsg_f, sp], W=[pc])
                for m in range(2):
                    M(lambda e: e.matmul(pl[:, 384 + m:385 + m], lhsT=sp[:, m * 128:(m + 1) * 128], rhs=ones_f[:, 0:1],
                                         start=True, stop=True), R=[sp, ones_f], W=[pl])
                yield
                A(lambda e: e.activation(out=Eq[:, :], in_=pc[:, 0:256], func=AF.Exp, scale=-1.0 / 16), R=[pc], W=[Eq])
                A(lambda e: e.activation(out=Ek[:, :], in_=pc[:, 0:256], func=AF.Exp, scale=1.0 / 16), R=[pc], W=[Ek])
                yield
                A(lambda e: e.activation(out=Ee[:, :], in_=pc[:, 256:512], func=AF.Exp, scale=1.0 / 16), R=[pc], W=[Ee])
                A(lambda e: e.activation(out=dec[:, 0:2], in_=pl[:, 384:386], func=AF.Exp, scale=-1.0 / 16), R=[pl], W=[dec])
                yield
                V(lambda e: e.scalar_tensor_tensor(out=qd[:, :], in0=pqk[:, 0:256], scalar=0.125, in1=Eq[:, :],
                                                   op0=ALU.mult, op1=ALU.mult), R=[pqk, Eq], W=[qd])
                V(lambda e: e.tensor_tensor(out=ki[:, :], in0=pqk[:, 256:512], in1=Ek[:, :], op=ALU.mult),
                  R=[pqk, Ek], W=[ki])
                yield
                V(lambda e: e.tensor_tensor(out=ke[:, :], in0=pqk[:, 256:512], in1=Ee[:, :], op=ALU.mult),
                  R=[pqk, Ee], W=[ke])
                pT = ps()
                pTb = pT[:, :].bitcast(BF16)
                for c in range(2):
                    M(lambda e: e.transpose(out=pTb[:, c * 128:(c + 1) * 128], in_=qd[:, c * 128:(c + 1) * 128],
                                            identity=ident_bf[:, :]), R=[qd, ident_bf], W=[pT])
                for c in range(2):
                    M(lambda e: e.transpose(out=pTb[:, (2 + c) * 128:(3 + c) * 128], in_=ki[:, c * 128:(c + 1) * 128],
                                            identity=ident_bf[:, :]), R=[ki, ident_bf], W=[pT])
                yield ("set", ("mid", t))
                A(lambda e: e.activation(out=qkT[:, :], in_=pTb[:, 0:256], func=AF.Copy), R=[pT], W=[qkT])
                for r in range(2):
                    A(lambda e: e.activation(
                        out=kz[r * 64:(r + 1) * 64, :].rearrange("p (m r v) -> p m r v", m=2, r=2, v=128)[:, :, r, :],
                        in_=_v3(pTb[r * 64:(r + 1) * 64, 256:512], 2, 128), func=AF.Copy), R=[pT], W=[kz])
                yield
                pA = ps()
                for h in range(4):
                    m, r = h // 2, h % 2
                    M(lambda e: e.matmul(pA[:, h * 128:(h + 1) * 128], lhsT=kz[:, h * 128:(h + 1) * 128],
                                         rhs=qkT[:, m * 128:(m + 1) * 128], start=True, stop=True),
                      R=[qkT, kz], W=[pA])
                pU = ps()
                for h in range(4):
                    m = h // 2
                    M(lambda e: e.matmul(pU[:, h * 128:(h + 1) * 128], lhsT=ke[:, m * 128:(m + 1) * 128],
                                         rhs=vbf[:, h * 128:(h + 1) * 128], start=True, stop=True), R=[ke, vbf], W=[pU])
                yield
                V(lambda e: e.tensor_tensor(out=_v3(AT[:, :], 4, 128), in0=_v3(pA[:, :], 4, 128),
                                            in1=_bc_mid(tri_bf[:, :], 4), op=ALU.mult), R=[pA, tri_bf], W=[AT])
                yield
                if t > 0:
                    yield ("need", ("S", t - 1))
                po = ps()
                for h in range(4):
                    m, r = h // 2, h % 2
                    M(lambda e: e.matmul(po[:, h * 128:(h + 1) * 128], lhsT=AT[:, h * 128:(h + 1) * 128],
                                         rhs=vbf[:, h * 128:(h + 1) * 128], start=True, stop=first), R=[AT, vbf], W=[po])
                    if not first:
                        M(lambda e: e.matmul(po[:, h * 128:(h + 1) * 128], lhsT=qkT[:, m * 128:(m + 1) * 128],
                                             rhs=Sb[:, h * 128:(h + 1) * 128], start=False, stop=True),
                          R=[qkT, Sb], W=[po])
                yield
                for h in range(4):
                    m, r = h // 2, h % 2
                    V(lambda e: e.scalar_tensor_tensor(out=S[r * 64:(r + 1) * 64, h * 128:(h + 1) * 128],
                                                       in0=S[r * 64:(r + 1) * 64, h * 128:(h + 1) * 128],
                                                       scalar=dec[r * 64:(r + 1) * 64, m:m + 1],
                                                       in1=pU[r * 64:(r + 1) * 64, h * 128:(h + 1) * 128],
                                                       op0=ALU.mult, op1=ALU.add), R=[S, dec, pU], W=[S])
                yield
                A(lambda e: e.activation(out=Sb[:, :], in_=S[:, :], func=AF.Copy), R=[S], W=[Sb])
                yield ("set", ("S", t))
                A(lambda e: e.activation(out=sqo[:, :], in_=po[:, :], func=AF.Square), R=[po], W=[sqo])
                yield
                V(lambda e: e.reduce_sum(out=ssb[:, 0:4], in_=_v3(sqo[:, :], 4, 128), axis=AX.X), R=[sqo], W=[ssb])
                yield
                A(lambda e: e.activation(out=ssb[:, 0:4], in_=ssb[:, 0:4], func=AF.Ln, scale=1.0 / 128, bias=EPS),
                  R=[ssb], W=[ssb])
                A(lambda e: e.activation(out=ssb[:, 0:4], in_=ssb[:, 0:4], func=AF.Exp, scale=-0.5), R=[ssb], W=[ssb])
                yield
                V(lambda e: e.tensor_tensor(out=_v3(t1[:, :], 4, 128), in0=_v3(po[:, :], 4, 128),
                                            in1=_bc_last(ssb[:, 0:4], 128), op=ALU.mult), R=[po, ssb], W=[t1])
                yield
                V(lambda e: e.tensor_tensor(out=_v3(t1[:, :], 4, 128), in0=_v3(t1[:, :], 4, 128),
                                            in1=_bc_mid(gainB[:, l * 128:(l + 1) * 128], 4), op=ALU.mult),
                  R=[t1, gainB], W=[t1])
                V(lambda e: e.tensor_tensor(out=yb[:, :], in0=t1[:, :], in1=sgB[:, t * 512:(t + 1) * 512], op=ALU.mult),
                  R=[t1, sgB], W=[yb])
                yield
                pT2, pT2b = transposes_to(None, yb, 4, yb)
                yield
                A(lambda e: e.activation(out=_v3(yT[1][:, :], 4, UNIT)[:, :, t * 128:(t + 1) * 128],
                                         in_=_v3(pT2b[:, 0:512], 4, 128), func=AF.Copy), R=[pT2], W=[yT[1]])

            run_pipelined(stagger(tile, TPU))
            w_release(jqk)
            w_release(jv)
            w_release(jd)

        def stage_ssd(u, l):
            arena.reset()
            first_unit = (u % upseq == 0)
            sgC = arena.alloc("sgC", TPU * 512, BF16)
            xbcT = arena.alloc("xbcT", 8 * UNIT, BF16)
            mark = arena.off
            raws = [arena.alloc("raw%d" % i, 516) for i in range(3)]
            accs = [arena.alloc("acc%d" % i, 512) for i in range(3)]
            S, Sb = S_ssd[l], S_ssd_bf[l]
            ct = ctail[l]
            jz, wz = w_next("C_z")
            wzf = wv(wz, 512)
            for t in range(TPU):
                p = ps()
                proj_tok(p, 512, wzf, t, wt=wz)
                A(lambda e: e.activation(out=sgC[:, t * 512:(t + 1) * 512], in_=p[:, :], func=AF.Silu), R=[p], W=[sgC])
            w_release(jz)
            jx, wx = w_next("C_x")
            jbc, wbc = w_next("C_bc")
            if first_unit:
                V(lambda e: e.memset(ct[:, :], 0.0), W=[ct])

            def conv(i, b, ch):
                wt = wx if ch < 4 else wbc
                c0 = (ch % 4) * 128
                p = ps()
                for k in range(KC):
                    M(lambda e: e.matmul(p[:, :], lhsT=wt[:, k * 512 + c0: k * 512 + c0 + 128], rhs=hv(k, b * 512, 512),
                                         start=(k == 0), stop=(k == KC - 1)), R=[wt, hT_b[b]], W=[p])
                yield
                rw, ac = raws[i % 3], accs[i % 3]
                ci = l * 32 + ch * 4
                A(lambda e: e.activation(out=rw[:, 3:515], in_=p[:, :], func=AF.Copy), R=[p], W=[rw])
                A(lambda e: e.activation(out=ac[:, :], in_=p[:, :], func=AF.Identity, scale=cw[:, ci + 3:ci + 4],
                                         bias=cb[:, l * 8 + ch:l * 8 + ch + 1]), R=[p, cw, cb], W=[ac])
                yield
                if b > 0:
                    yield ("need", ("ct", b - 1, ch))
                V(lambda e: e.tensor_copy(out=rw[:, 0:3], in_=ct[:, ch * 3:(ch + 1) * 3]), R=[ct], W=[rw])
                yield
                for kk in range(3):
                    V(lambda e: e.scalar_tensor_tensor(out=ac[:, :], in0=rw[:, kk:kk + 512], scalar=cw[:, ci + kk:ci + kk + 1],
                                                       in1=ac[:, :], op0=ALU.mult, op1=ALU.add), R=[rw, cw, ac], W=[ac])
                    yield
                V(lambda e: e.tensor_copy(out=ct[:, ch * 3:(ch + 1) * 3], in_=rw[:, 512:515]), R=[rw], W=[ct])
                yield ("set", ("ct", b, ch))
                A(lambda e: e.activation(out=xbcT[:, ch * UNIT + b * 512: ch * UNIT + (b + 1) * 512], in_=ac[:, :],
                                         func=AF.Silu), R=[ac], W=[xbcT])

            run_pipelined([conv(b * 8 + ch, b, ch) for b in range(BPU) for ch in range(8)], depth=3)
            w_release(jx)
            w_release(jbc)
            jdt, wdt = w_next("C_dt")
            arena.rewind(mark, raws + accs)
            D2 = lambda name, n, dt=F32: [arena.alloc("%s%d" % (name, i), n, dt) for i in range(2)]
            x1s, dtts, aas, sms, sscs = D2("x1", 8), D2("dtt", 8), D2("aa", 8), D2("sm", 24), D2("ssc", 2)
            Rms, xBs, xdts, xdds = D2("Rm", 1024), D2("xB", 768, BF16), D2("xdt", 512, BF16), D2("xdd", 512, BF16)
            ycs = D2("yc", 512, BF16)
            Lm = arena.alloc("Lm", 1024)
            CBm = arena.alloc("CBm", 256)
            Wm = arena.alloc("Wm", 1024, BF16)
            if first_unit:
                V(lambda e: e.memset(S[:, :], 0.0), W=[S])
                V(lambda e: e.memset(Sb[:, :], 0.0), W=[Sb])

            def xc(c, t):
                return xbcT[:, c * UNIT + t * 128: c * UNIT + (t + 1) * 128]

            def tile(t):
                first = first_unit and t == 0
                i2 = t % 2
                x1, dtt, aa, sm, ssc = x1s[i2], dtts[i2], aas[i2], sms[i2], sscs[i2]
                Rm, xB, xdt, xdd, yc = Rms[i2], xBs[i2], xdts[i2], xdds[i2], ycs[i2]
                y1 = T(Rm[:, 0:512], Rm.b)
                y2 = T(Rm[:, 512:1024], Rm.b)
                bank = lambda j: psum[4 * i2 + j]
                pd = bank(0)
                for k in range(KC):
                    M(lambda e: e.matmul(pd[:, 0:8], lhsT=hv(k, t * 128, 128), rhs=wdt[:, k * 8:(k + 1) * 8],
                                         start=(k == 0), stop=(k == KC - 1)), R=[hT_b[t // 4], wdt], W=[pd])
                yield
                V(lambda e: e.tensor_tensor(out=x1[:, 0:8], in0=pd[:, 0:8], in1=dtb[:, l * 8:(l + 1) * 8], op=ALU.add),
                  R=[pd, dtb], W=[x1])
                yield
                A(lambda e: e.activation(out=x1[:, 0:8], in_=x1[:, 0:8], func=AF.Exp), R=[x1], W=[x1])
                A(lambda e: e.activation(out=dtt[:, 0:8], in_=x1[:, 0:8], func=AF.Ln, bias=1.0), R=[x1], W=[dtt])
                yield
                V(lambda e: e.tensor_tensor(out=aa[:, 0:8], in0=dtt[:, 0:8], in1=Abc[:, l * 8:(l + 1) * 8], op=ALU.mult),
                  R=[dtt, Abc], W=[aa])
                P(lambda e: e.tensor_tensor(out=_v3(Rm[:, :], 8, 128), in0=_bc_mid(tri_f[:, :], 8),
                                            in1=_bc_last(aa[:, 0:8], 128), op=ALU.mult), R=[tri_f, aa], W=[Rm])
                yield
                pseg = [bank(1), bank(2)]
                for hf in range(2):
                    M(lambda e: e.matmul(pseg[hf][:, :], lhsT=sg_f[:, :], rhs=Rm[:, hf * 512:(hf + 1) * 512],
                                         start=True, stop=True), R=[sg_f, Rm], W=[pseg[hf]])
                M(lambda e: e.matmul(pd[:, 8:16], lhsT=tri_f[:, :], rhs=aa[:, 0:8], start=True, stop=True),
                  R=[tri_f, aa], W=[pd])
                M(lambda e: e.matmul(pd[:, 16:24], lhsT=sg_f[:, :], rhs=aa[:, 0:8], start=True, stop=True),
                  R=[sg_f, aa], W=[pd])
                M(lambda e: e.matmul(pd[:, 24:32], lhsT=ones_f[:, :], rhs=aa[:, 0:8], start=True, stop=True),
                  R=[ones_f, aa], W=[pd])
                pT = bank(3)
                pTb = pT[:, :].bitcast(BF16)
                for c in range(6):
                    M(lambda e: e.transpose(out=pTb[:, c * 128:(c + 1) * 128], in_=xc(c, t), identity=ident_bf[:, :]),
                      R=[xbcT, ident_bf], W=[pT])
                yield
                if t > 0:
                    yield ("need", ("LmF", t - 1))
                for hf in range(2):
                    A(lambda e: e.activation(out=Lm[:, hf * 512:(hf + 1) * 512], in_=pseg[hf][:, :], func=AF.Exp),
                      R=[pseg[hf]], W=[Lm])
                    yield
                A(lambda e: e.activation(out=sm[:, 0:24], in_=pd[:, 8:32], func=AF.Exp), R=[pd], W=[sm])
                A(lambda e: e.activation(out=xB[:, :], in_=pTb[:, 0:768], func=AF.Copy), R=[pT], W=[xB])
                pcb = bank(1)
                for g in range(2):
                    M(lambda e: e.matmul(pcb[:, g * 128:(g + 1) * 128], lhsT=xc(4 + g, t), rhs=xc(6 + g, t),
                                         start=True, stop=True), R=[xbcT], W=[pcb])
                yield ("set", ("mid", t))
                V(lambda e: e.tensor_tensor(out=_v3(CBm[:, :], 2, 128), in0=_v3(pcb[:, 0:256], 2, 128),
                                            in1=_bc_mid(tri_bf[:, :], 2), op=ALU.mult), R=[pcb, tri_bf], W=[CBm])
                yield
                if t > 0:
                    yield ("need", ("WmF", t - 1))
                V(lambda e: e.tensor_tensor(
                    out=Wm[:, :].rearrange("p (g a l) -> p g a l", g=2, a=4, l=128),
                    in0=Lm[:, :].rearrange("p (g a l) -> p g a l", g=2, a=4, l=128),
                    in1=_v3(CBm[:, :], 2, 128).unsqueeze(2).broadcast_to([128, 2, 4, 128]), op=ALU.mult),
                  R=[Lm, CBm], W=[Wm])
                yield ("set", ("LmF", t))
                P(lambda e: e.tensor_tensor(out=_v3(xdt[:, :], 8, 64), in0=_v3(xB[:, 0:512], 8, 64),
                                            in1=_bc_last(dtt[:, 0:8], 64), op=ALU.mult), R=[xB, dtt], W=[xdt])
                P(lambda e: e.tensor_tensor(out=_v3(xdd[:, :], 8, 64), in0=_v3(xdt[:, :], 8, 64),
                                            in1=_bc_last(sm[:, 8:16], 64), op=ALU.mult), R=[xdt, sm], W=[xdd])
                yield
                py = bank(2)
                for h in range(8):
                    M(lambda e: e.matmul(py[:, h * 64:(h + 1) * 64], lhsT=Wm[:, h * 128:(h + 1) * 128],
                                         rhs=xdt[:, h * 64:(h + 1) * 64], start=True, stop=True), R=[Wm, xdt], W=[py])
                yield ("set", ("WmF", t))
                pu = bank(3)
                for g in range(2):
                    M(lambda e: e.matmul(pu[:, g * 256:(g + 1) * 256], lhsT=xB[:, 512 + g * 128:512 + (g + 1) * 128],
                                         rhs=xdd[:, g * 256:(g + 1) * 256], start=True, stop=True), R=[xB, xdd], W=[pu])
                yield
                P(lambda e: e.tensor_tensor(out=_v3(y2[:, :], 8, 64), in0=_v3(xB[:, 0:512], 8, 64),
                                            in1=_bc_last(Dbc[:, l * 8:(l + 1) * 8], 64), op=ALU.mult), R=[xB, Dbc], W=[y2])
                yield
                if t > 0:
                    yield ("need", ("S", t - 1))
                if not first:
                    pyo = bank(0)
                    for g in range(2):
                        M(lambda e: e.matmul(pyo[:, g * 256:(g + 1) * 256], lhsT=xc(6 + g, t), rhs=Sb[:, g * 256:(g + 1) * 256],
                                             start=True, stop=True), R=[xbcT, Sb], W=[pyo])
                    yield
                V(lambda e: e.tensor_tensor(out=_v3(S[:, :], 8, 64), in0=_v3(S[:, :], 8, 64),
                                            in1=_bc_last(sm[:, 16:24], 64), op=ALU.mult), R=[S, sm], W=[S])
                V(lambda e: e.tensor_tensor(out=S[:, :], in0=S[:, :], in1=pu[:, :], op=ALU.add), R=[S, pu], W=[S])
                yield
                A(lambda e: e.activation(out=Sb[:, :], in_=S[:, :], func=AF.Copy), R=[S], W=[Sb])
                yield ("set", ("S", t))
                if not first:
                    V(lambda e: e.tensor_tensor(out=_v3(y1[:, :], 8, 64), in0=_v3(pyo[:, :], 8, 64),
                                                in1=_bc_last(sm[:, 0:8], 64), op=ALU.mult), R=[pyo, sm], W=[y1])
                    yield
                    V(lambda e: e.tensor_tensor(out=y1[:, :], in0=y1[:, :], in1=py[:, :], op=ALU.add), R=[y1, py], W=[y1])
                    yield
                    V(lambda e: e.tensor_tensor(out=y1[:, :], in0=y1[:, :], in1=y2[:, :], op=ALU.add), R=[y1, y2], W=[y1])
                else:
                    V(lambda e: e.tensor_tensor(out=y1[:, :], in0=y2[:, :], in1=py[:, :], op=ALU.add), R=[y2, py], W=[y1])
                yield
                V(lambda e: e.tensor_tensor(out=y1[:, :], in0=y1[:, :], in1=sgC[:, t * 512:(t + 1) * 512], op=ALU.mult),
                  R=[y1, sgC], W=[y1])
                yield
                A(lambda e: e.activation(out=y2[:, :], in_=y1[:, :], func=AF.Square), R=[y1], W=[y2])
                yield
                V(lambda e: e.reduce_sum(out=ssc[:, 0:2], in_=_v3(y2[:, :], 2, 256), axis=AX.X), R=[y2], W=[ssc])
                yield
                A(lambda e: e.activation(out=ssc[:, 0:2], in_=ssc[:, 0:2], func=AF.Ln, scale=1.0 / 256, bias=EPS),
                  R=[ssc], W=[ssc])
                A(lambda e: e.activation(out=ssc[:, 0:2], in_=ssc[:, 0:2], func=AF.Exp, scale=-0.5), R=[ssc], W=[ssc])
                yield
                V(lambda e: e.tensor_tensor(out=_v3(y1[:, :], 2, 256), in0=_v3(y1[:, :], 2, 256),
                                            in1=_bc_last(ssc[:, 0:2], 256), op=ALU.mult), R=[y1, ssc], W=[y1])
                yield
                P(lambda e: e.tensor_tensor(out=yc[:, :], in0=y1[:, :], in1=gainC[:, l * 512:(l + 1) * 512], op=ALU.mult),
                  R=[y1, gainC], W=[yc])
                yield
                pT2, pT2b = transposes_to(None, yc, 4, yc, p=bank(1))
                yield
                A(lambda e: e.activation(out=_v3(yT[2][:, :], 4, UNIT)[:, :, t * 128:(t + 1) * 128],
                                         in_=_v3(pT2b[:, 0:512], 4, 128), func=AF.Copy), R=[pT2], W=[yT[2]])

            run_pipelined(stagger(tile, TPU))
            w_release(jdt)

        def stage_merge(u, l):
            arena.reset()
            tgs = [[arena.alloc("tg%d_%d" % (i, j), 512, BF16) for j in range(3)] for i in range(2)]
            mms = [[arena.alloc("mm%d_%d" % (i, j), 512) for j in range(3)] for i in range(2)]
            mT = arena.alloc("mT", KC * UNIT, BF16)
            mT_b = [mT.b, mT.b]
            cnt = 0
            for g4 in range(2):
                jj, wb, wm = [], [], []
                for br in range(3):
                    j, w = w_next("M_br%d_%d" % (br, g4))
                    jj.append(j)
                    wb.append(w)
                    j, w = w_next("M_mg%d_%d" % (br, g4))
                    jj.append(j)
                    wm.append(w)
                for dcl in range(4):
                    dc = g4 * 4 + dcl
                    for b in range(BPU):
                        par = cnt % 2
                        cnt += 1
                        for br in range(3):
                            pu_, pg = ps(), ps()
                            for k in range(4):
                                M(lambda e: e.matmul(pu_[:, :], lhsT=wb[br][:, k * 512 + dcl * 128: k * 512 + (dcl + 1) * 128],
                                                     rhs=yT[br][:, k * UNIT + b * 512: k * UNIT + (b + 1) * 512],
                                                     start=(k == 0), stop=(k == 3)), R=[wb[br], yT[br]], W=[pu_])
                            for k in range(KC):
                                M(lambda e: e.matmul(pg[:, :], lhsT=wm[br][:, k * 512 + dcl * 128: k * 512 + (dcl + 1) * 128],
                                                     rhs=hv(k, b * 512, 512), start=(k == 0), stop=(k == KC - 1)),
                                  R=[wm[br], hT_b[b]], W=[pg])
                            tg, mm = tgs[par][br], mms[par][br]
                            A(lambda e: e.activation(out=tg[:, :], in_=pg[:, :], func=AF.Tanh, scale=0.5), R=[pg], W=[tg])
                            V(lambda e: e.scalar_tensor_tensor(out=mm[:, :], in0=tg[:, :], scalar=1.0, in1=pu_[:, :],
                                                               op0=ALU.add, op1=ALU.mult), R=[tg, pu_], W=[mm])
                        m0, m1, m2 = mms[par]
                        P(lambda e: e.tensor_tensor(out=m0[:, :], in0=m0[:, :], in1=m1[:, :], op=ALU.add), R=[m0, m1], W=[m0])
                        V(lambda e: e.tensor_tensor(out=mT[:, dc * UNIT + b * 512: dc * UNIT + (b + 1) * 512], in0=m0[:, :],
                                                    in1=m2[:, :], op=ALU.add), R=[m0, m2], W=[mT_b[b]])
                for j in jj:
                    w_release(j)
            jo0, wo0 = w_next("O_0")
            jo1, wo1 = w_next("O_1")
            for b in range(BPU):
                for oc in range(KC):
                    wo, ocl = (wo0, wo1)[oc // 4], oc % 4
                    p = ps()
                    for k in range(KC):
                        M(lambda e: e.matmul(p[:, :], lhsT=wo[:, k * 512 + ocl * 128: k * 512 + (ocl + 1) * 128],
                                             rhs=mT[:, k * UNIT + b * 512: k * UNIT + (b + 1) * 512],
                                             start=(k == 0), stop=(k == KC - 1)), R=[wo, mT_b[b]], W=[p])
                    V(lambda e: e.scalar_tensor_tensor(out=xv(oc, b), in0=p[:, :], scalar=0.5, in1=xv(oc, b),
                                                       op0=ALU.mult, op1=ALU.add), R=[p, xT_b[oc][b]], W=[xT_b[oc][b]])
                    if l == depth - 1 and dbg is None:
                        store_x(u, oc, b)
            w_release(jo0)
            w_release(jo1)

        for u in range(n_units):
            load_x(u)
            rope_tables(u)
            for l in range(depth):
                stage_norm(l)
                stage_attn(u, l)
                if dbg == "attn":
                    break
                stage_gla(u, l)
                if dbg == "gla":
                    break
                stage_ssd(u, l)
                if dbg == "ssd":
                    break
                stage_merge(u, l)
                if dbg == "layer":
                    break
            if dbg is not None:
                break

        if dbg is not None:
            def dump(name, t, shape, dt=F32):
                o = nc.dram_tensor(name, shape, dt, kind="ExternalOutput").ap()
                C.dma("sp", o, t, s_dbg, dbg_cnt, R=[hT_b[0], hT_b[1], yT[0], yT[1], yT[2], cs2, sn2])
            dump("d_hT", hT[:, :], [128, KC * UNIT], BF16)
            dump("d_yT0", yT[0][:, :], [128, 4 * UNIT], BF16)
            dump("d_yT1", yT[1][:, :], [128, 4 * UNIT], BF16)
            dump("d_yT2", yT[2][:, :], [128, 4 * UNIT], BF16)
            dump("d_cs2", cs2[:, :], [128, TPU * 64])
            dump("d_sn2", sn2[:, :], [128, TPU * 64])
            for k in range(KC):
                for b in range(BPU):
                    store_x(0, k, b)
        for k in range(KC):
            for b in range(BPU):
                if o_cnt[k][b][0]:
                    C.E["sp"].wait_ge(s_o[k][b], o_cnt[k][b][0])
        if dbg_cnt[0]:
            C.E["sp"].wait_ge(s_dbg, dbg_cnt[0])
        if needed is not None:
            print("instructions:", C.n_inst, "sem-incs:", sum(len(v) for v in needed.values()), "sems:", C.nsem)
    return nc, C.waited


def build(n_seq, seq_len, depth, dbg=None):
    _, waited = build1(n_seq, seq_len, depth, dbg, None)
    nc, _ = build1(n_seq, seq_len, depth, dbg, waited)
    return nc


_INPUT_ORDER = ["norm_g", "w_in", "attn_q_norm", "attn_k_norm", "attn_sinks", "gla_w_gate_up", "gla_b_gate",
                "gla_out_norm", "ssd_conv_w", "ssd_conv_b", "ssd_dt_bias", "ssd_A_log", "ssd_D", "ssd_out_norm",
                "w_branch", "w_out"]


def kernel(**inputs):
    x = np.ascontiguousarray(np.asarray(inputs["x"], dtype=np.float32))
    positions = np.asarray(inputs["positions"]).astype(np.int32)
    B, S, dm = x.shape
    depth = int(np.asarray(inputs["w_in"]).shape[0])
    assert dm == D and B % NCORES == 0
    per = B // NCORES
    nc = build(per, S, depth)
    inv_freq = (np.float32(10000.0) ** (-(np.arange(0, 64, 2, dtype=np.float32)) / np.float32(64))).astype(np.float32)
    shared = {k: np.ascontiguousarray(np.asarray(inputs[k], dtype=np.float32)) for k in _INPUT_ORDER}
    shared["inv_freq"] = inv_freq.reshape(1, 32)
    in_maps = []
    for c in range(NCORES):
        xs = x[c * per:(c + 1) * per].reshape(per * S, D)
        m = dict(shared)
        m["xT"] = np.ascontiguousarray(xs.T)
        pos = positions[c * per:(c + 1) * per].reshape(per * S // UNIT, TPU, 128)
        m["pos"] = np.ascontiguousarray(pos.transpose(0, 2, 1))
        in_maps.append(m)
    res = run_bass_kernel_spmd(nc, in_maps, core_ids=list(range(NCORES)))
    outs = [np.asarray(r["outT"]).T.reshape(per, S, D) for r in res.results]
    return np.ascontiguousarray(np.concatenate(outs, axis=0).astype(np.float32))
```

```python
import math
import os
GLA_STOP = int(os.environ.get('GLA_STOP', '99'))
STRICT = int(os.environ.get('STRICT', '0'))
GLA_VAR = int(os.environ.get('GLA_VAR', '0'))
ATT_PAD = int(os.environ.get('ATT_PAD', '1'))
PIPE = int(os.environ.get('PIPE', '1'))
STAGGER = int(os.environ.get('STAGGER', '1'))
GLA_NOINTER = int(os.environ.get('GLA_NOINTER', '0'))
from contextlib import ExitStack

import numpy as np
import concourse.bass as bass
import concourse.mybir as mybir
from concourse.bass_utils import run_bass_kernel_spmd

F32 = mybir.dt.float32
BF16 = mybir.dt.bfloat16
I32 = mybir.dt.int32
AF = mybir.ActivationFunctionType
ALU = mybir.AluOpType
AX = mybir.AxisListType

D = 1024
KC = 8
NCORES = 8
IN_DIM = 7448
EPS = 1e-6
UNIT = 1024
TPU = UNIT // 128
BPU = UNIT // 512
RING = 6
RING_ELEMS = 4096
TWO_PI = 2.0 * math.pi

C_AQ, C_AK, C_AV, C_AG = 0, 512, 640, 768
C_GQ, C_GK, C_GV, C_GG, C_GD = 1280, 1536, 1792, 2304, 2816
C_XBC, C_DT, C_Z, C_MG = 2832, 3856, 3864, 4376


class Buf:
    __slots__ = ("name", "R", "W", "psum")

    def __init__(self, name, psum=False):
        self.name = name
        self.R = {}
        self.W = {}
        self.psum = psum


class T:
    def __init__(self, t, buf=None, name=None):
        self.t = t
        self.b = buf if buf is not None else Buf(name or "t")

    def __getitem__(self, k):
        return self.t[k]


def _bufs(lst):
    out = []
    for x in lst:
        if x is None:
            continue
        out.append(x.b if isinstance(x, T) else x)
    return out


class Ctx:
    def __init__(self, nc, es, needed=None):
        self.nc = nc
        self.es = es
        self.rank = None
        if needed is not None:
            self.rank = {k: {v: i + 1 for i, v in enumerate(sorted(vs))} for k, vs in needed.items()}
        self.waited = {k: set() for k in ("pe", "act", "dve", "pool")}
        self.E = {"pe": nc.tensor, "act": nc.scalar, "dve": nc.vector, "pool": nc.gpsimd, "sp": nc.sync}
        self.sem = {k: es.enter_context(nc.semaphore("sem_" + k)) for k in ("pe", "act", "dve", "pool")}
        self.cnt = {k: 0 for k in self.sem}
        self.seen = {k: {} for k in self.E}
        self.nsem = 0
        self.epoch_bufs = []
        self.inherit = ({}, {})
        self.n_inst = 0

    def sb(self, name, shape, dt=F32):
        return T(self.es.enter_context(self.nc.sbuf_tensor(name, shape, dt)), name=name)

    def new_sem(self, name):
        self.nsem += 1
        return self.es.enter_context(self.nc.semaphore(name))

    def _wait(self, eng, deps):
        e = self.E[eng]
        seen = self.seen[eng]
        for key, (sem, val) in deps.items():
            if seen.get(key, 0) >= val:
                continue
            if key in self.waited:
                self.waited[key].add(val)
                e.wait_ge(sem, self.rank[key][val] if self.rank is not None else val)
            else:
                e.wait_ge(sem, val)
            seen[key] = val

    def _deps(self, eng, R, W):
        deps = {}

        def add(key, sv):
            if key == eng and eng == "pe":
                return
            cur = deps.get(key)
            if cur is None or cur[1] < sv[1]:
                deps[key] = sv

        for b in R:
            for k, sv in b.W.items():
                add(k, sv)
            if b.psum:
                for k, sv in b.R.items():
                    if k != eng:
                        add(k, sv)
        for b in W:
            for k, sv in b.R.items():
                if k != eng or STRICT:
                    add(k, sv)
            for k, sv in b.W.items():
                if k != eng or STRICT:
                    add(k, sv)
        return deps

    def op(self, eng, fn, R=(), W=()):
        R = _bufs(R)
        W = _bufs(W)
        self._wait(eng, self._deps(eng, R, W))
        inst = fn(self.E[eng])
        self.cnt[eng] += 1
        self.n_inst += 1
        if self.rank is None or self.cnt[eng] in self.rank[eng]:
            inst.then_inc(self.sem[eng], 1)
        sv = (self.sem[eng], self.cnt[eng])
        for b in R:
            b.R[eng] = sv
        for b in W:
            b.W[eng] = sv
        return inst

    def dma(self, q, out, in_, sem, semstate, R=(), W=(), **kw):
        R = _bufs(R)
        W = _bufs(W)
        self._wait(q, self._deps(q, R, W))
        self.E[q].dma_start(out=out, in_=in_, **kw).then_inc(sem, 16)
        semstate[0] += 16
        key = "dma:" + str(id(sem))
        sv = (sem, semstate[0])
        for b in R:
            b.R[key] = sv
        for b in W:
            b.W[key] = sv

    def new_epoch(self):
        R, W = {}, {}
        for b in self.epoch_bufs:
            for src in (b.R, b.W):
                for k, sv in src.items():
                    if k not in R or R[k][1] < sv[1]:
                        R[k] = sv
        for k, sv in self.inherit[0].items():
            if k not in R or R[k][1] < sv[1]:
                R[k] = sv
        self.inherit = (R, dict(R))
        self.epoch_bufs = []

    def scratch_buf(self, name):
        b = Buf(name)
        b.R = dict(self.inherit[0])
        b.W = dict(self.inherit[1])
        self.epoch_bufs.append(b)
        return b


class Arena:
    def __init__(self, C, name, nbytes):
        self.C = C
        self.n32 = nbytes // 4
        self.t = C.es.enter_context(C.nc.sbuf_tensor(name, [128, self.n32], F32))
        self.off = 0

    def reset(self):
        self.off = 0
        self.C.new_epoch()

    def rewind(self, mark, old):
        R = dict(self.C.inherit[0])
        for t in old:
            for src in (t.b.R, t.b.W):
                for k, sv in src.items():
                    if k not in R or R[k][1] < sv[1]:
                        R[k] = sv
        self.C.inherit = (R, dict(R))
        self.off = mark

    def alloc(self, name, nelem, dt=F32, parts=128):
        n32 = nelem if dt in (F32, I32) else (nelem + 1) // 2
        assert self.off + n32 <= self.n32, (name, self.off, n32, self.n32)
        ap = self.t[0:parts, self.off:self.off + n32]
        if dt != F32:
            ap = ap.bitcast(dt)
        self.off += n32
        return T(ap, self.C.scratch_buf(name))


def _v3(ap, a, b):
    return ap.rearrange("p (a b) -> p a b", a=a, b=b)


def _bc_last(ap2, n):
    return ap2.unsqueeze(2).broadcast_to([ap2.shape[0], ap2.shape[1], n])


def _bc_mid(ap2, n):
    return ap2.unsqueeze(1).broadcast_to([ap2.shape[0], n, ap2.shape[1]])


def build1(n_seq, seq_len, depth, dbg=None, needed=None):
    assert seq_len % UNIT == 0
    upseq = seq_len // UNIT
    n_units = n_seq * upseq
    NT = n_seq * seq_len
    nc = bass.Bass("TRN2", target_bir_lowering=False)

    def din(name, shape, dt=F32):
        return nc.dram_tensor(name, shape, dt, kind="ExternalInput")

    xT_d = din("xT", [D, NT]).ap()
    pos_d = din("pos", [n_units, 128, TPU], I32).ap()
    invf_d = din("inv_freq", [1, 32])
    norm_g_d = din("norm_g", [depth, D]).ap()
    w_in_d = din("w_in", [depth, D, IN_DIM]).ap()
    qn_d = din("attn_q_norm", [depth, 64])
    kn_d = din("attn_k_norm", [depth, 64])
    sinks_d = din("attn_sinks", [depth, 8])
    wup_d = din("gla_w_gate_up", [depth, 16, 256]).ap()
    bg_d = din("gla_b_gate", [depth, 256]).ap()
    gon_d = din("gla_out_norm", [depth, 128])
    cw_d = din("ssd_conv_w", [depth, 4, D]).ap()
    cb_d = din("ssd_conv_b", [depth, D]).ap()
    dtb_d = din("ssd_dt_bias", [depth, 8])
    alog_d = din("ssd_A_log", [depth, 8])
    dsk_d = din("ssd_D", [depth, 8])
    son_d = din("ssd_out_norm", [depth, 512])
    wbr_d = din("w_branch", [depth, 3, 512, D]).ap()
    wout_d = din("w_out", [depth, D, D]).ap()
    outT_d = nc.dram_tensor("outT", [D, NT], F32, kind="ExternalOutput").ap()
    dbg_out = {}

    with ExitStack() as es:
        C = Ctx(nc, es, needed)
        es.enter_context(nc.allow_non_contiguous_dma(reason="small parameter layouts"))

        def bcast_src(handle, row, n, reps=None):
            if reps is None:
                return bass.AP(handle, row * n, [[0, 128], [1, n]])
            return bass.AP(handle, row * n, [[0, 128], [0, reps], [1, n]])

        xT = C.sb("xTs", [128, KC * UNIT])
        xT_b = [[Buf("xT%d_%d" % (k, b)) for b in range(BPU)] for k in range(KC)]
        hT = C.sb("hT", [128, KC * UNIT], BF16)
        hT_b = [Buf("hT%d" % b) for b in range(BPU)]
        yT = [C.sb("yT%d" % i, [128, 4 * UNIT], BF16) for i in range(3)]
        ring = [C.sb("ring%d" % i, [128, RING_ELEMS], BF16) for i in range(RING)]
        ring_sem = [C.new_sem("rsem%d" % i) for i in range(RING)]
        ring_cnt = [[0] for _ in range(RING)]
        arena = Arena(C, "arena", 49 * 1024)

        def xv(k, b):
            return xT[:, k * UNIT + b * 512: k * UNIT + (b + 1) * 512]

        def hv(k, lo, n):
            return hT[:, k * UNIT + lo: k * UNIT + lo + n]

        ones_bf = C.sb("ones_bf", [128, 128], BF16)
        ident_bf = C.sb("ident_bf", [128, 128], BF16)
        tri_f = C.sb("tri_f", [128, 128])
        sg_f = C.sb("sg_f", [128, 128])
        nsg_f = C.sb("nsg_f", [128, 128])
        ones_f = C.sb("ones_f", [128, 128])
        tri_bf = C.sb("tri_bf", [128, 128], BF16)
        sg_bf = C.sb("sg_bf", [128, 128], BF16)
        g_col = C.sb("g_col", [128, depth * KC])
        qkgain = C.sb("qkgain", [128, depth * 640])
        expsink = C.sb("expsink", [128, depth * 8])
        wup_bf = C.sb("wup_bf", [32, depth * 256], BF16)
        gainB = C.sb("gainB", [128, depth * 128])
        cw = C.sb("cw", [128, depth * 32])
        cb = C.sb("cb", [128, depth * 8])
        dtb = C.sb("dtb", [128, depth * 8])
        Abc = C.sb("Abc", [128, depth * 8])
        Dbc = C.sb("Dbc", [128, depth * 8])
        gainC = C.sb("gainC", [128, depth * 512])
        invf = C.sb("invf", [128, 32])
        posi = C.sb("posi", [128, TPU], I32)
        cs2 = C.sb("cs2", [128, TPU * 64])
        sn2 = C.sb("sn2", [128, TPU * 64])
        kT_st = [[C.sb("kT%d_%d" % (l, i), [128, 256], BF16) for i in range(3)] for l in range(depth)]
        v_st = [[C.sb("vaug%d_%d" % (l, i), [128, 130], BF16) for i in range(3)] for l in range(depth)]
        S_gla = [C.sb("Sgla%d" % l, [128, 512]) for l in range(depth)]
        S_gla_bf = [C.sb("Sglab%d" % l, [128, 512], BF16) for l in range(depth)]
        S_ssd = [C.sb("Sssd%d" % l, [128, 512]) for l in range(depth)]
        S_ssd_bf = [C.sb("Sssdb%d" % l, [128, 512], BF16) for l in range(depth)]
        ctail = [C.sb("ctail%d" % l, [128, 24]) for l in range(depth)]

        psum = [T(es.enter_context(nc.psum_tensor("ps%d" % i, [128, 512], F32)), buf=Buf("ps%d" % i, psum=True)) for i in range(8)]
        ps_i = [0]

        def ps():
            p = psum[ps_i[0] % 8]
            ps_i[0] += 1
            return p

        identf = arena.alloc("identf", 128)
        wup_f = arena.alloc("wup_f", depth * 256)
        s_const = C.new_sem("s_const")
        cst = [0]

        def cdma(out, in_, W):
            C.dma("sp", out, in_, s_const, cst, W=[W])

        for l in range(depth):
            cdma(g_col[:, l * KC:(l + 1) * KC], norm_g_d[l].rearrange("(k p) -> p k", p=128), g_col)
            cdma(_v3(qkgain[:, l * 640: l * 640 + 512], 8, 64), bcast_src(qn_d, l, 64, 8), qkgain)
            cdma(_v3(qkgain[:, l * 640 + 512:(l + 1) * 640], 2, 64), bcast_src(kn_d, l, 64, 2), qkgain)
            cdma(expsink[:, l * 8:(l + 1) * 8], bcast_src(sinks_d, l, 8), expsink)
            cdma(wup_f[0:16, l * 256:(l + 1) * 256], wup_d[l], wup_f)
            cdma(wup_f[16:17, l * 256:(l + 1) * 256], bg_d[l:l + 1, :], wup_f)
            cdma(gainB[:, l * 128:(l + 1) * 128], bcast_src(gon_d, l, 128), gainB)
            for k in range(4):
                cdma(_v3(cw[:, l * 32:(l + 1) * 32], 8, 4)[:, :, k], cw_d[l, k].rearrange("(c p) -> p c", p=128), cw)
            cdma(cb[:, l * 8:(l + 1) * 8], cb_d[l].rearrange("(c p) -> p c", p=128), cb)
            cdma(dtb[:, l * 8:(l + 1) * 8], bcast_src(dtb_d, l, 8), dtb)
            cdma(Abc[:, l * 8:(l + 1) * 8], bcast_src(alog_d, l, 8), Abc)
            cdma(Dbc[:, l * 8:(l + 1) * 8], bcast_src(dsk_d, l, 8), Dbc)
            cdma(gainC[:, l * 512:(l + 1) * 512], bcast_src(son_d, l, 512), gainC)
        cdma(invf[:, :], bcast_src(invf_d, 0, 32), invf)
        for t_ in (g_col, qkgain, expsink, wup_f, gainB, cw, cb, dtb, Abc, Dbc, gainC, invf):
            for k_ in list(t_.b.W):
                t_.b.W[k_] = (s_const, cst[0])

        P = lambda fn, R=(), W=(): C.op("pool", fn, R, W)
        V = lambda fn, R=(), W=(): C.op("dve", fn, R, W)
        A = lambda fn, R=(), W=(): C.op("act", fn, R, W)
        M = lambda fn, R=(), W=(): C.op("pe", fn, R, W)

        P(lambda e: e.memset(ones_f[:, :], 1.0), W=[ones_f])
        P(lambda e: e.memset(tri_f[:, :], 1.0), W=[tri_f])
        P(lambda e: e.memset(sg_f[:, :], 1.0), W=[sg_f])
        P(lambda e: e.memset(identf[:, :], 0.0), W=[identf])
        P(lambda e: e.affine_select(out=tri_f[:, :], in_=tri_f[:, :], pattern=[[1, 128]], compare_op=ALU.is_ge,
                                    fill=0.0, base=0, channel_multiplier=-1), R=[tri_f], W=[tri_f])
        P(lambda e: e.affine_select(out=sg_f[:, :], in_=sg_f[:, :], pattern=[[-1, 128]], compare_op=ALU.is_gt,
                                    fill=0.0, base=0, channel_multiplier=1), R=[sg_f], W=[sg_f])
        P(lambda e: e.affine_select(out=identf[:, :], in_=identf[:, :], pattern=[[-1, 128]], compare_op=ALU.not_equal,
                                    fill=1.0, base=0, channel_multiplier=1), R=[identf], W=[identf])
        V(lambda e: e.tensor_copy(out=ones_bf[:, :], in_=ones_f[:, :]), R=[ones_f], W=[ones_bf])
        V(lambda e: e.tensor_copy(out=tri_bf[:, :], in_=tri_f[:, :]), R=[tri_f], W=[tri_bf])
        V(lambda e: e.tensor_copy(out=sg_bf[:, :], in_=sg_f[:, :]), R=[sg_f], W=[sg_bf])
        V(lambda e: e.tensor_copy(out=ident_bf[:, :], in_=identf[:, :]), R=[identf], W=[ident_bf])
        V(lambda e: e.tensor_scalar(out=nsg_f[:, :], in0=sg_f[:, :], scalar1=-1.0, scalar2=None, op0=ALU.mult),
          R=[sg_f], W=[nsg_f])
        A(lambda e: e.activation(out=expsink[:, :], in_=expsink[:, :], func=AF.Exp), R=[expsink], W=[expsink])
        A(lambda e: e.activation(out=Abc[:, :], in_=Abc[:, :], func=AF.Exp), R=[Abc], W=[Abc])
        V(lambda e: e.tensor_scalar(out=Abc[:, :], in0=Abc[:, :], scalar1=-1.0, scalar2=None, op0=ALU.mult),
          R=[Abc], W=[Abc])
        V(lambda e: e.tensor_copy(out=wup_bf[0:17, :], in_=wup_f[0:17, :]), R=[wup_f], W=[wup_bf])
        for l in range(depth):
            for i in range(3):
                V(lambda e, t=v_st[l][i]: e.memset(t[:, :], 1.0), W=[v_st[l][i]])
                V(lambda e, t=kT_st[l][i]: e.memset(t[:, :], 0.0), W=[kT_st[l][i]])

        def w_in_piece(l, c0, n):
            return w_in_d[l][:, c0:c0 + n].rearrange("(k p) c -> p k c", p=128), KC * n

        pieces = []
        for u in range(n_units):
            for l in range(depth):
                pl = [("A_qkv", [w_in_piece(l, C_AQ, 512)]), ("A_kv", [w_in_piece(l, C_AK, 256)]),
                      ("A_g", [w_in_piece(l, C_AG, 512)]),
                      ("B_g", [w_in_piece(l, C_GG, 512)]), ("B_qk", [w_in_piece(l, C_GQ, 512)]),
                      ("B_v", [w_in_piece(l, C_GV, 512)]), ("B_d", [w_in_piece(l, C_GD, 16)]),
                      ("C_z", [w_in_piece(l, C_Z, 512)]), ("C_x", [w_in_piece(l, C_XBC, 512)]),
                      ("C_bc", [w_in_piece(l, C_XBC + 512, 512)]), ("C_dt", [w_in_piece(l, C_DT, 8)])]
                for g4 in range(2):
                    for br in range(3):
                        pl.append(("M_br%d_%d" % (br, g4),
                                   [(wbr_d[l, br][:, g4 * 512:(g4 + 1) * 512].rearrange("(k p) c -> p k c", p=128), 4 * 512)]))
                        pl.append(("M_mg%d_%d" % (br, g4), [w_in_piece(l, C_MG + br * 1024 + g4 * 512, 512)]))
                for oh in range(2):
                    pl.append(("O_%d" % oh, [(wout_d[l][:, oh * 512:(oh + 1) * 512].rearrange("(k p) c -> p k c", p=128), KC * 512)]))
                pieces.extend(pl)
        wst = {"issued": 0, "released": set(), "next": 0}

        def w_pump():
            while wst["issued"] < len(pieces):
                j = wst["issued"]
                if j >= RING and (j - RING) not in wst["released"]:
                    break
                if j - wst["next"] >= RING:
                    break
                slot = j % RING
                name, parts = pieces[j]
                off = 0
                for src, n in parts:
                    kk = src.shape[1]
                    dst = ring[slot][:, off:off + n].rearrange("p (k c) -> p k c", k=kk)
                    C.dma("pool", dst, src, ring_sem[slot], ring_cnt[slot], W=[ring[slot]])
                    off += n
                wst["issued"] += 1

        def w_next(name):
            j = wst["next"]
            assert pieces[j][0] == name, (pieces[j][0], name)
            w_pump()
            assert wst["issued"] > j, "weight ring too small at piece %d %s" % (j, name)
            wst["next"] += 1
            return j, ring[j % RING]

        def w_release(j):
            wst["released"].add(j)
            w_pump()

        def wv(rt, n):
            return lambda k, c0=0, cn=None: rt[:, k * n + c0: k * n + (n if cn is None else c0 + cn)]

        s_x = [[C.new_sem("s_x%d_%d" % (k, b)) for b in range(BPU)] for k in range(KC)]
        x_cnt = [[[0] for b in range(BPU)] for k in range(KC)]
        s_o = [[C.new_sem("s_o%d_%d" % (k, b)) for b in range(BPU)] for k in range(KC)]
        o_cnt = [[[0] for b in range(BPU)] for k in range(KC)]
        s_dbg = C.new_sem("s_dbg")
        dbg_cnt = [0]
        s_pos = C.new_sem("s_pos")
        pos_cnt = [0]

        def load_x_block(u, b):
            t0 = u * UNIT
            for k in range(KC):
                C.dma("sp", xv(k, b), xT_d[k * 128:(k + 1) * 128, t0 + b * 512: t0 + (b + 1) * 512], s_x[k][b], x_cnt[k][b],
                      W=[xT_b[k][b]])

        def load_x(u):
            for b in range(BPU):
                load_x_block(u, b)

        def store_x(u, k, b):
            t0 = u * UNIT
            C.dma("sp", outT_d[k * 128:(k + 1) * 128, t0 + b * 512: t0 + (b + 1) * 512], xv(k, b), s_o[k][b], o_cnt[k][b],
                  R=[xT_b[k][b]])

        def rope_tables(u):
            arena.reset()
            s_p = None
            C.dma("sp", posi[:, :], pos_d[u], s_pos, pos_cnt, W=[posi])
            posf = arena.alloc("posf", TPU)
            ang = arena.alloc("ang", TPU * 32)
            kf = arena.alloc("kf", TPU * 32)
            ki = arena.alloc("ki", TPU * 32, I32)
            V(lambda e: e.tensor_copy(out=posf[:, :], in_=posi[:, :]), R=[posi], W=[posf])
            V(lambda e: e.tensor_tensor(out=_v3(ang[:, :], TPU, 32), in0=_bc_last(posf[:, :], 32),
                                        in1=_bc_mid(invf[:, :], TPU), op=ALU.mult), R=[posf, invf], W=[ang])
            for shift, half in ((0.0, "sin"), (0.5 * math.pi, "cos")):
                V(lambda e: e.tensor_scalar(out=kf[:, :], in0=ang[:, :], scalar1=shift, scalar2=1.0 / TWO_PI,
                                            op0=ALU.add, op1=ALU.mult), R=[ang], W=[kf])
                V(lambda e: e.tensor_copy(out=ki[:, :], in_=kf[:, :]), R=[kf], W=[ki])
                V(lambda e: e.tensor_copy(out=kf[:, :], in_=ki[:, :]), R=[ki], W=[kf])
                V(lambda e: e.scalar_tensor_tensor(out=kf[:, :], in0=kf[:, :], scalar=-TWO_PI, in1=ang[:, :],
                                                   op0=ALU.mult, op1=ALU.add), R=[kf, ang], W=[kf])
                if half == "sin":
                    A(lambda e: e.activation(out=_v3(sn2[:, :], TPU, 64)[:, :, 32:64], in_=_v3(kf[:, :], TPU, 32),
                                             func=AF.Sin, bias=0.0, scale=1.0), R=[kf], W=[sn2])
                    V(lambda e: e.tensor_scalar(out=_v3(sn2[:, :], TPU, 64)[:, :, 0:32],
                                                in0=_v3(sn2[:, :], TPU, 64)[:, :, 32:64], scalar1=-1.0, scalar2=None,
                                                op0=ALU.mult), R=[sn2], W=[sn2])
                else:
                    A(lambda e: e.activation(out=_v3(cs2[:, :], TPU, 64)[:, :, 0:32], in_=_v3(kf[:, :], TPU, 32),
                                             func=AF.Sin, bias=shift, scale=1.0), R=[kf], W=[cs2])
                    V(lambda e: e.tensor_copy(out=_v3(cs2[:, :], TPU, 64)[:, :, 32:64],
                                              in_=_v3(cs2[:, :], TPU, 64)[:, :, 0:32]), R=[cs2], W=[cs2])

        def stage_norm(l):
            arena.reset()
            sq = [arena.alloc("sq%d" % i, KC * 512, BF16) for i in range(1)]
            lnv = arena.alloc("lnv", 512)
            rstd = [arena.alloc("rstd%d" % i, 512) for i in range(2)]
            for b in range(BPU):
                s = sq[0]
                for k in range(KC):
                    A(lambda e: e.activation(out=s[:, k * 512:(k + 1) * 512], in_=xv(k, b), func=AF.Square),
                      R=[xT_b[k][b]], W=[s])
                p = ps()
                for k in range(KC):
                    M(lambda e: e.matmul(p[:, :], lhsT=ones_bf[:, :], rhs=s[:, k * 512:(k + 1) * 512],
                                         start=(k == 0), stop=(k == KC - 1)), R=[ones_bf, s], W=[p])
                A(lambda e: e.activation(out=lnv[:, :], in_=p[:, :], func=AF.Ln, scale=1.0 / D, bias=EPS),
                  R=[p], W=[lnv])
                r = rstd[b % 2]
                A(lambda e: e.activation(out=r[:, :], in_=lnv[:, :], func=AF.Exp, scale=-0.5), R=[lnv], W=[r])
                for k in range(KC):
                    V(lambda e: e.scalar_tensor_tensor(out=hv(k, b * 512, 512), in0=xv(k, b),
                                                       scalar=g_col[:, l * KC + k: l * KC + k + 1], in1=r[:, :],
                                                       op0=ALU.mult, op1=ALU.mult),
                      R=[xT_b[k][b], g_col, r], W=[hT_b[b]])

        def proj_tok(p, n_out, wfn, t, c0=0, wt=None, pcol=0):
            for k in range(KC):
                M(lambda e: e.matmul(p[:, pcol:pcol + n_out], lhsT=hv(k, t * 128, 128), rhs=wfn(k, c0, n_out),
                                     start=(k == 0), stop=(k == KC - 1)), R=[hT_b[t // 4], wt], W=[p])

        def transposes_to(dst_fn, src, nchunks, Rsrc, evac="act", p=None):
            if p is None:
                p = ps()
            pb = p[:, :].bitcast(BF16)
            for c in range(nchunks):
                M(lambda e: e.transpose(out=pb[:, c * 128:(c + 1) * 128], in_=src[:, c * 128:(c + 1) * 128],
                                        identity=ident_bf[:, :]), R=[Rsrc, ident_bf], W=[p])
            return p, pb

        def run_pipelined(gens, depth=2):
            if not PIPE:
                depth = 1
            tokens = set()
            active, blocked, idx = [], {}, 0
            while active or idx < len(gens):
                while len(active) < depth and idx < len(gens):
                    active.append(gens[idx])
                    idx += 1
                progressed = False
                for g in list(active):
                    need = blocked.get(id(g))
                    if need is not None:
                        if need not in tokens:
                            continue
                        blocked[id(g)] = None
                    progressed = True
                    try:
                        r = next(g)
                    except StopIteration:
                        active.remove(g)
                        continue
                    if r is not None:
                        kind, tok = r
                        if kind == "set":
                            tokens.add(tok)
                        elif tok not in tokens:
                            blocked[id(g)] = tok
                assert progressed, "pipeline deadlock"

        def stagger(mk, n):
            def wrap(t):
                if t > 0 and STAGGER:
                    yield ("need", ("mid", t - 1))
                yield from mk(t)
            return [wrap(t) for t in range(n)]

        def stage_attn(u, l):
            arena.reset()
            first_unit = (u % upseq == 0)
            sgA = arena.alloc("sgA", TPU * 512, BF16)
            sqbs = [arena.alloc("sqb%d" % i, 640) for i in range(2)]
            sss = [arena.alloc("ss%d" % i, 16) for i in range(2)]
            qkns = [arena.alloc("qkn%d" % i, 640) for i in range(2)]
            tmp1s = [arena.alloc("tmp1%d" % i, 640) for i in range(2)]
            tmp2s = [arena.alloc("tmp2%d" % i, 640) for i in range(2)]
            qkr = [arena.alloc("qkr%d" % i, 640, BF16) for i in range(2)]
            qTs = [arena.alloc("qT%d" % i, 512, BF16) for i in range(2)]
            Eb = [[arena.alloc("E%d_%d" % (i, j), 512, BF16) for j in range(4)] for i in range(2)]
            dens = [arena.alloc("den%d" % i, 16) for i in range(2)]
            yAs = [arena.alloc("yA%d" % i, 512) for i in range(2)]
            y_a = [arena.alloc("y_a%d" % i, 512, BF16) for i in range(2)]
            jq, wq = w_next("A_qkv")
            jk, wk = w_next("A_kv")
            jg, wg = w_next("A_g")
            wqf, wkf, wgf = wv(wq, 512), wv(wk, 256), wv(wg, 512)
            for t in range(TPU):
                p = ps()
                proj_tok(p, 512, wgf, t, wt=wg)
                A(lambda e: e.activation(out=sgA[:, t * 512:(t + 1) * 512], in_=p[:, :], func=AF.Silu), R=[p], W=[sgA])
            w_release(jg)

            def tile(t):
                gt = u * TPU + t
                first = first_unit and t == 0
                kT_c, kT_p = kT_st[l][gt % 3], kT_st[l][(gt + 2) % 3]
                v_c, v_p = v_st[l][gt % 3], v_st[l][(gt + 2) % 3]
                sqb, ss, qkn, tmp1, tmp2 = sqbs[t % 2], sss[t % 2], qkns[t % 2], tmp1s[t % 2], tmp2s[t % 2]
                den, yA = dens[t % 2], yAs[t % 2]
                p1, p2 = ps(), ps()
                proj_tok(p1, 512, wqf, t, wt=wq)
                proj_tok(p2, 256, wkf, t, wt=wk)
                yield
                A(lambda e: e.activation(out=sqb[:, 0:512], in_=p1[:, :], func=AF.Square), R=[p1], W=[sqb])
                A(lambda e: e.activation(out=sqb[:, 512:640], in_=p2[:, 0:128], func=AF.Square), R=[p2], W=[sqb])
                yield
                V(lambda e: e.reduce_sum(out=ss[:, 0:10], in_=_v3(sqb[:, :], 10, 64), axis=AX.X), R=[sqb], W=[ss])
                yield
                A(lambda e: e.activation(out=ss[:, 0:10], in_=ss[:, 0:10], func=AF.Ln, scale=1.0 / 64, bias=EPS),
                  R=[ss], W=[ss])
                A(lambda e: e.activation(out=ss[:, 0:10], in_=ss[:, 0:10], func=AF.Exp, scale=-0.5), R=[ss], W=[ss])
                yield
                V(lambda e: e.tensor_tensor(out=_v3(qkn[:, 0:512], 8, 64), in0=_v3(p1[:, :], 8, 64),
                                            in1=_bc_last(ss[:, 0:8], 64), op=ALU.mult), R=[p1, ss], W=[qkn])
                V(lambda e: e.tensor_tensor(out=_v3(qkn[:, 512:640], 2, 64), in0=_v3(p2[:, 0:128], 2, 64),
                                            in1=_bc_last(ss[:, 8:10], 64), op=ALU.mult), R=[p2, ss], W=[qkn])
                yield
                A(lambda e: e.activation(out=_v3(v_c[:, :], 2, 65)[:, :, 0:64], in_=_v3(p2[:, 128:256], 2, 64),
                                         func=AF.Copy), R=[p2], W=[v_c])
                V(lambda e: e.tensor_tensor(out=qkn[:, :], in0=qkn[:, :], in1=qkgain[:, l * 640:(l + 1) * 640],
                                            op=ALU.mult), R=[qkn, qkgain], W=[qkn])
                yield
                cs_t = cs2[:, t * 64:(t + 1) * 64]
                sn_t = sn2[:, t * 64:(t + 1) * 64]
                V(lambda e: e.tensor_tensor(out=_v3(tmp1[:, :], 10, 64), in0=_v3(qkn[:, :], 10, 64),
                                            in1=_bc_mid(cs_t, 10), op=ALU.mult), R=[qkn, cs2], W=[tmp1])
                V(lambda e: e.tensor_tensor(out=_v3(tmp2[:, :], 10, 64)[:, :, 0:32], in0=_v3(qkn[:, :], 10, 64)[:, :, 32:64],
                                            in1=_bc_mid(sn_t[:, 0:32], 10), op=ALU.mult), R=[qkn, sn2], W=[tmp2])
                V(lambda e: e.tensor_tensor(out=_v3(tmp2[:, :], 10, 64)[:, :, 32:64], in0=_v3(qkn[:, :], 10, 64)[:, :, 0:32],
                                            in1=_bc_mid(sn_t[:, 32:64], 10), op=ALU.mult), R=[qkn, sn2], W=[tmp2])
                yield
                qk = qkr[t % 2]
                V(lambda e: e.tensor_tensor(
                    out=qk[:, 0:512].rearrange("p (a g d) -> p g a d", a=4, g=2, d=64),
                    in0=tmp1[:, 0:512].rearrange("p (g a d) -> p g a d", g=2, a=4, d=64),
                    in1=tmp2[:, 0:512].rearrange("p (g a d) -> p g a d", g=2, a=4, d=64), op=ALU.add),
                  R=[tmp1, tmp2], W=[qk])
                V(lambda e: e.tensor_tensor(out=qk[:, 512:640], in0=tmp1[:, 512:640], in1=tmp2[:, 512:640], op=ALU.add),
                  R=[tmp1, tmp2], W=[qk])
                yield
                pT, pTb = transposes_to(None, qk, 5, qk)
                yield ("set", ("mid", t))
                qT = qTs[t % 2]
                A(lambda e: e.activation(out=qT[:, :], in_=pTb[:, 0:512], func=AF.Copy), R=[pT], W=[qT])
                for g in range(2):
                    V(lambda e: e.tensor_copy(out=kT_c[g * 64:(g + 1) * 64, g * 128:(g + 1) * 128],
                                              in_=pTb[g * 64:(g + 1) * 64, 512:640]), R=[pT], W=[kT_c])
                yield ("set", ("kv", gt))
                if not first and t > 0:
                    yield ("need", ("kv", gt - 1))
                E = Eb[t % 2]
                blocks = [("c", kT_c, v_c)] if first else [("p", kT_p, v_p), ("c", kT_c, v_c)]
                for g in range(2):
                    for bi, (tag, kTt, _) in enumerate(blocks):
                        p = ps()
                        M(lambda e: e.matmul(p[:, :], lhsT=kTt[:, g * 128:(g + 1) * 128], rhs=qT[:, :],
                                             start=True, stop=True), R=[kTt, qT], W=[p])
                        yield
                        Et = E[g * 2 + bi]
                        A(lambda e: e.activation(out=Et[:, :], in_=p[:, :], func=AF.Exp, scale=0.125), R=[p], W=[Et])
                        yield
                        msk = tri_bf if tag == "c" else sg_bf
                        V(lambda e: e.tensor_tensor(out=_v3(Et[:, :], 4, 128), in0=_v3(Et[:, :], 4, 128),
                                                    in1=_bc_mid(msk[:, :], 4), op=ALU.mult), R=[Et, msk], W=[Et])
                yield
                po = [ps(), ps()]
                for h in range(8):
                    g, a = h // 4, h % 4
                    for bi, (tag, _, vt) in enumerate(blocks):
                        Et = E[g * 2 + bi]
                        M(lambda e: e.matmul(po[g][:, a * 65:(a + 1) * 65], lhsT=Et[:, a * 128:(a + 1) * 128],
                                             rhs=vt[:, g * 65:(g + 1) * 65], start=(bi == 0),
                                             stop=(bi == len(blocks) - 1)), R=[Et, vt], W=[po[g]])
                yield
                for g in range(2):
                    V(lambda e: e.tensor_tensor(out=den[:, g * 4:(g + 1) * 4], in0=_v3(po[g][:, 0:260], 4, 65)[:, :, 64],
                                                in1=expsink[:, l * 8 + g * 4: l * 8 + (g + 1) * 4], op=ALU.add),
                      R=[po[g], expsink], W=[den])
                V(lambda e: e.reciprocal(out=den[:, 0:8], in_=den[:, 0:8]), R=[den], W=[den])
                for g in range(2):
                    V(lambda e: e.tensor_tensor(out=_v3(yA[:, g * 256:(g + 1) * 256], 4, 64),
                                                in0=_v3(po[g][:, 0:260], 4, 65)[:, :, 0:64],
                                                in1=_bc_last(den[:, g * 4:(g + 1) * 4], 64), op=ALU.mult),
                      R=[po[g], den], W=[yA])
                ya = y_a[t % 2]
                V(lambda e: e.tensor_tensor(out=ya[:, :], in0=yA[:, :], in1=sgA[:, t * 512:(t + 1) * 512], op=ALU.mult),
                  R=[yA, sgA], W=[ya])
                yield
                pT2, pT2b = transposes_to(None, ya, 4, ya)
                yield
                A(lambda e: e.activation(out=_v3(yT[0][:, :], 4, UNIT)[:, :, t * 128:(t + 1) * 128],
                                         in_=_v3(pT2b[:, 0:512], 4, 128), func=AF.Copy), R=[pT2], W=[yT[0]])

            run_pipelined(stagger(tile, TPU))
            w_release(jq)
            w_release(jk)

        def stage_gla(u, l):
            arena.reset()
            first_unit = (u % upseq == 0)
            sgB = arena.alloc("sgB", TPU * 512, BF16)
            D2 = lambda name, n, dt=F32: [arena.alloc("%s%d" % (name, i), n, dt) for i in range(2)]
            e1s, sps, Eqs, Eks, Ees, decs = D2("e1", 256), D2("sp", 256), D2("Eq", 256), D2("Ek", 256), D2("Ee", 256), D2("dec", 2)
            qds, kis, kes = D2("qd", 256, BF16), D2("ki", 256, BF16), D2("ke", 256, BF16)
            vbfs, qkTs, kzs, ATs = D2("vbf", 512, BF16), D2("qkT", 256, BF16), D2("kz", 512, BF16), D2("AT", 512, BF16)
            sqos, ssbs, t1s, ybs = D2("sqo", 512), D2("ssb", 4), D2("t1", 512), D2("yb", 512, BF16)
            gdTs = D2("gdT", 128, BF16)
            S, Sb = S_gla[l], S_gla_bf[l]
            jg, wg = w_next("B_g")
            wgf = wv(wg, 512)
            for t in range(TPU):
                p = ps()
                proj_tok(p, 512, wgf, t, wt=wg)
                A(lambda e: e.activation(out=sgB[:, t * 512:(t + 1) * 512], in_=p[:, :], func=AF.Silu), R=[p], W=[sgB])
            w_release(jg)
            jqk, wqk = w_next("B_qk")
            jv, wvv = w_next("B_v")
            jd, wd = w_next("B_d")
            wqkf, wvf = wv(wqk, 512), wv(wvv, 512)
            for kz_ in kzs:
                V(lambda e: e.memset(kz_[:, :], 0.0), W=[kz_])
            for g_ in gdTs:
                V(lambda e: e.memset(g_[0:32, :], 1.0), W=[g_])
            if first_unit:
                V(lambda e: e.memset(S[:, :], 0.0), W=[S])
                V(lambda e: e.memset(Sb[:, :], 0.0), W=[Sb])

            def tile(t):
                first = first_unit and t == 0
                i2 = t % 2
                e1, sp, Eq, Ek, Ee, dec = e1s[i2], sps[i2], Eqs[i2], Eks[i2], Ees[i2], decs[i2]
                qd, ki, ke, vbf, qkT, kz, AT = qds[i2], kis[i2], kes[i2], vbfs[i2], qkTs[i2], kzs[i2], ATs[i2]
                sqo, ssb, t1, yb, gdTt = sqos[i2], ssbs[i2], t1s[i2], ybs[i2], gdTs[i2]
                pqk, pv, pl = ps(), ps(), ps()
                proj_tok(pqk, 512, wqkf, t, wt=wqk)
                proj_tok(pv, 512, wvf, t, wt=wvv)
                for k in range(KC):
                    M(lambda e: e.matmul(pl[0:16, 256:384], lhsT=wd[:, k * 16:(k + 1) * 16], rhs=hv(k, t * 128, 128),
                                         start=(k == 0), stop=(k == KC - 1)), R=[wd, hT_b[t // 4]], W=[pl])
                yield
                A(lambda e: e.activation(out=gdTt[0:16, :], in_=pl[0:16, 256:384], func=AF.Copy), R=[pl], W=[gdTt])
                A(lambda e: e.activation(out=vbf[:, :], in_=pv[:, :], func=AF.Copy), R=[pv], W=[vbf])
                yield
                M(lambda e: e.matmul(pl[:, 0:256], lhsT=gdTt[0:17, :], rhs=wup_bf[0:17, l * 256:(l + 1) * 256],
                                     start=True, stop=True), R=[gdTt, wup_bf], W=[pl])
                yield
                A(lambda e: e.activation(out=e1[:, :], in_=pl[:, 0:256], func=AF.Exp, scale=-1.0), R=[pl], W=[e1])
                A(lambda e: e.activation(out=sp[:, :], in_=e1[:, :], func=AF.Ln, bias=1.0), R=[e1], W=[sp])
                yield
                pc = ps()
                M(lambda e: e.matmul(pc[:, 0:256], lhsT=tri_f[:, :], rhs=sp[:, :], start=True, stop=True),
                  R=[tri_f, sp], W=[pc])
                M(lambda e: e.matmul(pc[:, 256:512], lhsT=nsg_f[:, :], rhs=sp[:, :], start=True, stop=True),
                  R=[nsg_f, sp], W=[pc])
                for m in range(2):
                    M(lambda e: e.matmul(pl[:, 384 + m:385 + m], lhsT=sp[:, m * 128:(m + 1) * 128], rhs=ones_f[:, 0:1],
                                         start=True, stop=True), R=[sp, ones_f], W=[pl])
                yield
                A(lambda e: e.activation(out=Eq[:, :], in_=pc[:, 0:256], func=AF.Exp, scale=-1.0 / 16), R=[pc], W=[Eq])
                A(lambda e: e.activation(out=Ek[:, :], in_=pc[:, 0:256], func=AF.Exp, scale=1.0 / 16), R=[pc], W=[Ek])
                yield
                A(lambda e: e.activation(out=Ee[:, :], in_=pc[:, 256:512], func=AF.Exp, scale=1.0 / 16), R=[pc], W=[Ee])
                A(lambda e: e.activation(out=dec[:, 0:2], in_=pl[:, 384:386], func=AF.Exp, scale=-1.0 / 16), R=[pl], W=[dec])
                yield
                V(lambda e: e.scalar_tensor_tensor(out=qd[:, :], in0=pqk[:, 0:256], scalar=0.125, in1=Eq[:, :],
                                                   op0=ALU.mult, op1=ALU.mult), R=[pqk, Eq], W=[qd])
                V(lambda e: e.tensor_tensor(out=ki[:, :], in0=pqk[:, 256:512], in1=Ek[:, :], op=ALU.mult),
                  R=[pqk, Ek], W=[ki])
                yield
                V(lambda e: e.tensor_tensor(out=ke[:, :], in0=pqk[:, 256:512], in1=Ee[:, :], op=ALU.mult),
                  R=[pqk, Ee], W=[ke])
                pT = ps()
                pTb = pT[:, :].bitcast(BF16)
                for c in range(2):
                    M(lambda e: e.transpose(out=pTb[:, c * 128:(c + 1) * 128], in_=qd[:, c * 128:(c + 1) * 128],
                                            identity=ident_bf[:, :]), R=[qd, ident_bf], W=[pT])
                for c in range(2):
                    M(lambda e: e.transpose(out=pTb[:, (2 + c) * 128:(3 + c) * 128], in_=ki[:, c * 128:(c + 1) * 128],
                                            identity=ident_bf[:, :]), R=[ki, ident_bf], W=[pT])
                yield ("set", ("mid", t))
                A(lambda e: e.activation(out=qkT[:, :], in_=pTb[:, 0:256], func=AF.Copy), R=[pT], W=[qkT])
                for r in range(2):
                    A(lambda e: e.activation(
                        out=kz[r * 64:(r + 1) * 64, :].rearrange("p (m r v) -> p m r v", m=2, r=2, v=128)[:, :, r, :],
                        in_=_v3(pTb[r * 64:(r + 1) * 64, 256:512], 2, 128), func=AF.Copy), R=[pT], W=[kz])
                yield
                pA = ps()
                for h in range(4):
                    m, r = h // 2, h % 2
                    M(lambda e: e.matmul(pA[:, h * 128:(h + 1) * 128], lhsT=kz[:, h * 128:(h + 1) * 128],
                                         rhs=qkT[:, m * 128:(m + 1) * 128], start=True, stop=True),
                      R=[qkT, kz], W=[pA])
                pU = ps()
                for h in range(4):
                    m = h // 2
                    M(lambda e: e.matmul(pU[:, h * 128:(h + 1) * 128], lhsT=ke[:, m * 128:(m + 1) * 128],
                                         rhs=vbf[:, h * 128:(h + 1) * 128], start=True, stop=True), R=[ke, vbf], W=[pU])
                yield
                V(lambda e: e.tensor_tensor(out=_v3(AT[:, :], 4, 128), in0=_v3(pA[:, :], 4, 128),
                                            in1=_bc_mid(tri_bf[:, :], 4), op=ALU.mult), R=[pA, tri_bf], W=[AT])
                yield
                if t > 0:
                    yield ("need", ("S", t - 1))
                po = ps()
                for h in range(4):
                    m, r = h // 2, h % 2
                    M(lambda e: e.matmul(po[:, h * 128:(h + 1) * 128], lhsT=AT[:, h * 128:(h + 1) * 128],
                                         rhs=vbf[:, h * 128:(h + 1) * 128], start=True, stop=first), R=[AT, vbf], W=[po])
                    if not first:
                        M(lambda e: e.matmul(po[:, h * 128:(h + 1) * 128], lhsT=qkT[:, m * 128:(m + 1) * 128],
                                             rhs=Sb[:, h * 128:(h + 1) * 128], start=False, stop=True),
                          R=[qkT, Sb], W=[po])
                yield
                for h in range(4):
                    m, r = h // 2, h % 2
                    V(lambda e: e.scalar_tensor_tensor(out=S[r * 64:(r + 1) * 64, h * 128:(h + 1) * 128],
                                                       in0=S[r * 64:(r + 1) * 64, h * 128:(h + 1) * 128],
                                                       scalar=dec[r * 64:(r + 1) * 64, m:m + 1],
                                                       in1=pU[r * 64:(r + 1) * 64, h * 128:(h + 1) * 128],
                                                       op0=ALU.mult, op1=ALU.add), R=[S, dec, pU], W=[S])
                yield
                A(lambda e: e.activation(out=Sb[:, :], in_=S[:, :], func=AF.Copy), R=[S], W=[Sb])
                yield ("set", ("S", t))
                A(lambda e: e.activation(out=sqo[:, :], in_=po[:, :], func=AF.Square), R=[po], W=[sqo])
                yield
                V(lambda e: e.reduce_sum(out=ssb[:, 0:4], in_=_v3(sqo[:, :], 4, 128), axis=AX.X), R=[sqo], W=[ssb])
                yield
                A(lambda e: e.activation(out=ssb[:, 0:4], in_=ssb[:, 0:4], func=AF.Ln, scale=1.0 / 128, bias=EPS),
                  R=[ssb], W=[ssb])
                A(lambda e: e.activation(out=ssb[:, 0:4], in_=ssb[:, 0:4], func=AF.Exp, scale=-0.5), R=[ssb], W=[ssb])
                yield
                V(lambda e: e.tensor_tensor(out=_v3(t1[:, :], 4, 128), in0=_v3(po[:, :], 4, 128),
                                            in1=_bc_last(ssb[:, 0:4], 128), op=ALU.mult), R=[po, ssb], W=[t1])
                yield
                V(lambda e: e.tensor_tensor(out=_v3(t1[:, :], 4, 128), in0=_v3(t1[:, :], 4, 128),
                                            in1=_bc_mid(gainB[:, l * 128:(l + 1) * 128], 4), op=ALU.mult),
                  R=[t1, gainB], W=[t1])
                V(lambda e: e.tensor_tensor(out=yb[:, :], in0=t1[:, :], in1=sgB[:, t * 512:(t + 1) * 512], op=ALU.mult),
                  R=[t1, sgB], W=[yb])
                yield
                pT2, pT2b = transposes_to(None, yb, 4, yb)
                yield
                A(lambda e: e.activation(out=_v3(yT[1][:, :], 4, UNIT)[:, :, t * 128:(t + 1) * 128],
                                         in_=_v3(pT2b[:, 0:512], 4, 128), func=AF.Copy), R=[pT2], W=[yT[1]])

            run_pipelined(stagger(tile, TPU))
            w_release(jqk)
            w_release(jv)
            w_release(jd)

        def stage_ssd(u, l):
            arena.reset()
            first_unit = (u % upseq == 0)
            sgC = arena.alloc("sgC", TPU * 512, BF16)
            xbcT = arena.alloc("xbcT", 8 * UNIT, BF16)
            mark = arena.off
            raws = [arena.alloc("raw%d" % i, 516) for i in range(3)]
            accs = [arena.alloc("acc%d" % i, 512) for i in range(3)]
            S, Sb = S_ssd[l], S_ssd_bf[l]
            ct = ctail[l]
            jz, wz = w_next("C_z")
            wzf = wv(wz, 512)
            for t in range(TPU):
                p = ps()
                proj_tok(p, 512, wzf, t, wt=wz)
                A(lambda e: e.activation(out=sgC[:, t * 512:(t + 1) * 512], in_=p[:, :], func=AF.Silu), R=[p], W=[sgC])
            w_release(jz)
            jx, wx = w_next("C_x")
            jbc, wbc = w_next("C_bc")
            if first_unit:
                V(lambda e: e.memset(ct[:, :], 0.0), W=[ct])

            def conv(i, b, ch):
                wt = wx if ch < 4 else wbc
                c0 = (ch % 4) * 128
                p = ps()
                for k in range(KC):
                    M(lambda e: e.matmul(p[:, :], lhsT=wt[:, k * 512 + c0: k * 512 + c0 + 128], rhs=hv(k, b * 512, 512),
                                         start=(k == 0), stop=(k == KC - 1)), R=[wt, hT_b[b]], W=[p])
                yield
                rw, ac = raws[i % 3], accs[i % 3]
                ci = l * 32 + ch * 4
                A(lambda e: e.activation(out=rw[:, 3:515], in_=p[:, :], func=AF.Copy), R=[p], W=[rw])
                A(lambda e: e.activation(out=ac[:, :], in_=p[:, :], func=AF.Identity, scale=cw[:, ci + 3:ci + 4],
                                         bias=cb[:, l * 8 + ch:l * 8 + ch + 1]), R=[p, cw, cb], W=[ac])
                yield
                if b > 0:
                    yield ("need", ("ct", b - 1, ch))
                V(lambda e: e.tensor_copy(out=rw[:, 0:3], in_=ct[:, ch * 3:(ch + 1) * 3]), R=[ct], W=[rw])
                yield
                for kk in range(3):
                    V(lambda e: e.scalar_tensor_tensor(out=ac[:, :], in0=rw[:, kk:kk + 512], scalar=cw[:, ci + kk:ci + kk + 1],
                                                       in1=ac[:, :], op0=ALU.mult, op1=ALU.add), R=[rw, cw, ac], W=[ac])
                    yield
                V(lambda e: e.tensor_copy(out=ct[:, ch * 3:(ch + 1) * 3], in_=rw[:, 512:515]), R=[rw], W=[ct])
                yield ("set", ("ct", b, ch))
                A(lambda e: e.activation(out=xbcT[:, ch * UNIT + b * 512: ch * UNIT + (b + 1) * 512], in_=ac[:, :],
                                         func=AF.Silu), R=[ac], W=[xbcT])

            run_pipelined([conv(b * 8 + ch, b, ch) for b in range(BPU) for ch in range(8)], depth=3)
            w_release(jx)
            w_release(jbc)
            jdt, wdt = w_next("C_dt")
            arena.rewind(mark, raws + accs)
            D2 = lambda name, n, dt=F32: [arena.alloc("%s%d" % (name, i), n, dt) for i in range(2)]
            x1s, dtts, aas, sms, sscs = D2("x1", 8), D2("dtt", 8), D2("aa", 8), D2("sm", 24), D2("ssc", 2)
            Rms, xBs, xdts, xdds = D2("Rm", 1024), D2("xB", 768, BF16), D2("xdt", 512, BF16), D2("xdd", 512, BF16)
            ycs = D2("yc", 512, BF16)
            Lm = arena.alloc("Lm", 1024)
            CBm = arena.alloc("CBm", 256)
            Wm = arena.alloc("Wm", 1024, BF16)
            if first_unit:
                V(lambda e: e.memset(S[:, :], 0.0), W=[S])
                V(lambda e: e.memset(Sb[:, :], 0.0), W=[Sb])

            def xc(c, t):
                return xbcT[:, c * UNIT + t * 128: c * UNIT + (t + 1) * 128]

            def tile(t):
                first = first_unit and t == 0
                i2 = t % 2
                x1, dtt, aa, sm, ssc = x1s[i2], dtts[i2], aas[i2], sms[i2], sscs[i2]
                Rm, xB, xdt, xdd, yc = Rms[i2], xBs[i2], xdts[i2], xdds[i2], ycs[i2]
                y1 = T(Rm[:, 0:512], Rm.b)
                y2 = T(Rm[:, 512:1024], Rm.b)
                bank = lambda j: psum[4 * i2 + j]
                pd = bank(0)
                for k in range(KC):
                    M(lambda e: e.matmul(pd[:, 0:8], lhsT=hv(k, t * 128, 128), rhs=wdt[:, k * 8:(k + 1) * 8],
                                         start=(k == 0), stop=(k == KC - 1)), R=[hT_b[t // 4], wdt], W=[pd])
                yield
                V(lambda e: e.tensor_tensor(out=x1[:, 0:8], in0=pd[:, 0:8], in1=dtb[:, l * 8:(l + 1) * 8], op=ALU.add),
                  R=[pd, dtb], W=[x1])
                yield
                A(lambda e: e.activation(out=x1[:, 0:8], in_=x1[:, 0:8], func=AF.Exp), R=[x1], W=[x1])
                A(lambda e: e.activation(out=dtt[:, 0:8], in_=x1[:, 0:8], func=AF.Ln, bias=1.0), R=[x1], W=[dtt])
                yield
                V(lambda e: e.tensor_tensor(out=aa[:, 0:8], in0=dtt[:, 0:8], in1=Abc[:, l * 8:(l + 1) * 8], op=ALU.mult),
                  R=[dtt, Abc], W=[aa])
                V(lambda e: e.tensor_tensor(out=_v3(Rm[:, :], 8, 128), in0=_bc_mid(tri_f[:, :], 8),
                                            in1=_bc_last(aa[:, 0:8], 128), op=ALU.mult), R=[tri_f, aa], W=[Rm])
                yield
                pseg = [bank(1), bank(2)]
                for hf in range(2):
                    M(lambda e: e.matmul(pseg[hf][:, :], lhsT=sg_f[:, :], rhs=Rm[:, hf * 512:(hf + 1) * 512],
                                         start=True, stop=True), R=[sg_f, Rm], W=[pseg[hf]])
                M(lambda e: e.matmul(pd[:, 8:16], lhsT=tri_f[:, :], rhs=aa[:, 0:8], start=True, stop=True),
                  R=[tri_f, aa], W=[pd])
                M(lambda e: e.matmul(pd[:, 16:24], lhsT=sg_f[:, :], rhs=aa[:, 0:8], start=True, stop=True),
                  R=[sg_f, aa], W=[pd])
                M(lambda e: e.matmul(pd[:, 24:32], lhsT=ones_f[:, :], rhs=aa[:, 0:8], start=True, stop=True),
                  R=[ones_f, aa], W=[pd])
                pT = bank(3)
                pTb = pT[:, :].bitcast(BF16)
                for c in range(6):
                    M(lambda e: e.transpose(out=pTb[:, c * 128:(c + 1) * 128], in_=xc(c, t), identity=ident_bf[:, :]),
                      R=[xbcT, ident_bf], W=[pT])
                yield
                if t > 0:
                    yield ("need", ("LmF", t - 1))
                for hf in range(2):
                    A(lambda e: e.activation(out=Lm[:, hf * 512:(hf + 1) * 512], in_=pseg[hf][:, :], func=AF.Exp),
                      R=[pseg[hf]], W=[Lm])
                    yield
                A(lambda e: e.activation(out=sm[:, 0:24], in_=pd[:, 8:32], func=AF.Exp), R=[pd], W=[sm])
                A(lambda e: e.activation(out=xB[:, :], in_=pTb[:, 0:768], func=AF.Copy), R=[pT], W=[xB])
                pcb = bank(1)
                for g in range(2):
                    M(lambda e: e.matmul(pcb[:, g * 128:(g + 1) * 128], lhsT=xc(4 + g, t), rhs=xc(6 + g, t),
                                         start=True, stop=True), R=[xbcT], W=[pcb])
                yield ("set", ("mid", t))
                V(lambda e: e.tensor_tensor(out=_v3(CBm[:, :], 2, 128), in0=_v3(pcb[:, 0:256], 2, 128),
                                            in1=_bc_mid(tri_bf[:, :], 2), op=ALU.mult), R=[pcb, tri_bf], W=[CBm])
                yield
                if t > 0:
                    yield ("need", ("WmF", t - 1))
                V(lambda e: e.tensor_tensor(
                    out=Wm[:, :].rearrange("p (g a l) -> p g a l", g=2, a=4, l=128),
                    in0=Lm[:, :].rearrange("p (g a l) -> p g a l", g=2, a=4, l=128),
                    in1=_v3(CBm[:, :], 2, 128).unsqueeze(2).broadcast_to([128, 2, 4, 128]), op=ALU.mult),
                  R=[Lm, CBm], W=[Wm])
                yield ("set", ("LmF", t))
                V(lambda e: e.tensor_tensor(out=_v3(xdt[:, :], 8, 64), in0=_v3(xB[:, 0:512], 8, 64),
                                            in1=_bc_last(dtt[:, 0:8], 64), op=ALU.mult), R=[xB, dtt], W=[xdt])
                V(lambda e: e.tensor_tensor(out=_v3(xdd[:, :], 8, 64), in0=_v3(xdt[:, :], 8, 64),
                                            in1=_bc_last(sm[:, 8:16], 64), op=ALU.mult), R=[xdt, sm], W=[xdd])
                yield
                py = bank(2)
                for h in range(8):
                    M(lambda e: e.matmul(py[:, h * 64:(h + 1) * 64], lhsT=Wm[:, h * 128:(h + 1) * 128],
                                         rhs=xdt[:, h * 64:(h + 1) * 64], start=True, stop=True), R=[Wm, xdt], W=[py])
                yield ("set", ("WmF", t))
                pu = bank(3)
                for g in range(2):
                    M(lambda e: e.matmul(pu[:, g * 256:(g + 1) * 256], lhsT=xB[:, 512 + g * 128:512 + (g + 1) * 128],
                                         rhs=xdd[:, g * 256:(g + 1) * 256], start=True, stop=True), R=[xB, xdd], W=[pu])
                yield
                V(lambda e: e.tensor_tensor(out=_v3(y2[:, :], 8, 64), in0=_v3(xB[:, 0:512], 8, 64),
                                            in1=_bc_last(Dbc[:, l * 8:(l + 1) * 8], 64), op=ALU.mult), R=[xB, Dbc], W=[y2])
                yield
                if t > 0:
                    yield ("need", ("S", t - 1))
                if not first:
                    pyo = bank(0)
                    for g in range(2):
                        M(lambda e: e.matmul(pyo[:, g * 256:(g + 1) * 256], lhsT=xc(6 + g, t), rhs=Sb[:, g * 256:(g + 1) * 256],
                                             start=True, stop=True), R=[xbcT, Sb], W=[pyo])
                    yield
                V(lambda e: e.tensor_tensor(out=_v3(S[:, :], 8, 64), in0=_v3(S[:, :], 8, 64),
                                            in1=_bc_last(sm[:, 16:24], 64), op=ALU.mult), R=[S, sm], W=[S])
                V(lambda e: e.tensor_tensor(out=S[:, :], in0=S[:, :], in1=pu[:, :], op=ALU.add), R=[S, pu], W=[S])
                yield
                A(lambda e: e.activation(out=Sb[:, :], in_=S[:, :], func=AF.Copy), R=[S], W=[Sb])
                yield ("set", ("S", t))
                if not first:
                    V(lambda e: e.tensor_tensor(out=_v3(y1[:, :], 8, 64), in0=_v3(pyo[:, :], 8, 64),
                                                in1=_bc_last(sm[:, 0:8], 64), op=ALU.mult), R=[pyo, sm], W=[y1])
                    yield
                    V(lambda e: e.tensor_tensor(out=y1[:, :], in0=y1[:, :], in1=py[:, :], op=ALU.add), R=[y1, py], W=[y1])
                    yield
                    V(lambda e: e.tensor_tensor(out=y1[:, :], in0=y1[:, :], in1=y2[:, :], op=ALU.add), R=[y1, y2], W=[y1])
                else:
                    V(lambda e: e.tensor_tensor(out=y1[:, :], in0=y2[:, :], in1=py[:, :], op=ALU.add), R=[y2, py], W=[y1])
                yield
                V(lambda e: e.tensor_tensor(out=y1[:, :], in0=y1[:, :], in1=sgC[:, t * 512:(t + 1) * 512], op=ALU.mult),
                  R=[y1, sgC], W=[y1])
                yield
                A(lambda e: e.activation(out=y2[:, :], in_=y1[:, :], func=AF.Square), R=[y1], W=[y2])
                yield
                V(lambda e: e.reduce_sum(out=ssc[:, 0:2], in_=_v3(y2[:, :], 2, 256), axis=AX.X), R=[y2], W=[ssc])
                yield
                A(lambda e: e.activation(out=ssc[:, 0:2], in_=ssc[:, 0:2], func=AF.Ln, scale=1.0 / 256, bias=EPS),
                  R=[ssc], W=[ssc])
                A(lambda e: e.activation(out=ssc[:, 0:2], in_=ssc[:, 0:2], func=AF.Exp, scale=-0.5), R=[ssc], W=[ssc])
                yield
                V(lambda e: e.tensor_tensor(out=_v3(y1[:, :], 2, 256), in0=_v3(y1[:, :], 2, 256),
                                            in1=_bc_last(ssc[:, 0:2], 256), op=ALU.mult), R=[y1, ssc], W=[y1])
                yield
                V(lambda e: e.tensor_tensor(out=yc[:, :], in0=y1[:, :], in1=gainC[:, l * 512:(l + 1) * 512], op=ALU.mult),
                  R=[y1, gainC], W=[yc])
                yield
                pT2, pT2b = transposes_to(None, yc, 4, yc, p=bank(1))
                yield
                A(lambda e: e.activation(out=_v3(yT[2][:, :], 4, UNIT)[:, :, t * 128:(t + 1) * 128],
                                         in_=_v3(pT2b[:, 0:512], 4, 128), func=AF.Copy), R=[pT2], W=[yT[2]])

            run_pipelined(stagger(tile, TPU))
            w_release(jdt)

        def stage_merge(u, l):
            arena.reset()
            tgs = [[arena.alloc("tg%d_%d" % (i, j), 512, BF16) for j in range(3)] for i in range(2)]
            mms = [[arena.alloc("mm%d_%d" % (i, j), 512) for j in range(3)] for i in range(2)]
            mT = arena.alloc("mT", KC * UNIT, BF16)
            mT_b = [mT.b, mT.b]
            cnt = 0
            for g4 in range(2):
                jj, wb, wm = [], [], []
                for br in range(3):
                    j, w = w_next("M_br%d_%d" % (br, g4))
                    jj.append(j)
                    wb.append(w)
                    j, w = w_next("M_mg%d_%d" % (br, g4))
                    jj.append(j)
                    wm.append(w)
                for dcl in range(4):
                    dc = g4 * 4 + dcl
                    for b in range(BPU):
                        par = cnt % 2
                        cnt += 1
                        for br in range(3):
                            pu_, pg = ps(), ps()
                            for k in range(4):
                                M(lambda e: e.matmul(pu_[:, :], lhsT=wb[br][:, k * 512 + dcl * 128: k * 512 + (dcl + 1) * 128],
                                                     rhs=yT[br][:, k * UNIT + b * 512: k * UNIT + (b + 1) * 512],
                                                     start=(k == 0), stop=(k == 3)), R=[wb[br], yT[br]], W=[pu_])
                            for k in range(KC):
                                M(lambda e: e.matmul(pg[:, :], lhsT=wm[br][:, k * 512 + dcl * 128: k * 512 + (dcl + 1) * 128],
                                                     rhs=hv(k, b * 512, 512), start=(k == 0), stop=(k == KC - 1)),
                                  R=[wm[br], hT_b[b]], W=[pg])
                            tg, mm = tgs[par][br], mms[par][br]
                            A(lambda e: e.activation(out=tg[:, :], in_=pg[:, :], func=AF.Tanh, scale=0.5), R=[pg], W=[tg])
                            V(lambda e: e.scalar_tensor_tensor(out=mm[:, :], in0=tg[:, :], scalar=1.0, in1=pu_[:, :],
                                                               op0=ALU.add, op1=ALU.mult), R=[tg, pu_], W=[mm])
                        m0, m1, m2 = mms[par]
                        V(lambda e: e.tensor_tensor(out=m0[:, :], in0=m0[:, :], in1=m1[:, :], op=ALU.add), R=[m0, m1], W=[m0])
                        V(lambda e: e.tensor_tensor(out=mT[:, dc * UNIT + b * 512: dc * UNIT + (b + 1) * 512], in0=m0[:, :],
                                                    in1=m2[:, :], op=ALU.add), R=[m0, m2], W=[mT_b[b]])
                for j in jj:
                    w_release(j)
            jo0, wo0 = w_next("O_0")
            jo1, wo1 = w_next("O_1")
            for b in range(BPU):
                for oc in range(KC):
                    wo, ocl = (wo0, wo1)[oc // 4], oc % 4
                    p = ps()
                    for k in range(KC):
                        M(lambda e: e.matmul(p[:, :], lhsT=wo[:, k * 512 + ocl * 128: k * 512 + (ocl + 1) * 128],
                                             rhs=mT[:, k * UNIT + b * 512: k * UNIT + (b + 1) * 512],
                                             start=(k == 0), stop=(k == KC - 1)), R=[wo, mT_b[b]], W=[p])
                    V(lambda e: e.scalar_tensor_tensor(out=xv(oc, b), in0=p[:, :], scalar=0.5, in1=xv(oc, b),
                                                       op0=ALU.mult, op1=ALU.add), R=[p, xT_b[oc][b]], W=[xT_b[oc][b]])
                    if l == depth - 1 and dbg is None:
                        store_x(u, oc, b)
                if l == depth - 1 and dbg is None and u + 1 < n_units:
                    load_x_block(u + 1, b)
            w_release(jo0)
            w_release(jo1)

        for u in range(n_units):
            if u == 0 or dbg is not None:
                load_x(u)
            rope_tables(u)
            for l in range(depth):
                stage_norm(l)
                stage_attn(u, l)
                if dbg == "attn":
                    break
                stage_gla(u, l)
                if dbg == "gla":
                    break
                stage_ssd(u, l)
                if dbg == "ssd":
                    break
                stage_merge(u, l)
                if dbg == "layer":
                    break
            if dbg is not None:
                break

        if dbg is not None:
            def dump(name, t, shape, dt=F32):
                o = nc.dram_tensor(name, shape, dt, kind="ExternalOutput").ap()
                C.dma("sp", o, t, s_dbg, dbg_cnt, R=[hT_b[0], hT_b[1], yT[0], yT[1], yT[2], cs2, sn2])
            dump("d_hT", hT[:, :], [128, KC * UNIT], BF16)
            dump("d_yT0", yT[0][:, :], [128, 4 * UNIT], BF16)
            dump("d_yT1", yT[1][:, :], [128, 4 * UNIT], BF16)
            dump("d_yT2", yT[2][:, :], [128, 4 * UNIT], BF16)
            dump("d_cs2", cs2[:, :], [128, TPU * 64])
            dump("d_sn2", sn2[:, :], [128, TPU * 64])
            for k in range(KC):
                for b in range(BPU):
                    store_x(0, k, b)
        for k in range(KC):
            for b in range(BPU):
                if o_cnt[k][b][0]:
                    C.E["sp"].wait_ge(s_o[k][b], o_cnt[k][b][0])
        if dbg_cnt[0]:
            C.E["sp"].wait_ge(s_dbg, dbg_cnt[0])
        if needed is not None:
            print("instructions:", C.n_inst, "sem-incs:", sum(len(v) for v in needed.values()), "sems:", C.nsem)
    return nc, C.waited


def build(n_seq, seq_len, depth, dbg=None):
    _, waited = build1(n_seq, seq_len, depth, dbg, None)
    nc, _ = build1(n_seq, seq_len, depth, dbg, waited)
    return nc


_INPUT_ORDER = ["norm_g", "w_in", "attn_q_norm", "attn_k_norm", "attn_sinks", "gla_w_gate_up", "gla_b_gate",
                "gla_out_norm", "ssd_conv_w", "ssd_conv_b", "ssd_dt_bias", "ssd_A_log", "ssd_D", "ssd_out_norm",
                "w_branch", "w_out"]


def kernel(**inputs):
    x = np.ascontiguousarray(np.asarray(inputs["x"], dtype=np.float32))
    positions = np.asarray(inputs["positions"]).astype(np.int32)
    B, S, dm = x.shape
    depth = int(np.asarray(inputs["w_in"]).shape[0])
    assert dm == D and B % NCORES == 0
    per = B // NCORES
    nc = build(per, S, depth)
    inv_freq = (np.float32(10000.0) ** (-(np.arange(0, 64, 2, dtype=np.float32)) / np.float32(64))).astype(np.float32)
    shared = {k: np.ascontiguousarray(np.asarray(inputs[k], dtype=np.float32)) for k in _INPUT_ORDER}
    shared["inv_freq"] = inv_freq.reshape(1, 32)
    in_maps = []
    for c in range(NCORES):
        xs = x[c * per:(c + 1) * per].reshape(per * S, D)
        m = dict(shared)
        m["xT"] = np.ascontiguousarray(xs.T)
        pos = positions[c * per:(c + 1) * per].reshape(per * S // UNIT, TPU, 128)
        m["pos"] = np.ascontiguousarray(pos.transpose(0, 2, 1))
        in_maps.append(m)
    res = run_bass_kernel_spmd(nc, in_maps, core_ids=list(range(NCORES)))
    outs = [np.asarray(r["outT"]).T.reshape(per, S, D) for r in res.results]
    return np.ascontiguousarray(np.concatenate(outs, axis=0).astype(np.float32))
```

```python
import math
import os
GLA_STOP = int(os.environ.get('GLA_STOP', '99'))
STRICT = int(os.environ.get('STRICT', '0'))
GLA_VAR = int(os.environ.get('GLA_VAR', '0'))
ATT_PAD = int(os.environ.get('ATT_PAD', '1'))
PIPE = int(os.environ.get('PIPE', '1'))
STAGGER = int(os.environ.get('STAGGER', '1'))
GLA_NOINTER = int(os.environ.get('GLA_NOINTER', '0'))
from contextlib import ExitStack

import numpy as np
import concourse.bass as bass
import concourse.mybir as mybir
from concourse.bass_utils import run_bass_kernel_spmd

F32 = mybir.dt.float32
BF16 = mybir.dt.bfloat16
I32 = mybir.dt.int32
AF = mybir.ActivationFunctionType
ALU = mybir.AluOpType
AX = mybir.AxisListType

D = 1024
KC = 8
NCORES = 8
IN_DIM = 7448
EPS = 1e-6
UNIT = 1024
TPU = UNIT // 128
BPU = UNIT // 512
RING = 6
RING_ELEMS = 4096
TWO_PI = 2.0 * math.pi

C_AQ, C_AK, C_AV, C_AG = 0, 512, 640, 768
C_GQ, C_GK, C_GV, C_GG, C_GD = 1280, 1536, 1792, 2304, 2816
C_XBC, C_DT, C_Z, C_MG = 2832, 3856, 3864, 4376


class Buf:
    __slots__ = ("name", "R", "W", "psum")

    def __init__(self, name, psum=False):
        self.name = name
        self.R = {}
        self.W = {}
        self.psum = psum


class T:
    def __init__(self, t, buf=None, name=None):
        self.t = t
        self.b = buf if buf is not None else Buf(name or "t")

    def __getitem__(self, k):
        return self.t[k]


def _bufs(lst):
    out = []
    for x in lst:
        if x is None:
            continue
        out.append(x.b if isinstance(x, T) else x)
    return out


class Ctx:
    def __init__(self, nc, es, needed=None):
        self.nc = nc
        self.es = es
        self.rank = None
        if needed is not None:
            self.rank = {k: {v: i + 1 for i, v in enumerate(sorted(vs))} for k, vs in needed.items()}
        self.waited = {k: set() for k in ("pe", "act", "dve", "pool")}
        self.E = {"pe": nc.tensor, "act": nc.scalar, "dve": nc.vector, "pool": nc.gpsimd, "sp": nc.sync}
        self.sem = {k: es.enter_context(nc.semaphore("sem_" + k)) for k in ("pe", "act", "dve", "pool")}
        self.cnt = {k: 0 for k in self.sem}
        self.seen = {k: {} for k in self.E}
        self.nsem = 0
        self.epoch_bufs = []
        self.inherit = ({}, {})
        self.n_inst = 0

    def sb(self, name, shape, dt=F32):
        return T(self.es.enter_context(self.nc.sbuf_tensor(name, shape, dt)), name=name)

    def new_sem(self, name):
        self.nsem += 1
        return self.es.enter_context(self.nc.semaphore(name))

    def _wait(self, eng, deps):
        e = self.E[eng]
        seen = self.seen[eng]
        for key, (sem, val) in deps.items():
            if seen.get(key, 0) >= val:
                continue
            if key in self.waited:
                self.waited[key].add(val)
                e.wait_ge(sem, self.rank[key][val] if self.rank is not None else val)
            else:
                e.wait_ge(sem, val)
            seen[key] = val

    def _deps(self, eng, R, W):
        deps = {}

        def add(key, sv):
            if key == eng and eng == "pe":
                return
            cur = deps.get(key)
            if cur is None or cur[1] < sv[1]:
                deps[key] = sv

        for b in R:
            for k, sv in b.W.items():
                add(k, sv)
            if b.psum:
                for k, sv in b.R.items():
                    if k != eng:
                        add(k, sv)
        for b in W:
            for k, sv in b.R.items():
                if k != eng or STRICT:
                    add(k, sv)
            for k, sv in b.W.items():
                if k != eng or STRICT:
                    add(k, sv)
        return deps

    def op(self, eng, fn, R=(), W=()):
        R = _bufs(R)
        W = _bufs(W)
        self._wait(eng, self._deps(eng, R, W))
        inst = fn(self.E[eng])
        self.cnt[eng] += 1
        self.n_inst += 1
        if self.rank is None or self.cnt[eng] in self.rank[eng]:
            inst.then_inc(self.sem[eng], 1)
        sv = (self.sem[eng], self.cnt[eng])
        for b in R:
            b.R[eng] = sv
        for b in W:
            b.W[eng] = sv
        return inst

    def dma(self, q, out, in_, sem, semstate, R=(), W=(), **kw):
        R = _bufs(R)
        W = _bufs(W)
        self._wait(q, self._deps(q, R, W))
        self.E[q].dma_start(out=out, in_=in_, **kw).then_inc(sem, 16)
        semstate[0] += 16
        key = "dma:" + str(id(sem))
        sv = (sem, semstate[0])
        for b in R:
            b.R[key] = sv
        for b in W:
            b.W[key] = sv

    def new_epoch(self):
        R, W = {}, {}
        for b in self.epoch_bufs:
            for src in (b.R, b.W):
                for k, sv in src.items():
                    if k not in R or R[k][1] < sv[1]:
                        R[k] = sv
        for k, sv in self.inherit[0].items():
            if k not in R or R[k][1] < sv[1]:
                R[k] = sv
        self.inherit = (R, dict(R))
        self.epoch_bufs = []

    def scratch_buf(self, name):
        b = Buf(name)
        b.R = dict(self.inherit[0])
        b.W = dict(self.inherit[1])
        self.epoch_bufs.append(b)
        return b


class Arena:
    def __init__(self, C, name, nbytes):
        self.C = C
        self.n32 = nbytes // 4
        self.t = C.es.enter_context(C.nc.sbuf_tensor(name, [128, self.n32], F32))
        self.off = 0

    def reset(self):
        self.off = 0
        self.C.new_epoch()

    def rewind(self, mark, old):
        R = dict(self.C.inherit[0])
        for t in old:
            for src in (t.b.R, t.b.W):
                for k, sv in src.items():
                    if k not in R or R[k][1] < sv[1]:
                        R[k] = sv
        self.C.inherit = (R, dict(R))
        self.off = mark

    def alloc(self, name, nelem, dt=F32, parts=128):
        n32 = nelem if dt in (F32, I32) else (nelem + 1) // 2
        assert self.off + n32 <= self.n32, (name, self.off, n32, self.n32)
        ap = self.t[0:parts, self.off:self.off + n32]
        if dt != F32:
            ap = ap.bitcast(dt)
        self.off += n32
        return T(ap, self.C.scratch_buf(name))


def _v3(ap, a, b):
    return ap.rearrange("p (a b) -> p a b", a=a, b=b)


def _bc_last(ap2, n):
    return ap2.unsqueeze(2).broadcast_to([ap2.shape[0], ap2.shape[1], n])


def _bc_mid(ap2, n):
    return ap2.unsqueeze(1).broadcast_to([ap2.shape[0], n, ap2.shape[1]])


def build1(n_seq, seq_len, depth, dbg=None, needed=None):
    assert seq_len % UNIT == 0
    upseq = seq_len // UNIT
    n_units = n_seq * upseq
    NT = n_seq * seq_len
    nc = bass.Bass("TRN2", target_bir_lowering=False)

    def din(name, shape, dt=F32):
        return nc.dram_tensor(name, shape, dt, kind="ExternalInput")

    xT_d = din("xT", [D, NT]).ap()
    pos_d = din("pos", [n_units, 128, TPU], I32).ap()
    invf_d = din("inv_freq", [1, 32])
    norm_g_d = din("norm_g", [depth, D]).ap()
    w_in_d = din("w_in", [depth, D, IN_DIM]).ap()
    qn_d = din("attn_q_norm", [depth, 64])
    kn_d = din("attn_k_norm", [depth, 64])
    sinks_d = din("attn_sinks", [depth, 8])
    wup_d = din("gla_w_gate_up", [depth, 16, 256]).ap()
    bg_d = din("gla_b_gate", [depth, 256]).ap()
    gon_d = din("gla_out_norm", [depth, 128])
    cw_d = din("ssd_conv_w", [depth, 4, D]).ap()
    cb_d = din("ssd_conv_b", [depth, D]).ap()
    dtb_d = din("ssd_dt_bias", [depth, 8])
    alog_d = din("ssd_A_log", [depth, 8])
    dsk_d = din("ssd_D", [depth, 8])
    son_d = din("ssd_out_norm", [depth, 512])
    wbr_d = din("w_branch", [depth, 3, 512, D]).ap()
    wout_d = din("w_out", [depth, D, D]).ap()
    outT_d = nc.dram_tensor("outT", [D, NT], F32, kind="ExternalOutput").ap()
    dbg_out = {}

    with ExitStack() as es:
        C = Ctx(nc, es, needed)
        es.enter_context(nc.allow_non_contiguous_dma(reason="small parameter layouts"))

        def bcast_src(handle, row, n, reps=None):
            if reps is None:
                return bass.AP(handle, row * n, [[0, 128], [1, n]])
            return bass.AP(handle, row * n, [[0, 128], [0, reps], [1, n]])

        xT = C.sb("xTs", [128, KC * UNIT])
        xT_b = [[Buf("xT%d_%d" % (k, b)) for b in range(BPU)] for k in range(KC)]
        hT = C.sb("hT", [128, KC * UNIT], BF16)
        hT_b = [Buf("hT%d" % b) for b in range(BPU)]
        yT = [C.sb("yT%d" % i, [128, 4 * UNIT], BF16) for i in range(3)]
        ring = [C.sb("ring%d" % i, [128, RING_ELEMS], BF16) for i in range(RING)]
        ring_sem = [C.new_sem("rsem%d" % i) for i in range(RING)]
        ring_cnt = [[0] for _ in range(RING)]
        arena = Arena(C, "arena", 49 * 1024)

        def xv(k, b):
            return xT[:, k * UNIT + b * 512: k * UNIT + (b + 1) * 512]

        def hv(k, lo, n):
            return hT[:, k * UNIT + lo: k * UNIT + lo + n]

        ones_bf = C.sb("ones_bf", [128, 128], BF16)
        ident_bf = C.sb("ident_bf", [128, 128], BF16)
        tri_f = C.sb("tri_f", [128, 128])
        sg_f = C.sb("sg_f", [128, 128])
        nsg_f = C.sb("nsg_f", [128, 128])
        ones_f = C.sb("ones_f", [128, 128])
        mb_cur = C.sb("mb_cur", [128, 512], BF16)
        mb_prv = C.sb("mb_prv", [128, 512], BF16)
        tri_bf = C.sb("tri_bf", [128, 128], BF16)
        sg_bf = C.sb("sg_bf", [128, 128], BF16)
        g_col = C.sb("g_col", [128, depth * KC])
        qkgain = C.sb("qkgain", [128, depth * 640])
        expsink = C.sb("expsink", [128, depth * 8])
        wup_bf = C.sb("wup_bf", [32, depth * 256], BF16)
        gainB = C.sb("gainB", [128, depth * 128])
        cw = C.sb("cw", [128, depth * 32])
        cb = C.sb("cb", [128, depth * 8])
        dtb = C.sb("dtb", [128, depth * 8])
        Abc = C.sb("Abc", [128, depth * 8])
        Dbc = C.sb("Dbc", [128, depth * 8])
        gainC = C.sb("gainC", [128, depth * 512])
        invf = C.sb("invf", [128, 32])
        posi = C.sb("posi", [128, TPU], I32)
        cs2 = C.sb("cs2", [128, TPU * 64])
        sn2 = C.sb("sn2", [128, TPU * 64])
        kT_st = [[C.sb("kT%d_%d" % (l, i), [128, 256], BF16) for i in range(3)] for l in range(depth)]
        v_st = [[C.sb("vaug%d_%d" % (l, i), [128, 130], BF16) for i in range(3)] for l in range(depth)]
        S_gla = [C.sb("Sgla%d" % l, [128, 512]) for l in range(depth)]
        S_gla_bf = [C.sb("Sglab%d" % l, [128, 512], BF16) for l in range(depth)]
        S_ssd = [C.sb("Sssd%d" % l, [128, 512]) for l in range(depth)]
        S_ssd_bf = [C.sb("Sssdb%d" % l, [128, 512], BF16) for l in range(depth)]
        ctail = [C.sb("ctail%d" % l, [128, 24]) for l in range(depth)]

        psum = [T(es.enter_context(nc.psum_tensor("ps%d" % i, [128, 512], F32)), buf=Buf("ps%d" % i, psum=True)) for i in range(8)]
        ps_i = [0]

        def ps():
            p = psum[ps_i[0] % 8]
            ps_i[0] += 1
            return p

        identf = arena.alloc("identf", 128)
        wup_f = arena.alloc("wup_f", depth * 256)
        s_const = C.new_sem("s_const")
        cst = [0]

        def cdma(out, in_, W):
            C.dma("sp", out, in_, s_const, cst, W=[W])

        for l in range(depth):
            cdma(g_col[:, l * KC:(l + 1) * KC], norm_g_d[l].rearrange("(k p) -> p k", p=128), g_col)
            cdma(_v3(qkgain[:, l * 640: l * 640 + 512], 8, 64), bcast_src(qn_d, l, 64, 8), qkgain)
            cdma(_v3(qkgain[:, l * 640 + 512:(l + 1) * 640], 2, 64), bcast_src(kn_d, l, 64, 2), qkgain)
            cdma(expsink[:, l * 8:(l + 1) * 8], bcast_src(sinks_d, l, 8), expsink)
            cdma(wup_f[0:16, l * 256:(l + 1) * 256], wup_d[l], wup_f)
            cdma(wup_f[16:17, l * 256:(l + 1) * 256], bg_d[l:l + 1, :], wup_f)
            cdma(gainB[:, l * 128:(l + 1) * 128], bcast_src(gon_d, l, 128), gainB)
            for k in range(4):
                cdma(_v3(cw[:, l * 32:(l + 1) * 32], 8, 4)[:, :, k], cw_d[l, k].rearrange("(c p) -> p c", p=128), cw)
            cdma(cb[:, l * 8:(l + 1) * 8], cb_d[l].rearrange("(c p) -> p c", p=128), cb)
            cdma(dtb[:, l * 8:(l + 1) * 8], bcast_src(dtb_d, l, 8), dtb)
            cdma(Abc[:, l * 8:(l + 1) * 8], bcast_src(alog_d, l, 8), Abc)
            cdma(Dbc[:, l * 8:(l + 1) * 8], bcast_src(dsk_d, l, 8), Dbc)
            cdma(gainC[:, l * 512:(l + 1) * 512], bcast_src(son_d, l, 512), gainC)
        cdma(invf[:, :], bcast_src(invf_d, 0, 32), invf)
        for t_ in (g_col, qkgain, expsink, wup_f, gainB, cw, cb, dtb, Abc, Dbc, gainC, invf):
            for k_ in list(t_.b.W):
                t_.b.W[k_] = (s_const, cst[0])

        P = lambda fn, R=(), W=(): C.op("pool", fn, R, W)
        V = lambda fn, R=(), W=(): C.op("dve", fn, R, W)
        A = lambda fn, R=(), W=(): C.op("act", fn, R, W)
        M = lambda fn, R=(), W=(): C.op("pe", fn, R, W)

        P(lambda e: e.memset(ones_f[:, :], 1.0), W=[ones_f])
        P(lambda e: e.memset(tri_f[:, :], 1.0), W=[tri_f])
        P(lambda e: e.memset(sg_f[:, :], 1.0), W=[sg_f])
        P(lambda e: e.memset(identf[:, :], 0.0), W=[identf])
        P(lambda e: e.affine_select(out=tri_f[:, :], in_=tri_f[:, :], pattern=[[1, 128]], compare_op=ALU.is_ge,
                                    fill=0.0, base=0, channel_multiplier=-1), R=[tri_f], W=[tri_f])
        P(lambda e: e.affine_select(out=sg_f[:, :], in_=sg_f[:, :], pattern=[[-1, 128]], compare_op=ALU.is_gt,
                                    fill=0.0, base=0, channel_multiplier=1), R=[sg_f], W=[sg_f])
        P(lambda e: e.affine_select(out=identf[:, :], in_=identf[:, :], pattern=[[-1, 128]], compare_op=ALU.not_equal,
                                    fill=1.0, base=0, channel_multiplier=1), R=[identf], W=[identf])
        V(lambda e: e.tensor_copy(out=ones_bf[:, :], in_=ones_f[:, :]), R=[ones_f], W=[ones_bf])
        V(lambda e: e.tensor_copy(out=tri_bf[:, :], in_=tri_f[:, :]), R=[tri_f], W=[tri_bf])
        V(lambda e: e.tensor_copy(out=sg_bf[:, :], in_=sg_f[:, :]), R=[sg_f], W=[sg_bf])
        V(lambda e: e.tensor_copy(out=ident_bf[:, :], in_=identf[:, :]), R=[identf], W=[ident_bf])
        V(lambda e: e.tensor_scalar(out=nsg_f[:, :], in0=sg_f[:, :], scalar1=-1.0, scalar2=None, op0=ALU.mult),
          R=[sg_f], W=[nsg_f])
        V(lambda e: e.tensor_scalar(out=_v3(mb_cur[:, :], 4, 128), in0=_bc_mid(tri_f[:, :], 4), scalar1=-1.0, scalar2=1.0e5,
                                    op0=ALU.add, op1=ALU.mult), R=[tri_f], W=[mb_cur])
        V(lambda e: e.tensor_scalar(out=_v3(mb_prv[:, :], 4, 128), in0=_bc_mid(sg_f[:, :], 4), scalar1=-1.0, scalar2=1.0e5,
                                    op0=ALU.add, op1=ALU.mult), R=[sg_f], W=[mb_prv])
        A(lambda e: e.activation(out=expsink[:, :], in_=expsink[:, :], func=AF.Exp), R=[expsink], W=[expsink])
        A(lambda e: e.activation(out=Abc[:, :], in_=Abc[:, :], func=AF.Exp), R=[Abc], W=[Abc])
        V(lambda e: e.tensor_scalar(out=Abc[:, :], in0=Abc[:, :], scalar1=-1.0, scalar2=None, op0=ALU.mult),
          R=[Abc], W=[Abc])
        V(lambda e: e.tensor_copy(out=wup_bf[0:17, :], in_=wup_f[0:17, :]), R=[wup_f], W=[wup_bf])
        for l in range(depth):
            for i in range(3):
                V(lambda e, t=v_st[l][i]: e.memset(t[:, :], 1.0), W=[v_st[l][i]])
                V(lambda e, t=kT_st[l][i]: e.memset(t[:, :], 0.0), W=[kT_st[l][i]])

        def w_in_piece(l, c0, n):
            return w_in_d[l][:, c0:c0 + n].rearrange("(k p) c -> p k c", p=128), KC * n

        pieces = []
        for u in range(n_units):
            for l in range(depth):
                pl = [("A_qkv", [w_in_piece(l, C_AQ, 512)]), ("A_kv", [w_in_piece(l, C_AK, 256)]),
                      ("A_g", [w_in_piece(l, C_AG, 512)]),
                      ("B_g", [w_in_piece(l, C_GG, 512)]), ("B_qk", [w_in_piece(l, C_GQ, 512)]),
                      ("B_v", [w_in_piece(l, C_GV, 512)]), ("B_d", [w_in_piece(l, C_GD, 16)]),
                      ("C_z", [w_in_piece(l, C_Z, 512)]), ("C_x", [w_in_piece(l, C_XBC, 512)]),
                      ("C_bc", [w_in_piece(l, C_XBC + 512, 512)]), ("C_dt", [w_in_piece(l, C_DT, 8)])]
                for g4 in range(2):
                    for br in range(3):
                        pl.append(("M_br%d_%d" % (br, g4),
                                   [(wbr_d[l, br][:, g4 * 512:(g4 + 1) * 512].rearrange("(k p) c -> p k c", p=128), 4 * 512)]))
                        pl.append(("M_mg%d_%d" % (br, g4), [w_in_piece(l, C_MG + br * 1024 + g4 * 512, 512)]))
                for oh in range(2):
                    pl.append(("O_%d" % oh, [(wout_d[l][:, oh * 512:(oh + 1) * 512].rearrange("(k p) c -> p k c", p=128), KC * 512)]))
                pieces.extend(pl)
        wst = {"issued": 0, "released": set(), "next": 0}

        def w_pump():
            while wst["issued"] < len(pieces):
                j = wst["issued"]
                if j >= RING and (j - RING) not in wst["released"]:
                    break
                if j - wst["next"] >= RING:
                    break
                slot = j % RING
                name, parts = pieces[j]
                off = 0
                for src, n in parts:
                    kk = src.shape[1]
                    dst = ring[slot][:, off:off + n].rearrange("p (k c) -> p k c", k=kk)
                    C.dma("pool", dst, src, ring_sem[slot], ring_cnt[slot], W=[ring[slot]])
                    off += n
                wst["issued"] += 1

        def w_next(name):
            j = wst["next"]
            assert pieces[j][0] == name, (pieces[j][0], name)
            w_pump()
            assert wst["issued"] > j, "weight ring too small at piece %d %s" % (j, name)
            wst["next"] += 1
            return j, ring[j % RING]

        def w_release(j):
            wst["released"].add(j)
            w_pump()

        def wv(rt, n):
            return lambda k, c0=0, cn=None: rt[:, k * n + c0: k * n + (n if cn is None else c0 + cn)]

        s_x = [[C.new_sem("s_x%d_%d" % (k, b)) for b in range(BPU)] for k in range(KC)]
        x_cnt = [[[0] for b in range(BPU)] for k in range(KC)]
        s_o = [[C.new_sem("s_o%d_%d" % (k, b)) for b in range(BPU)] for k in range(KC)]
        o_cnt = [[[0] for b in range(BPU)] for k in range(KC)]
        s_dbg = C.new_sem("s_dbg")
        dbg_cnt = [0]
        s_pos = C.new_sem("s_pos")
        pos_cnt = [0]

        def load_x_block(u, b):
            t0 = u * UNIT
            for k in range(KC):
                C.dma("sp", xv(k, b), xT_d[k * 128:(k + 1) * 128, t0 + b * 512: t0 + (b + 1) * 512], s_x[k][b], x_cnt[k][b],
                      W=[xT_b[k][b]])

        def load_x(u):
            for b in range(BPU):
                load_x_block(u, b)

        def store_x(u, k, b):
            t0 = u * UNIT
            C.dma("sp", outT_d[k * 128:(k + 1) * 128, t0 + b * 512: t0 + (b + 1) * 512], xv(k, b), s_o[k][b], o_cnt[k][b],
                  R=[xT_b[k][b]])

        def rope_tables(u):
            arena.reset()
            s_p = None
            C.dma("sp", posi[:, :], pos_d[u], s_pos, pos_cnt, W=[posi])
            posf = arena.alloc("posf", TPU)
            ang = arena.alloc("ang", TPU * 32)
            kf = arena.alloc("kf", TPU * 32)
            ki = arena.alloc("ki", TPU * 32, I32)
            V(lambda e: e.tensor_copy(out=posf[:, :], in_=posi[:, :]), R=[posi], W=[posf])
            V(lambda e: e.tensor_tensor(out=_v3(ang[:, :], TPU, 32), in0=_bc_last(posf[:, :], 32),
                                        in1=_bc_mid(invf[:, :], TPU), op=ALU.mult), R=[posf, invf], W=[ang])
            for shift, half in ((0.0, "sin"), (0.5 * math.pi, "cos")):
                V(lambda e: e.tensor_scalar(out=kf[:, :], in0=ang[:, :], scalar1=shift, scalar2=1.0 / TWO_PI,
                                            op0=ALU.add, op1=ALU.mult), R=[ang], W=[kf])
                V(lambda e: e.tensor_copy(out=ki[:, :], in_=kf[:, :]), R=[kf], W=[ki])
                V(lambda e: e.tensor_copy(out=kf[:, :], in_=ki[:, :]), R=[ki], W=[kf])
                V(lambda e: e.scalar_tensor_tensor(out=kf[:, :], in0=kf[:, :], scalar=-TWO_PI, in1=ang[:, :],
                                                   op0=ALU.mult, op1=ALU.add), R=[kf, ang], W=[kf])
                if half == "sin":
                    A(lambda e: e.activation(out=_v3(sn2[:, :], TPU, 64)[:, :, 32:64], in_=_v3(kf[:, :], TPU, 32),
                                             func=AF.Sin, bias=0.0, scale=1.0), R=[kf], W=[sn2])
                    V(lambda e: e.tensor_scalar(out=_v3(sn2[:, :], TPU, 64)[:, :, 0:32],
                                                in0=_v3(sn2[:, :], TPU, 64)[:, :, 32:64], scalar1=-1.0, scalar2=None,
                                                op0=ALU.mult), R=[sn2], W=[sn2])
                else:
                    A(lambda e: e.activation(out=_v3(cs2[:, :], TPU, 64)[:, :, 0:32], in_=_v3(kf[:, :], TPU, 32),
                                             func=AF.Sin, bias=shift, scale=1.0), R=[kf], W=[cs2])
                    V(lambda e: e.tensor_copy(out=_v3(cs2[:, :], TPU, 64)[:, :, 32:64],
                                              in_=_v3(cs2[:, :], TPU, 64)[:, :, 0:32]), R=[cs2], W=[cs2])

        def stage_norm(l):
            arena.reset()
            sq = [arena.alloc("sq%d" % i, KC * 512, BF16) for i in range(2)]
            lnv = arena.alloc("lnv", 512)
            rstd = [arena.alloc("rstd%d" % i, 512) for i in range(2)]
            for b in range(BPU):
                s = sq[b % 2]
                for k in range(KC):
                    A(lambda e: e.activation(out=s[:, k * 512:(k + 1) * 512], in_=xv(k, b), func=AF.Square),
                      R=[xT_b[k][b]], W=[s])
            for b in range(BPU):
                s = sq[b % 2]
                p = ps()
                for k in range(KC):
                    M(lambda e: e.matmul(p[:, :], lhsT=ones_bf[:, :], rhs=s[:, k * 512:(k + 1) * 512],
                                         start=(k == 0), stop=(k == KC - 1)), R=[ones_bf, s], W=[p])
                A(lambda e: e.activation(out=lnv[:, :], in_=p[:, :], func=AF.Ln, scale=1.0 / D, bias=EPS),
                  R=[p], W=[lnv])
                r = rstd[b % 2]
                A(lambda e: e.activation(out=r[:, :], in_=lnv[:, :], func=AF.Exp, scale=-0.5), R=[lnv], W=[r])
                for k in range(KC):
                    V(lambda e: e.scalar_tensor_tensor(out=hv(k, b * 512, 512), in0=xv(k, b),
                                                       scalar=g_col[:, l * KC + k: l * KC + k + 1], in1=r[:, :],
                                                       op0=ALU.mult, op1=ALU.mult),
                      R=[xT_b[k][b], g_col, r], W=[hT_b[b]])

        def proj_tok(p, n_out, wfn, t, c0=0, wt=None, pcol=0):
            for k in range(KC):
                M(lambda e: e.matmul(p[:, pcol:pcol + n_out], lhsT=hv(k, t * 128, 128), rhs=wfn(k, c0, n_out),
                                     start=(k == 0), stop=(k == KC - 1)), R=[hT_b[t // 4], wt], W=[p])

        def transposes_to(dst_fn, src, nchunks, Rsrc, evac="act", p=None):
            if p is None:
                p = ps()
            pb = p[:, :].bitcast(BF16)
            for c in range(nchunks):
                M(lambda e: e.transpose(out=pb[:, c * 128:(c + 1) * 128], in_=src[:, c * 128:(c + 1) * 128],
                                        identity=ident_bf[:, :]), R=[Rsrc, ident_bf], W=[p])
            return p, pb

        def run_pipelined(gens, depth=2):
            if not PIPE:
                depth = 1
            tokens = set()
            active, blocked, idx = [], {}, 0
            while active or idx < len(gens):
                while len(active) < depth and idx < len(gens):
                    active.append(gens[idx])
                    idx += 1
                progressed = False
                for g in list(active):
                    need = blocked.get(id(g))
                    if need is not None:
                        if need not in tokens:
                            continue
                        blocked[id(g)] = None
                    progressed = True
                    try:
                        r = next(g)
                    except StopIteration:
                        active.remove(g)
                        continue
                    if r is not None:
                        kind, tok = r
                        if kind == "set":
                            tokens.add(tok)
                        elif tok not in tokens:
                            blocked[id(g)] = tok
                assert progressed, "pipeline deadlock"

        def stagger(mk, n):
            def wrap(t):
                if t > 0 and STAGGER:
                    yield ("need", ("mid", t - 1))
                yield from mk(t)
            return [wrap(t) for t in range(n)]

        def stage_attn(u, l):
            arena.reset()
            first_unit = (u % upseq == 0)
            sgA = arena.alloc("sgA", TPU * 512, BF16)
            sqbs = [arena.alloc("sqb%d" % i, 640) for i in range(2)]
            sss = [arena.alloc("ss%d" % i, 16) for i in range(2)]
            qkns = [arena.alloc("qkn%d" % i, 640) for i in range(2)]
            tmp1s = [arena.alloc("tmp1%d" % i, 640) for i in range(2)]
            tmp2s = [arena.alloc("tmp2%d" % i, 640) for i in range(2)]
            qkr = [arena.alloc("qkr%d" % i, 640, BF16) for i in range(2)]
            qTs = [arena.alloc("qT%d" % i, 512, BF16) for i in range(2)]
            Eb = [[arena.alloc("E%d_%d" % (i, j), 512, BF16) for j in range(4)] for i in range(2)]
            dens = [arena.alloc("den%d" % i, 16) for i in range(2)]
            yAs = [arena.alloc("yA%d" % i, 512) for i in range(2)]
            y_a = [arena.alloc("y_a%d" % i, 512, BF16) for i in range(2)]
            jq, wq = w_next("A_qkv")
            jk, wk = w_next("A_kv")
            jg, wg = w_next("A_g")
            wqf, wkf, wgf = wv(wq, 512), wv(wk, 256), wv(wg, 512)
            for t in range(TPU):
                p = ps()
                proj_tok(p, 512, wgf, t, wt=wg)
                A(lambda e: e.activation(out=sgA[:, t * 512:(t + 1) * 512], in_=p[:, :], func=AF.Silu), R=[p], W=[sgA])
            w_release(jg)

            def tile(t):
                gt = u * TPU + t
                first = first_unit and t == 0
                kT_c, kT_p = kT_st[l][gt % 3], kT_st[l][(gt + 2) % 3]
                v_c, v_p = v_st[l][gt % 3], v_st[l][(gt + 2) % 3]
                sqb, ss, qkn, tmp1, tmp2 = sqbs[t % 2], sss[t % 2], qkns[t % 2], tmp1s[t % 2], tmp2s[t % 2]
                den, yA = dens[t % 2], yAs[t % 2]
                p1, p2 = ps(), ps()
                proj_tok(p1, 512, wqf, t, wt=wq)
                proj_tok(p2, 256, wkf, t, wt=wk)
                yield
                A(lambda e: e.activation(out=sqb[:, 0:512], in_=p1[:, :], func=AF.Square), R=[p1], W=[sqb])
                A(lambda e: e.activation(out=sqb[:, 512:640], in_=p2[:, 0:128], func=AF.Square), R=[p2], W=[sqb])
                yield
                V(lambda e: e.reduce_sum(out=ss[:, 0:10], in_=_v3(sqb[:, :], 10, 64), axis=AX.X), R=[sqb], W=[ss])
                yield
                A(lambda e: e.activation(out=ss[:, 0:10], in_=ss[:, 0:10], func=AF.Ln, scale=1.0 / 64, bias=EPS),
                  R=[ss], W=[ss])
                A(lambda e: e.activation(out=ss[:, 0:10], in_=ss[:, 0:10], func=AF.Exp, scale=-0.5), R=[ss], W=[ss])
                yield
                V(lambda e: e.tensor_tensor(out=_v3(qkn[:, 0:512], 8, 64), in0=_v3(p1[:, :], 8, 64),
                                            in1=_bc_last(ss[:, 0:8], 64), op=ALU.mult), R=[p1, ss], W=[qkn])
                V(lambda e: e.tensor_tensor(out=_v3(qkn[:, 512:640], 2, 64), in0=_v3(p2[:, 0:128], 2, 64),
                                            in1=_bc_last(ss[:, 8:10], 64), op=ALU.mult), R=[p2, ss], W=[qkn])
                yield
                A(lambda e: e.activation(out=_v3(v_c[:, :], 2, 65)[:, :, 0:64], in_=_v3(p2[:, 128:256], 2, 64),
                                         func=AF.Copy), R=[p2], W=[v_c])
                V(lambda e: e.tensor_tensor(out=qkn[:, :], in0=qkn[:, :], in1=qkgain[:, l * 640:(l + 1) * 640],
                                            op=ALU.mult), R=[qkn, qkgain], W=[qkn])
                yield
                cs_t = cs2[:, t * 64:(t + 1) * 64]
                sn_t = sn2[:, t * 64:(t + 1) * 64]
                V(lambda e: e.tensor_tensor(out=_v3(tmp1[:, :], 10, 64), in0=_v3(qkn[:, :], 10, 64),
                                            in1=_bc_mid(cs_t, 10), op=ALU.mult), R=[qkn, cs2], W=[tmp1])
                V(lambda e: e.tensor_tensor(out=_v3(tmp2[:, :], 10, 64)[:, :, 0:32], in0=_v3(qkn[:, :], 10, 64)[:, :, 32:64],
                                            in1=_bc_mid(sn_t[:, 0:32], 10), op=ALU.mult), R=[qkn, sn2], W=[tmp2])
                V(lambda e: e.tensor_tensor(out=_v3(tmp2[:, :], 10, 64)[:, :, 32:64], in0=_v3(qkn[:, :], 10, 64)[:, :, 0:32],
                                            in1=_bc_mid(sn_t[:, 32:64], 10), op=ALU.mult), R=[qkn, sn2], W=[tmp2])
                yield
                qk = qkr[t % 2]
                V(lambda e: e.tensor_tensor(
                    out=qk[:, 0:512].rearrange("p (a g d) -> p g a d", a=4, g=2, d=64),
                    in0=tmp1[:, 0:512].rearrange("p (g a d) -> p g a d", g=2, a=4, d=64),
                    in1=tmp2[:, 0:512].rearrange("p (g a d) -> p g a d", g=2, a=4, d=64), op=ALU.add),
                  R=[tmp1, tmp2], W=[qk])
                V(lambda e: e.tensor_tensor(out=qk[:, 512:640], in0=tmp1[:, 512:640], in1=tmp2[:, 512:640], op=ALU.add),
                  R=[tmp1, tmp2], W=[qk])
                yield
                pT, pTb = transposes_to(None, qk, 5, qk)
                yield ("set", ("mid", t))
                qT = qTs[t % 2]
                A(lambda e: e.activation(out=qT[:, :], in_=pTb[:, 0:512], func=AF.Copy), R=[pT], W=[qT])
                for g in range(2):
                    V(lambda e: e.tensor_copy(out=kT_c[g * 64:(g + 1) * 64, g * 128:(g + 1) * 128],
                                              in_=pTb[g * 64:(g + 1) * 64, 512:640]), R=[pT], W=[kT_c])
                yield ("set", ("kv", gt))
                if not first and t > 0:
                    yield ("need", ("kv", gt - 1))
                E = Eb[t % 2]
                blocks = [("c", kT_c, v_c)] if first else [("p", kT_p, v_p), ("c", kT_c, v_c)]
                for g in range(2):
                    for bi, (tag, kTt, _) in enumerate(blocks):
                        p = ps()
                        mb = mb_cur if tag == "c" else mb_prv
                        M(lambda e: e.matmul(p[:, :], lhsT=kTt[:, g * 128:(g + 1) * 128], rhs=qT[:, :],
                                             start=True, stop=False), R=[kTt, qT], W=[p])
                        M(lambda e: e.matmul(p[:, :], lhsT=ident_bf[:, :], rhs=mb[:, :],
                                             start=False, stop=True), R=[ident_bf, mb], W=[p])
                        yield
                        Et = E[g * 2 + bi]
                        A(lambda e: e.activation(out=Et[:, :], in_=p[:, :], func=AF.Exp, scale=0.125), R=[p], W=[Et])
                yield
                po = [ps(), ps()]
                for h in range(8):
                    g, a = h // 4, h % 4
                    for bi, (tag, _, vt) in enumerate(blocks):
                        Et = E[g * 2 + bi]
                        M(lambda e: e.matmul(po[g][:, a * 65:(a + 1) * 65], lhsT=Et[:, a * 128:(a + 1) * 128],
                                             rhs=vt[:, g * 65:(g + 1) * 65], start=(bi == 0),
                                             stop=(bi == len(blocks) - 1)), R=[Et, vt], W=[po[g]])
                yield
                for g in range(2):
                    V(lambda e: e.tensor_tensor(out=den[:, g * 4:(g + 1) * 4], in0=_v3(po[g][:, 0:260], 4, 65)[:, :, 64],
                                                in1=expsink[:, l * 8 + g * 4: l * 8 + (g + 1) * 4], op=ALU.add),
                      R=[po[g], expsink], W=[den])
                V(lambda e: e.reciprocal(out=den[:, 0:8], in_=den[:, 0:8]), R=[den], W=[den])
                for g in range(2):
                    V(lambda e: e.tensor_tensor(out=_v3(yA[:, g * 256:(g + 1) * 256], 4, 64),
                                                in0=_v3(po[g][:, 0:260], 4, 65)[:, :, 0:64],
                                                in1=_bc_last(den[:, g * 4:(g + 1) * 4], 64), op=ALU.mult),
                      R=[po[g], den], W=[yA])
                ya = y_a[t % 2]
                V(lambda e: e.tensor_tensor(out=ya[:, :], in0=yA[:, :], in1=sgA[:, t * 512:(t + 1) * 512], op=ALU.mult),
                  R=[yA, sgA], W=[ya])
                yield
                pT2, pT2b = transposes_to(None, ya, 4, ya)
                yield
                A(lambda e: e.activation(out=_v3(yT[0][:, :], 4, UNIT)[:, :, t * 128:(t + 1) * 128],
                                         in_=_v3(pT2b[:, 0:512], 4, 128), func=AF.Copy), R=[pT2], W=[yT[0]])

            run_pipelined(stagger(tile, TPU))
            w_release(jq)
            w_release(jk)

        def stage_gla(u, l):
            arena.reset()
            first_unit = (u % upseq == 0)
            sgB = arena.alloc("sgB", TPU * 512, BF16)
            D2 = lambda name, n, dt=F32: [arena.alloc("%s%d" % (name, i), n, dt) for i in range(2)]
            e1s, sps, Eqs, Eks, Ees, decs = D2("e1", 256), D2("sp", 256), D2("Eq", 256), D2("Ek", 256), D2("Ee", 256), D2("dec", 2)
            qds, kis, kes = D2("qd", 256, BF16), D2("ki", 256, BF16), D2("ke", 256, BF16)
            vbfs, qkTs, kzs, ATs = D2("vbf", 512, BF16), D2("qkT", 256, BF16), D2("kz", 512, BF16), D2("AT", 512, BF16)
            sqos, ssbs, t1s, ybs = D2("sqo", 512), D2("ssb", 4), D2("t1", 512), D2("yb", 512, BF16)
            gdTs = D2("gdT", 128, BF16)
            S, Sb = S_gla[l], S_gla_bf[l]
            jg, wg = w_next("B_g")
            wgf = wv(wg, 512)
            for t in range(TPU):
                p = ps()
                proj_tok(p, 512, wgf, t, wt=wg)
                A(lambda e: e.activation(out=sgB[:, t * 512:(t + 1) * 512], in_=p[:, :], func=AF.Silu), R=[p], W=[sgB])
            w_release(jg)
            jqk, wqk = w_next("B_qk")
            jv, wvv = w_next("B_v")
            jd, wd = w_next("B_d")
            wqkf, wvf = wv(wqk, 512), wv(wvv, 512)
            for kz_ in kzs:
                V(lambda e: e.memset(kz_[:, :], 0.0), W=[kz_])
            for g_ in gdTs:
                V(lambda e: e.memset(g_[0:32, :], 1.0), W=[g_])
            if first_unit:
                V(lambda e: e.memset(S[:, :], 0.0), W=[S])
                V(lambda e: e.memset(Sb[:, :], 0.0), W=[Sb])

            def tile(t):
                first = first_unit and t == 0
                i2 = t % 2
                e1, sp, Eq, Ek, Ee, dec = e1s[i2], sps[i2], Eqs[i2], Eks[i2], Ees[i2], decs[i2]
                qd, ki, ke, vbf, qkT, kz, AT = qds[i2], kis[i2], kes[i2], vbfs[i2], qkTs[i2], kzs[i2], ATs[i2]
                sqo, ssb, t1, yb, gdTt = sqos[i2], ssbs[i2], t1s[i2], ybs[i2], gdTs[i2]
                pqk, pv, pl = ps(), ps(), ps()
                proj_tok(pqk, 512, wqkf, t, wt=wqk)
                proj_tok(pv, 512, wvf, t, wt=wvv)
                for k in range(KC):
                    M(lambda e: e.matmul(pl[0:16, 256:384], lhsT=wd[:, k * 16:(k + 1) * 16], rhs=hv(k, t * 128, 128),
                                         start=(k == 0), stop=(k == KC - 1)), R=[wd, hT_b[t // 4]], W=[pl])
                yield
                A(lambda e: e.activation(out=gdTt[0:16, :], in_=pl[0:16, 256:384], func=AF.Copy), R=[pl], W=[gdTt])
                A(lambda e: e.activation(out=vbf[:, :], in_=pv[:, :], func=AF.Copy), R=[pv], W=[vbf])
                yield
                M(lambda e: e.matmul(pl[:, 0:256], lhsT=gdTt[0:17, :], rhs=wup_bf[0:17, l * 256:(l + 1) * 256],
                                     start=True, stop=True), R=[gdTt, wup_bf], W=[pl])
                yield
                A(lambda e: e.activation(out=e1[:, :], in_=pl[:, 0:256], func=AF.Exp, scale=-1.0), R=[pl], W=[e1])
                A(lambda e: e.activation(out=sp[:, :], in_=e1[:, :], func=AF.Ln, bias=1.0), R=[e1], W=[sp])
                yield
                pc = ps()
                M(lambda e: e.matmul(pc[:, 0:256], lhsT=tri_f[:, :], rhs=sp[:, :], start=True, stop=True),
                  R=[tri_f, sp], W=[pc])
                M(lambda e: e.matmul(pc[:, 256:512], lhsT=nsg_f[:, :], rhs=sp[:, :], start=True, stop=True),
                  R=[nsg_f, sp], W=[pc])
                for m in range(2):
                    M(lambda e: e.matmul(pl[:, 384 + m:385 + m], lhsT=sp[:, m * 128:(m + 1) * 128], rhs=ones_f[:, 0:1],
                                         start=True, stop=True), R=[sp, ones_f], W=[pl])
                yield
                A(lambda e: e.activation(out=Eq[:, :], in_=pc[:, 0:256], func=AF.Exp, scale=-1.0 / 16), R=[pc], W=[Eq])
                A(lambda e: e.activation(out=Ek[:, :], in_=pc[:, 0:256], func=AF.Exp, scale=1.0 / 16), R=[pc], W=[Ek])
                yield
                A(lambda e: e.activation(out=Ee[:, :], in_=pc[:, 256:512], func=AF.Exp, scale=1.0 / 16), R=[pc], W=[Ee])
                A(lambda e: e.activation(out=dec[:, 0:2], in_=pl[:, 384:386], func=AF.Exp, scale=-1.0 / 16), R=[pl], W=[dec])
                yield
                V(lambda e: e.scalar_tensor_tensor(out=qd[:, :], in0=pqk[:, 0:256], scalar=0.125, in1=Eq[:, :],
                                                   op0=ALU.mult, op1=ALU.mult), R=[pqk, Eq], W=[qd])
                V(lambda e: e.tensor_tensor(out=ki[:, :], in0=pqk[:, 256:512], in1=Ek[:, :], op=ALU.mult),
                  R=[pqk, Ek], W=[ki])
                yield
                V(lambda e: e.tensor_tensor(out=ke[:, :], in0=pqk[:, 256:512], in1=Ee[:, :], op=ALU.mult),
                  R=[pqk, Ee], W=[ke])
                pT = ps()
                pTb = pT[:, :].bitcast(BF16)
                for c in range(2):
                    M(lambda e: e.transpose(out=pTb[:, c * 128:(c + 1) * 128], in_=qd[:, c * 128:(c + 1) * 128],
                                            identity=ident_bf[:, :]), R=[qd, ident_bf], W=[pT])
                for c in range(2):
                    M(lambda e: e.transpose(out=pTb[:, (2 + c) * 128:(3 + c) * 128], in_=ki[:, c * 128:(c + 1) * 128],
                                            identity=ident_bf[:, :]), R=[ki, ident_bf], W=[pT])
                yield ("set", ("mid", t))
                A(lambda e: e.activation(out=qkT[:, :], in_=pTb[:, 0:256], func=AF.Copy), R=[pT], W=[qkT])
                for r in range(2):
                    A(lambda e: e.activation(
                        out=kz[r * 64:(r + 1) * 64, :].rearrange("p (m r v) -> p m r v", m=2, r=2, v=128)[:, :, r, :],
                        in_=_v3(pTb[r * 64:(r + 1) * 64, 256:512], 2, 128), func=AF.Copy), R=[pT], W=[kz])
                yield
                pA = ps()
                for h in range(4):
                    m, r = h // 2, h % 2
                    M(lambda e: e.matmul(pA[:, h * 128:(h + 1) * 128], lhsT=kz[:, h * 128:(h + 1) * 128],
                                         rhs=qkT[:, m * 128:(m + 1) * 128], start=True, stop=True),
                      R=[qkT, kz], W=[pA])
                pU = ps()
                for h in range(4):
                    m = h // 2
                    M(lambda e: e.matmul(pU[:, h * 128:(h + 1) * 128], lhsT=ke[:, m * 128:(m + 1) * 128],
                                         rhs=vbf[:, h * 128:(h + 1) * 128], start=True, stop=True), R=[ke, vbf], W=[pU])
                yield
                V(lambda e: e.tensor_tensor(out=_v3(AT[:, :], 4, 128), in0=_v3(pA[:, :], 4, 128),
                                            in1=_bc_mid(tri_bf[:, :], 4), op=ALU.mult), R=[pA, tri_bf], W=[AT])
                yield
                if t > 0:
                    yield ("need", ("S", t - 1))
                po = ps()
                for h in range(4):
                    m, r = h // 2, h % 2
                    M(lambda e: e.matmul(po[:, h * 128:(h + 1) * 128], lhsT=AT[:, h * 128:(h + 1) * 128],
                                         rhs=vbf[:, h * 128:(h + 1) * 128], start=True, stop=first), R=[AT, vbf], W=[po])
                    if not first:
                        M(lambda e: e.matmul(po[:, h * 128:(h + 1) * 128], lhsT=qkT[:, m * 128:(m + 1) * 128],
                                             rhs=Sb[:, h * 128:(h + 1) * 128], start=False, stop=True),
                          R=[qkT, Sb], W=[po])
                yield
                for h in range(4):
                    m, r = h // 2, h % 2
                    V(lambda e: e.scalar_tensor_tensor(out=S[r * 64:(r + 1) * 64, h * 128:(h + 1) * 128],
                                                       in0=S[r * 64:(r + 1) * 64, h * 128:(h + 1) * 128],
                                                       scalar=dec[r * 64:(r + 1) * 64, m:m + 1],
                                                       in1=pU[r * 64:(r + 1) * 64, h * 128:(h + 1) * 128],
                                                       op0=ALU.mult, op1=ALU.add), R=[S, dec, pU], W=[S])
                yield
                A(lambda e: e.activation(out=Sb[:, :], in_=S[:, :], func=AF.Copy), R=[S], W=[Sb])
                yield ("set", ("S", t))
                A(lambda e: e.activation(out=sqo[:, :], in_=po[:, :], func=AF.Square), R=[po], W=[sqo])
                yield
                V(lambda e: e.reduce_sum(out=ssb[:, 0:4], in_=_v3(sqo[:, :], 4, 128), axis=AX.X), R=[sqo], W=[ssb])
                yield
                A(lambda e: e.activation(out=ssb[:, 0:4], in_=ssb[:, 0:4], func=AF.Ln, scale=1.0 / 128, bias=EPS),
                  R=[ssb], W=[ssb])
                A(lambda e: e.activation(out=ssb[:, 0:4], in_=ssb[:, 0:4], func=AF.Exp, scale=-0.5), R=[ssb], W=[ssb])
                yield
                V(lambda e: e.tensor_tensor(out=_v3(t1[:, :], 4, 128), in0=_v3(po[:, :], 4, 128),
                                            in1=_bc_last(ssb[:, 0:4], 128), op=ALU.mult), R=[po, ssb], W=[t1])
                yield
                V(lambda e: e.tensor_tensor(out=_v3(t1[:, :], 4, 128), in0=_v3(t1[:, :], 4, 128),
                                            in1=_bc_mid(gainB[:, l * 128:(l + 1) * 128], 4), op=ALU.mult),
                  R=[t1, gainB], W=[t1])
                V(lambda e: e.tensor_tensor(out=yb[:, :], in0=t1[:, :], in1=sgB[:, t * 512:(t + 1) * 512], op=ALU.mult),
                  R=[t1, sgB], W=[yb])
                yield
                pT2, pT2b = transposes_to(None, yb, 4, yb)
                yield
                A(lambda e: e.activation(out=_v3(yT[1][:, :], 4, UNIT)[:, :, t * 128:(t + 1) * 128],
                                         in_=_v3(pT2b[:, 0:512], 4, 128), func=AF.Copy), R=[pT2], W=[yT[1]])

            run_pipelined(stagger(tile, TPU))
            w_release(jqk)
            w_release(jv)
            w_release(jd)

        def stage_ssd(u, l):
            arena.reset()
            first_unit = (u % upseq == 0)
            sgC = arena.alloc("sgC", TPU * 512, BF16)
            xbcT = arena.alloc("xbcT", 8 * UNIT, BF16)
            mark = arena.off
            raws = [arena.alloc("raw%d" % i, 516) for i in range(3)]
            accs = [arena.alloc("acc%d" % i, 512) for i in range(3)]
            S, Sb = S_ssd[l], S_ssd_bf[l]
            ct = ctail[l]
            jz, wz = w_next("C_z")
            wzf = wv(wz, 512)
            for t in range(TPU):
                p = ps()
                proj_tok(p, 512, wzf, t, wt=wz)
                A(lambda e: e.activation(out=sgC[:, t * 512:(t + 1) * 512], in_=p[:, :], func=AF.Silu), R=[p], W=[sgC])
            w_release(jz)
            jx, wx = w_next("C_x")
            jbc, wbc = w_next("C_bc")
            if first_unit:
                V(lambda e: e.memset(ct[:, :], 0.0), W=[ct])

            def conv(i, b, ch):
                wt = wx if ch < 4 else wbc
                c0 = (ch % 4) * 128
                p = ps()
                for k in range(KC):
                    M(lambda e: e.matmul(p[:, :], lhsT=wt[:, k * 512 + c0: k * 512 + c0 + 128], rhs=hv(k, b * 512, 512),
                                         start=(k == 0), stop=(k == KC - 1)), R=[wt, hT_b[b]], W=[p])
                yield
                rw, ac = raws[i % 3], accs[i % 3]
                ci = l * 32 + ch * 4
                A(lambda e: e.activation(out=rw[:, 3:515], in_=p[:, :], func=AF.Copy), R=[p], W=[rw])
                A(lambda e: e.activation(out=ac[:, :], in_=p[:, :], func=AF.Identity, scale=cw[:, ci + 3:ci + 4],
                                         bias=cb[:, l * 8 + ch:l * 8 + ch + 1]), R=[p, cw, cb], W=[ac])
                yield
                if b > 0:
                    yield ("need", ("ct", b - 1, ch))
                V(lambda e: e.tensor_copy(out=rw[:, 0:3], in_=ct[:, ch * 3:(ch + 1) * 3]), R=[ct], W=[rw])
                yield
                for kk in range(3):
                    V(lambda e: e.scalar_tensor_tensor(out=ac[:, :], in0=rw[:, kk:kk + 512], scalar=cw[:, ci + kk:ci + kk + 1],
                                                       in1=ac[:, :], op0=ALU.mult, op1=ALU.add), R=[rw, cw, ac], W=[ac])
                    yield
                V(lambda e: e.tensor_copy(out=ct[:, ch * 3:(ch + 1) * 3], in_=rw[:, 512:515]), R=[rw], W=[ct])
                yield ("set", ("ct", b, ch))
                A(lambda e: e.activation(out=xbcT[:, ch * UNIT + b * 512: ch * UNIT + (b + 1) * 512], in_=ac[:, :],
                                         func=AF.Silu), R=[ac], W=[xbcT])

            run_pipelined([conv(b * 8 + ch, b, ch) for b in range(BPU) for ch in range(8)], depth=3)
            w_release(jx)
            w_release(jbc)
            jdt, wdt = w_next("C_dt")
            arena.rewind(mark, raws + accs)
            D2 = lambda name, n, dt=F32: [arena.alloc("%s%d" % (name, i), n, dt) for i in range(2)]
            x1s, dtts, aas, sms, sscs = D2("x1", 8), D2("dtt", 8), D2("aa", 8), D2("sm", 24), D2("ssc", 2)
            Rms, xBs, xdts, xdds = D2("Rm", 1024), D2("xB", 768, BF16), D2("xdt", 512, BF16), D2("xdd", 512, BF16)
            ycs = D2("yc", 512, BF16)
            Lm = arena.alloc("Lm", 1024)
            CBm = arena.alloc("CBm", 256)
            Wm = arena.alloc("Wm", 1024, BF16)
            if first_unit:
                V(lambda e: e.memset(S[:, :], 0.0), W=[S])
                V(lambda e: e.memset(Sb[:, :], 0.0), W=[Sb])

            def xc(c, t):
                return xbcT[:, c * UNIT + t * 128: c * UNIT + (t + 1) * 128]

            def tile(t):
                first = first_unit and t == 0
                i2 = t % 2
                x1, dtt, aa, sm, ssc = x1s[i2], dtts[i2], aas[i2], sms[i2], sscs[i2]
                Rm, xB, xdt, xdd, yc = Rms[i2], xBs[i2], xdts[i2], xdds[i2], ycs[i2]
                y1 = T(Rm[:, 0:512], Rm.b)
                y2 = T(Rm[:, 512:1024], Rm.b)
                bank = lambda j: psum[4 * i2 + j]
                pd = bank(0)
                for k in range(KC):
                    M(lambda e: e.matmul(pd[:, 0:8], lhsT=hv(k, t * 128, 128), rhs=wdt[:, k * 8:(k + 1) * 8],
                                         start=(k == 0), stop=(k == KC - 1)), R=[hT_b[t // 4], wdt], W=[pd])
                yield
                V(lambda e: e.tensor_tensor(out=x1[:, 0:8], in0=pd[:, 0:8], in1=dtb[:, l * 8:(l + 1) * 8], op=ALU.add),
                  R=[pd, dtb], W=[x1])
                yield
                A(lambda e: e.activation(out=x1[:, 0:8], in_=x1[:, 0:8], func=AF.Exp), R=[x1], W=[x1])
                A(lambda e: e.activation(out=dtt[:, 0:8], in_=x1[:, 0:8], func=AF.Ln, bias=1.0), R=[x1], W=[dtt])
                yield
                V(lambda e: e.tensor_tensor(out=aa[:, 0:8], in0=dtt[:, 0:8], in1=Abc[:, l * 8:(l + 1) * 8], op=ALU.mult),
                  R=[dtt, Abc], W=[aa])
                V(lambda e: e.tensor_tensor(out=_v3(Rm[:, :], 8, 128), in0=_bc_mid(tri_f[:, :], 8),
                                            in1=_bc_last(aa[:, 0:8], 128), op=ALU.mult), R=[tri_f, aa], W=[Rm])
                yield
                pseg = [bank(1), bank(2)]
                for hf in range(2):
                    M(lambda e: e.matmul(pseg[hf][:, :], lhsT=sg_f[:, :], rhs=Rm[:, hf * 512:(hf + 1) * 512],
                                         start=True, stop=True), R=[sg_f, Rm], W=[pseg[hf]])
                M(lambda e: e.matmul(pd[:, 8:16], lhsT=tri_f[:, :], rhs=aa[:, 0:8], start=True, stop=True),
                  R=[tri_f, aa], W=[pd])
                M(lambda e: e.matmul(pd[:, 16:24], lhsT=sg_f[:, :], rhs=aa[:, 0:8], start=True, stop=True),
                  R=[sg_f, aa], W=[pd])
                M(lambda e: e.matmul(pd[:, 24:32], lhsT=ones_f[:, :], rhs=aa[:, 0:8], start=True, stop=True),
                  R=[ones_f, aa], W=[pd])
                pT = bank(3)
                pTb = pT[:, :].bitcast(BF16)
                for c in range(6):
                    M(lambda e: e.transpose(out=pTb[:, c * 128:(c + 1) * 128], in_=xc(c, t), identity=ident_bf[:, :]),
                      R=[xbcT, ident_bf], W=[pT])
                yield
                if t > 0:
                    yield ("need", ("LmF", t - 1))
                for hf in range(2):
                    A(lambda e: e.activation(out=Lm[:, hf * 512:(hf + 1) * 512], in_=pseg[hf][:, :], func=AF.Exp),
                      R=[pseg[hf]], W=[Lm])
                    yield
                A(lambda e: e.activation(out=sm[:, 0:24], in_=pd[:, 8:32], func=AF.Exp), R=[pd], W=[sm])
                A(lambda e: e.activation(out=xB[:, :], in_=pTb[:, 0:768], func=AF.Copy), R=[pT], W=[xB])
                pcb = bank(1)
                for g in range(2):
                    M(lambda e: e.matmul(pcb[:, g * 128:(g + 1) * 128], lhsT=xc(4 + g, t), rhs=xc(6 + g, t),
                                         start=True, stop=True), R=[xbcT], W=[pcb])
                yield ("set", ("mid", t))
                V(lambda e: e.tensor_tensor(out=_v3(CBm[:, :], 2, 128), in0=_v3(pcb[:, 0:256], 2, 128),
                                            in1=_bc_mid(tri_bf[:, :], 2), op=ALU.mult), R=[pcb, tri_bf], W=[CBm])
                yield
                if t > 0:
                    yield ("need", ("WmF", t - 1))
                V(lambda e: e.tensor_tensor(
                    out=Wm[:, :].rearrange("p (g a l) -> p g a l", g=2, a=4, l=128),
                    in0=Lm[:, :].rearrange("p (g a l) -> p g a l", g=2, a=4, l=128),
                    in1=_v3(CBm[:, :], 2, 128).unsqueeze(2).broadcast_to([128, 2, 4, 128]), op=ALU.mult),
                  R=[Lm, CBm], W=[Wm])
                yield ("set", ("LmF", t))
                V(lambda e: e.tensor_tensor(out=_v3(xdt[:, :], 8, 64), in0=_v3(xB[:, 0:512], 8, 64),
                                            in1=_bc_last(dtt[:, 0:8], 64), op=ALU.mult), R=[xB, dtt], W=[xdt])
                V(lambda e: e.tensor_tensor(out=_v3(xdd[:, :], 8, 64), in0=_v3(xdt[:, :], 8, 64),
                                            in1=_bc_last(sm[:, 8:16], 64), op=ALU.mult), R=[xdt, sm], W=[xdd])
                yield
                py = bank(2)
                for h in range(8):
                    M(lambda e: e.matmul(py[:, h * 64:(h + 1) * 64], lhsT=Wm[:, h * 128:(h + 1) * 128],
                                         rhs=xdt[:, h * 64:(h + 1) * 64], start=True, stop=True), R=[Wm, xdt], W=[py])
                yield ("set", ("WmF", t))
                pu = bank(3)
                for g in range(2):
                    M(lambda e: e.matmul(pu[:, g * 256:(g + 1) * 256], lhsT=xB[:, 512 + g * 128:512 + (g + 1) * 128],
                                         rhs=xdd[:, g * 256:(g + 1) * 256], start=True, stop=True), R=[xB, xdd], W=[pu])
                yield
                V(lambda e: e.tensor_tensor(out=_v3(y2[:, :], 8, 64), in0=_v3(xB[:, 0:512], 8, 64),
                                            in1=_bc_last(Dbc[:, l * 8:(l + 1) * 8], 64), op=ALU.mult), R=[xB, Dbc], W=[y2])
                yield
                if t > 0:
                    yield ("need", ("S", t - 1))
                if not first:
                    pyo = bank(0)
                    for g in range(2):
                        M(lambda e: e.matmul(pyo[:, g * 256:(g + 1) * 256], lhsT=xc(6 + g, t), rhs=Sb[:, g * 256:(g + 1) * 256],
                                             start=True, stop=True), R=[xbcT, Sb], W=[pyo])
                    yield
                V(lambda e: e.tensor_tensor(out=_v3(S[:, :], 8, 64), in0=_v3(S[:, :], 8, 64),
                                            in1=_bc_last(sm[:, 16:24], 64), op=ALU.mult), R=[S, sm], W=[S])
                V(lambda e: e.tensor_tensor(out=S[:, :], in0=S[:, :], in1=pu[:, :], op=ALU.add), R=[S, pu], W=[S])
                yield
                A(lambda e: e.activation(out=Sb[:, :], in_=S[:, :], func=AF.Copy), R=[S], W=[Sb])
                yield ("set", ("S", t))
                if not first:
                    V(lambda e: e.tensor_tensor(out=_v3(y1[:, :], 8, 64), in0=_v3(pyo[:, :], 8, 64),
                                                in1=_bc_last(sm[:, 0:8], 64), op=ALU.mult), R=[pyo, sm], W=[y1])
                    yield
                    V(lambda e: e.tensor_tensor(out=y1[:, :], in0=y1[:, :], in1=py[:, :], op=ALU.add), R=[y1, py], W=[y1])
                    yield
                    V(lambda e: e.tensor_tensor(out=y1[:, :], in0=y1[:, :], in1=y2[:, :], op=ALU.add), R=[y1, y2], W=[y1])
                else:
                    V(lambda e: e.tensor_tensor(out=y1[:, :], in0=y2[:, :], in1=py[:, :], op=ALU.add), R=[y2, py], W=[y1])
                yield
                V(lambda e: e.tensor_tensor(out=y1[:, :], in0=y1[:, :], in1=sgC[:, t * 512:(t + 1) * 512], op=ALU.mult),
                  R=[y1, sgC], W=[y1])
                yield
                A(lambda e: e.activation(out=y2[:, :], in_=y1[:, :], func=AF.Square), R=[y1], W=[y2])
                yield
                V(lambda e: e.reduce_sum(out=ssc[:, 0:2], in_=_v3(y2[:, :], 2, 256), axis=AX.X), R=[y2], W=[ssc])
                yield
                A(lambda e: e.activation(out=ssc[:, 0:2], in_=ssc[:, 0:2], func=AF.Ln, scale=1.0 / 256, bias=EPS),
                  R=[ssc], W=[ssc])
                A(lambda e: e.activation(out=ssc[:, 0:2], in_=ssc[:, 0:2], func=AF.Exp, scale=-0.5), R=[ssc], W=[ssc])
                yield
                V(lambda e: e.tensor_tensor(out=_v3(y1[:, :], 2, 256), in0=_v3(y1[:, :], 2, 256),
                                            in1=_bc_last(ssc[:, 0:2], 256), op=ALU.mult), R=[y1, ssc], W=[y1])
                yield
                V(lambda e: e.tensor_tensor(out=yc[:, :], in0=y1[:, :], in1=gainC[:, l * 512:(l + 1) * 512], op=ALU.mult),
                  R=[y1, gainC], W=[yc])
                yield
                pT2, pT2b = transposes_to(None, yc, 4, yc, p=bank(1))
                yield
                A(lambda e: e.activation(out=_v3(yT[2][:, :], 4, UNIT)[:, :, t * 128:(t + 1) * 128],
                                         in_=_v3(pT2b[:, 0:512], 4, 128), func=AF.Copy), R=[pT2], W=[yT[2]])

            run_pipelined(stagger(tile, TPU))
            w_release(jdt)

        def stage_merge(u, l):
            arena.reset()
            tgs = [[arena.alloc("tg%d_%d" % (i, j), 512, BF16) for j in range(3)] for i in range(2)]
            mms = [[arena.alloc("mm%d_%d" % (i, j), 512) for j in range(3)] for i in range(2)]
            mT = arena.alloc("mT", KC * UNIT, BF16)
            mT_b = [mT.b, mT.b]
            cnt = 0
            for g4 in range(2):
                jj, wb, wm = [], [], []
                for br in range(3):
                    j, w = w_next("M_br%d_%d" % (br, g4))
                    jj.append(j)
                    wb.append(w)
                    j, w = w_next("M_mg%d_%d" % (br, g4))
                    jj.append(j)
                    wm.append(w)
                for dcl in range(4):
                    dc = g4 * 4 + dcl
                    for b in range(BPU):
                        par = cnt % 2
                        cnt += 1
                        for br in range(3):
                            pu_, pg = ps(), ps()
                            for k in range(4):
                                M(lambda e: e.matmul(pu_[:, :], lhsT=wb[br][:, k * 512 + dcl * 128: k * 512 + (dcl + 1) * 128],
                                                     rhs=yT[br][:, k * UNIT + b * 512: k * UNIT + (b + 1) * 512],
                                                     start=(k == 0), stop=(k == 3)), R=[wb[br], yT[br]], W=[pu_])
                            for k in range(KC):
                                M(lambda e: e.matmul(pg[:, :], lhsT=wm[br][:, k * 512 + dcl * 128: k * 512 + (dcl + 1) * 128],
                                                     rhs=hv(k, b * 512, 512), start=(k == 0), stop=(k == KC - 1)),
                                  R=[wm[br], hT_b[b]], W=[pg])
                            tg, mm = tgs[par][br], mms[par][br]
                            A(lambda e: e.activation(out=tg[:, :], in_=pg[:, :], func=AF.Tanh, scale=0.5), R=[pg], W=[tg])
                            V(lambda e: e.scalar_tensor_tensor(out=mm[:, :], in0=tg[:, :], scalar=1.0, in1=pu_[:, :],
                                                               op0=ALU.add, op1=ALU.mult), R=[tg, pu_], W=[mm])
                        m0, m1, m2 = mms[par]
                        V(lambda e: e.tensor_tensor(out=m0[:, :], in0=m0[:, :], in1=m1[:, :], op=ALU.add), R=[m0, m1], W=[m0])
                        V(lambda e: e.tensor_tensor(out=mT[:, dc * UNIT + b * 512: dc * UNIT + (b + 1) * 512], in0=m0[:, :],
                                                    in1=m2[:, :], op=ALU.add), R=[m0, m2], W=[mT_b[b]])
                for j in jj:
                    w_release(j)
            jo0, wo0 = w_next("O_0")
            jo1, wo1 = w_next("O_1")
            for b in range(BPU):
                for oc in range(KC):
                    wo, ocl = (wo0, wo1)[oc // 4], oc % 4
                    p = ps()
                    for k in range(KC):
                        M(lambda e: e.matmul(p[:, :], lhsT=wo[:, k * 512 + ocl * 128: k * 512 + (ocl + 1) * 128],
                                             rhs=mT[:, k * UNIT + b * 512: k * UNIT + (b + 1) * 512],
                                             start=(k == 0), stop=(k == KC - 1)), R=[wo, mT_b[b]], W=[p])
                    V(lambda e: e.scalar_tensor_tensor(out=xv(oc, b), in0=p[:, :], scalar=0.5, in1=xv(oc, b),
                                                       op0=ALU.mult, op1=ALU.add), R=[p, xT_b[oc][b]], W=[xT_b[oc][b]])
                    if l == depth - 1 and dbg is None:
                        store_x(u, oc, b)
                if l == depth - 1 and dbg is None and u + 1 < n_units:
                    load_x_block(u + 1, b)
            w_release(jo0)
            w_release(jo1)

        for u in range(n_units):
            if u == 0 or dbg is not None:
                load_x(u)
            rope_tables(u)
            for l in range(depth):
                stage_norm(l)
                stage_attn(u, l)
                if dbg == "attn":
                    break
                stage_gla(u, l)
                if dbg == "gla":
                    break
                stage_ssd(u, l)
                if dbg == "ssd":
                    break
                stage_merge(u, l)
                if dbg == "layer":
                    break
            if dbg is not None:
                break

        if dbg is not None:
            def dump(name, t, shape, dt=F32):
                o = nc.dram_tensor(name, shape, dt, kind="ExternalOutput").ap()
                C.dma("sp", o, t, s_dbg, dbg_cnt, R=[hT_b[0], hT_b[1], yT[0], yT[1], yT[2], cs2, sn2])
            dump("d_hT", hT[:, :], [128, KC * UNIT], BF16)
            dump("d_yT0", yT[0][:, :], [128, 4 * UNIT], BF16)
            dump("d_yT1", yT[1][:, :], [128, 4 * UNIT], BF16)
            dump("d_yT2", yT[2][:, :], [128, 4 * UNIT], BF16)
            dump("d_cs2", cs2[:, :], [128, TPU * 64])
            dump("d_sn2", sn2[:, :], [128, TPU * 64])
            for k in range(KC):
                for b in range(BPU):
                    store_x(0, k, b)
        for k in range(KC):
            for b in range(BPU):
                if o_cnt[k][b][0]:
                    C.E["sp"].wait_ge(s_o[k][b], o_cnt[k][b][0])
        if dbg_cnt[0]:
            C.E["sp"].wait_ge(s_dbg, dbg_cnt[0])
        if needed is not None:
            print("instructions:", C.n_inst, "sem-incs:", sum(len(v) for v in needed.values()), "sems:", C.nsem)
    return nc, C.waited


def build(n_seq, seq_len, depth, dbg=None):
    _, waited = build1(n_seq, seq_len, depth, dbg, None)
    nc, _ = build1(n_seq, seq_len, depth, dbg, waited)
    return nc


_INPUT_ORDER = ["norm_g", "w_in", "attn_q_norm", "attn_k_norm", "attn_sinks", "gla_w_gate_up", "gla_b_gate",
                "gla_out_norm", "ssd_conv_w", "ssd_conv_b", "ssd_dt_bias", "ssd_A_log", "ssd_D", "ssd_out_norm",
                "w_branch", "w_out"]


def kernel(**inputs):
    x = np.ascontiguousarray(np.asarray(inputs["x"], dtype=np.float32))
    positions = np.asarray(inputs["positions"]).astype(np.int32)
    B, S, dm = x.shape
    depth = int(np.asarray(inputs["w_in"]).shape[0])
    assert dm == D and B % NCORES == 0
    per = B // NCORES
    nc = build(per, S, depth)
    inv_freq = (np.float32(10000.0) ** (-(np.arange(0, 64, 2, dtype=np.float32)) / np.float32(64))).astype(np.float32)
    shared = {k: np.ascontiguousarray(np.asarray(inputs[k], dtype=np.float32)) for k in _INPUT_ORDER}
    shared["inv_freq"] = inv_freq.reshape(1, 32)
    in_maps = []
    for c in range(NCORES):
        xs = x[c * per:(c + 1) * per].reshape(per * S, D)
        m = dict(shared)
        m["xT"] = np.ascontiguousarray(xs.T)
        pos = positions[c * per:(c + 1) * per].reshape(per * S // UNIT, TPU, 128)
        m["pos"] = np.ascontiguousarray(pos.transpose(0, 2, 1))
        in_maps.append(m)
    res = run_bass_kernel_spmd(nc, in_maps, core_ids=list(range(NCORES)))
    outs = [np.asarray(r["outT"]).T.reshape(per, S, D) for r in res.results]
    return np.ascontiguousarray(np.concatenate(outs, axis=0).astype(np.float32))
```
